# Optimizing a Trainium2 kernel written in Bass

```python
import math
import jax, jax.numpy as jnp
from jax import lax
import numpy as np

D_MODEL = 1024
BATCH = 2
SEQ = 8192
DEPTH = 2

N_MIXERS = 2
ATTN_HEADS = 16
ATTN_HEAD_DIM = D_MODEL // ATTN_HEADS
ROT_DIM = ATTN_HEAD_DIM // 4
ROPE_THETA = 500000.0
MOBA_BLOCK = 256
MOBA_TOPK = 3
Q_CHUNK = 64
MLSTM_HEADS = 8
MLSTM_QK_DIM = D_MODEL // (2 * MLSTM_HEADS)
MLSTM_V_DIM = D_MODEL // MLSTM_HEADS
MLSTM_CHUNK = 64
GATE_SOFTCAP = 15.0
MLSTM_IN_WIDTH = 2 * MLSTM_HEADS * MLSTM_QK_DIM + MLSTM_HEADS * MLSTM_V_DIM + D_MODEL + 2 * MLSTM_HEADS
D_FF = ((8 * D_MODEL + 3 * 256 - 1) // (3 * 256)) * 256
RMS_EPS = 1e-6

kernel_name = "hybrid_moba_mlstm_swiglu"


def rmsnorm(x, w):
    xf = x.astype(jnp.float32)
    y = xf * lax.rsqrt(jnp.mean(xf * xf, axis=-1, keepdims=True) + RMS_EPS)
    return (y * w.astype(jnp.float32)).astype(x.dtype)


def rotary_tables(seq):
    pos = jnp.arange(seq, dtype=jnp.float32)
    inv_freq = ROPE_THETA ** (-jnp.arange(0, ROT_DIM, 2, dtype=jnp.float32) / ROT_DIM)
    ang = pos[:, None] * inv_freq[None, :]
    return jnp.cos(ang), jnp.sin(ang)


def partial_rotary(x, cos, sin):
    xf = x.astype(jnp.float32)
    half = ROT_DIM // 2
    x1, x2, rest = xf[..., :half], xf[..., half:ROT_DIM], xf[..., ROT_DIM:]
    rot = jnp.concatenate([x1 * cos - x2 * sin, x2 * cos + x1 * sin, rest], axis=-1)
    return rot.astype(x.dtype)


def moba_attention(q, k, v):
    B, H, S, Dh = q.shape
    nb = -(-S // MOBA_BLOCK)
    pad = nb * MOBA_BLOCK - S
    kp = jnp.pad(k, ((0, 0), (0, 0), (0, pad), (0, 0)))
    vp = jnp.pad(v, ((0, 0), (0, 0), (0, pad), (0, 0)))
    kb = kp.reshape(B, H, nb, MOBA_BLOCK, Dh)
    vb = vp.reshape(B, H, nb, MOBA_BLOCK, Dh)
    kmean = jnp.mean(kb.astype(jnp.float32), axis=3)
    topk = min(MOBA_TOPK, nb)
    scale = Dh ** -0.5
    blk_ids = jnp.arange(nb)
    gather_blocks = jax.vmap(jax.vmap(lambda t, i: t[i]))

    def one_chunk(c):
        q0 = c * Q_CHUNK
        cur = q0 // MOBA_BLOCK
        qc = lax.dynamic_slice_in_dim(q, q0, Q_CHUNK, axis=2) * scale
        gate = jnp.einsum('bhqd,bhnd->bhqn', qc.astype(jnp.float32), kmean)
        gate = jnp.where(blk_ids[None, None, None, :] < cur, gate, -jnp.inf)
        _, sel = lax.top_k(gate, topk)
        valid = sel < cur
        kg = gather_blocks(kb, sel)
        vg = gather_blocks(vb, sel)
        s_sel = jnp.einsum('bhqd,bhqjkd->bhqjk', qc, kg, preferred_element_type=jnp.float32)
        s_sel = jnp.where(valid[..., None], s_sel, -jnp.inf).reshape(B, H, Q_CHUNK, topk * MOBA_BLOCK)
        k_own = lax.dynamic_index_in_dim(kb, cur, axis=2, keepdims=False)
        v_own = lax.dynamic_index_in_dim(vb, cur, axis=2, keepdims=False)
        s_own = jnp.einsum('bhqd,bhkd->bhqk', qc, k_own, preferred_element_type=jnp.float32)
        qpos = q0 + jnp.arange(Q_CHUNK)
        kpos = cur * MOBA_BLOCK + jnp.arange(MOBA_BLOCK)
        s_own = jnp.where(kpos[None, :] <= qpos[:, None], s_own, -jnp.inf)
        p = jax.nn.softmax(jnp.concatenate([s_sel, s_own], axis=-1), axis=-1)
        p_sel = p[..., :topk * MOBA_BLOCK].reshape(B, H, Q_CHUNK, topk, MOBA_BLOCK).astype(v.dtype)
        p_own = p[..., topk * MOBA_BLOCK:].astype(v.dtype)
        out = (jnp.einsum('bhqjk,bhqjkd->bhqd', p_sel, vg, preferred_element_type=jnp.float32)
               + jnp.einsum('bhqk,bhkd->bhqd', p_own, v_own, preferred_element_type=jnp.float32))
        return out.astype(q.dtype)

    outs = lax.map(one_chunk, jnp.arange(S // Q_CHUNK))
    return jnp.moveaxis(outs, 0, 2).reshape(B, H, S, Dh)


def moba_mixer(xn, w_qkv, w_o, cos, sin):
    B, S, _ = xn.shape
    qkv = jnp.einsum('bsd,de->bse', xn, w_qkv).reshape(B, S, 3, ATTN_HEADS, ATTN_HEAD_DIM)
    qkv = jnp.transpose(qkv, (2, 0, 3, 1, 4))
    q = partial_rotary(qkv[0], cos, sin)
    k = partial_rotary(qkv[1], cos, sin)
    o = moba_attention(q, k, qkv[2])
    o = jnp.transpose(o, (0, 2, 1, 3)).reshape(B, S, D_MODEL)
    return jnp.einsum('bsd,de->bse', o, w_o)


def mlstm_chunkwise(q, k, v, i_pre, f_pre):
    B, H, S, dk = q.shape
    dv = v.shape[-1]
    L = MLSTM_CHUNK
    NC = S // L
    qf = q.astype(jnp.float32).reshape(B, H, NC, L, dk) * (dk ** -0.5)
    kf = k.astype(jnp.float32).reshape(B, H, NC, L, dk)
    vf = v.astype(jnp.float32).reshape(B, H, NC, L, dv)
    logf = jax.nn.log_sigmoid(f_pre).reshape(B, H, NC, L)
    ig = i_pre.reshape(B, H, NC, L)
    b = jnp.cumsum(logf, axis=-1)
    bL = b[..., -1]
    causal = jnp.tril(jnp.ones((L, L), dtype=bool))
    dmat = jnp.where(causal, b[..., :, None] - b[..., None, :] + ig[..., None, :], -jnp.inf)
    m_intra = jnp.max(dmat, axis=-1)
    a = bL[..., None] - b + ig
    m_loc = jnp.max(a, axis=-1)
    w = jnp.exp(a - m_loc[..., None])
    c_loc = jnp.einsum('bhcl,bhclv,bhclk->bhcvk', w, vf, kf)
    n_loc = jnp.einsum('bhcl,bhclk->bhck', w, kf)

    def step(carry, inp):
        c_prev, n_prev, m_prev = carry
        c_l, n_l, m_l, bl = inp
        m_new = jnp.maximum(bl + m_prev, m_l)
        decay = jnp.exp(bl + m_prev - m_new)
        sc = jnp.exp(m_l - m_new)
        c_new = decay[..., None, None] * c_prev + sc[..., None, None] * c_l
        n_new = decay[..., None] * n_prev + sc[..., None] * n_l
        return (c_new, n_new, m_new), (c_prev, n_prev, m_prev)

    init = (jnp.zeros((B, H, dv, dk), jnp.float32), jnp.zeros((B, H, dk), jnp.float32),
            jnp.zeros((B, H), jnp.float32))
    xs = (jnp.moveaxis(c_loc, 2, 0), jnp.moveaxis(n_loc, 2, 0), jnp.moveaxis(m_loc, 2, 0),
          jnp.moveaxis(bL, 2, 0))
    _, (c_in, n_in, m_in) = lax.scan(step, init, xs)
    c_in = jnp.moveaxis(c_in, 0, 2)
    n_in = jnp.moveaxis(n_in, 0, 2)
    m_in = jnp.moveaxis(m_in, 0, 2)

    b_inter = b + m_in[..., None]
    m_t = jnp.maximum(b_inter, m_intra)
    inter = jnp.exp(b_inter - m_t)
    s_qk = jnp.einsum('bhcld,bhcsd->bhcls', qf, kf) * jnp.exp(dmat - m_t[..., None])
    num = (inter[..., None] * jnp.einsum('bhcld,bhcvd->bhclv', qf, c_in)
           + jnp.einsum('bhcls,bhcsv->bhclv', s_qk, vf))
    den = inter * jnp.einsum('bhcld,bhcd->bhcl', qf, n_in) + jnp.sum(s_qk, axis=-1)
    h = num / jnp.maximum(jnp.abs(den), jnp.exp(-m_t))[..., None]
    return h.reshape(B, H, S, dv)


def mlstm_mixer(xn, w_in, b_gates, head_norm, w_out):
    B, S, _ = xn.shape
    H, dk, dv = MLSTM_HEADS, MLSTM_QK_DIM, MLSTM_V_DIM
    proj = jnp.einsum('bsd,de->bse', xn, w_in)
    q_end = H * dk
    k_end = 2 * H * dk
    v_end = k_end + H * dv
    o_end = v_end + D_MODEL
    q = jnp.transpose(proj[..., :q_end].reshape(B, S, H, dk), (0, 2, 1, 3))
    k = jnp.transpose(proj[..., q_end:k_end].reshape(B, S, H, dk), (0, 2, 1, 3))
    v = jnp.transpose(proj[..., k_end:v_end].reshape(B, S, H, dv), (0, 2, 1, 3))
    o_gate = jax.nn.sigmoid(proj[..., v_end:o_end].astype(jnp.float32))
    gates = proj[..., o_end:].astype(jnp.float32) + b_gates.astype(jnp.float32)
    gates = GATE_SOFTCAP * jnp.tanh(gates / GATE_SOFTCAP)
    i_pre = jnp.transpose(gates[..., :H], (0, 2, 1))
    f_pre = jnp.transpose(gates[..., H:], (0, 2, 1))
    h = mlstm_chunkwise(q, k, v, i_pre, f_pre)
    h = h * lax.rsqrt(jnp.mean(h * h, axis=-1, keepdims=True) + RMS_EPS)
    h = jnp.transpose(h, (0, 2, 1, 3)).reshape(B, S, D_MODEL) * head_norm.astype(jnp.float32)
    y = (o_gate * h).astype(xn.dtype)
    return jnp.einsum('bsd,de->bse', y, w_out)


def swiglu(xn, w_gate_up, w_down):
    gu = jnp.einsum('bsd,df->bsf', xn, w_gate_up)
    g, u = gu[..., :D_FF], gu[..., D_FF:]
    return jnp.einsum('bsf,fd->bsd', jax.nn.silu(g) * u, w_down)


def setup_inputs(seed: int = 0) -> dict:
    key = jax.random.key(seed)
    ks = jax.random.split(key, 16)
    n_attn = (DEPTH + 1) // 2
    n_mlstm = DEPTH // 2
    out_scale = (2 * DEPTH) ** -0.5

    def dense(k, shape, fan_in, scale=1.0):
        return jax.random.normal(k, shape, jnp.float32) * (scale * fan_in ** -0.5)

    def gain(k, shape):
        return 1.0 + 0.02 * jax.random.normal(k, shape, jnp.float32)

    x = jax.random.normal(ks[0], (BATCH, SEQ, D_MODEL), jnp.float32)
    attn_norm = gain(ks[1], (n_attn, D_MODEL))
    attn_w_qkv = dense(ks[2], (n_attn, D_MODEL, 3 * D_MODEL), D_MODEL)
    attn_w_o = dense(ks[3], (n_attn, D_MODEL, D_MODEL), D_MODEL, out_scale)
    mlstm_norm = gain(ks[4], (n_mlstm, D_MODEL))
    mlstm_w_in = dense(ks[5], (n_mlstm, D_MODEL, MLSTM_IN_WIDTH), D_MODEL)
    i_bias = 0.1 * jax.random.normal(ks[6], (n_mlstm, MLSTM_HEADS), jnp.float32)
    f_bias = (jnp.linspace(3.0, 6.0, MLSTM_HEADS, dtype=jnp.float32)[None, :]
              + 0.1 * jax.random.normal(ks[7], (n_mlstm, MLSTM_HEADS), jnp.float32))
    mlstm_b_gates = jnp.concatenate([i_bias, f_bias], axis=-1)
    mlstm_head_norm = gain(ks[8], (n_mlstm, D_MODEL))
    mlstm_w_out = dense(ks[9], (n_mlstm, D_MODEL, D_MODEL), D_MODEL, out_scale)
    ffn_norm = gain(ks[10], (DEPTH, D_MODEL))
    ffn_w_gate_up = dense(ks[11], (DEPTH, D_MODEL, 2 * D_FF), D_MODEL)
    ffn_w_down = dense(ks[12], (DEPTH, D_FF, D_MODEL), D_FF, out_scale)
    final_norm = gain(ks[13], (D_MODEL,))
    return {"x": x, "attn_norm": attn_norm, "attn_w_qkv": attn_w_qkv, "attn_w_o": attn_w_o,
            "mlstm_norm": mlstm_norm, "mlstm_w_in": mlstm_w_in, "mlstm_b_gates": mlstm_b_gates,
            "mlstm_head_norm": mlstm_head_norm, "mlstm_w_out": mlstm_w_out,
            "ffn_norm": ffn_norm, "ffn_w_gate_up": ffn_w_gate_up, "ffn_w_down": ffn_w_down,
            "final_norm": final_norm}


def reference(x, attn_norm, attn_w_qkv, attn_w_o, mlstm_norm, mlstm_w_in, mlstm_b_gates,
              mlstm_head_norm, mlstm_w_out, ffn_norm, ffn_w_gate_up, ffn_w_down, final_norm):
    S = x.shape[1]
    cos, sin = rotary_tables(S)
    h = x
    for i in range(DEPTH):
        j = i // N_MIXERS
        if i % N_MIXERS == 0:
            h = h + moba_mixer(rmsnorm(h, attn_norm[j]), attn_w_qkv[j], attn_w_o[j], cos, sin)
        else:
            h = h + mlstm_mixer(rmsnorm(h, mlstm_norm[j]), mlstm_w_in[j], mlstm_b_gates[j],
                                mlstm_head_norm[j], mlstm_w_out[j])
        h = h + swiglu(rmsnorm(h, ffn_norm[i]), ffn_w_gate_up[i], ffn_w_down[i])
    return rmsnorm(h, final_norm)
```

```python
import math
from contextlib import ExitStack

import numpy as np
import ml_dtypes

import concourse.bass as bass
import concourse.mybir as mybir
from concourse.bass_utils import run_bass_kernel_spmd

F32 = mybir.dt.float32
BF16 = mybir.dt.bfloat16
AF = mybir.ActivationFunctionType
ALU = mybir.AluOpType
AX = mybir.AxisListType

D = 1024
B = 2
S = 8192
NCORE = 8
DFF = 2816
EPS = 1e-6
NEG = -30000.0


class T:
    __slots__ = ("name", "w", "r", "psum")

    def __init__(self, name, psum=False):
        self.name = name
        self.w = None
        self.r = {}
        self.psum = psum


def _flat(x):
    out = []
    for a in x:
        if isinstance(a, (list, tuple)):
            out.extend(_flat(a))
        elif a is not None:
            out.append(a)
    return out


class Prog:
    ENGS = ("pe", "act", "dve", "pool", "sp")
    NRING = 8

    def __init__(self):
        self.nc = bass.Bass("TRN2", target_bir_lowering=False)
        self.ins = {e: [] for e in self.ENGS}
        self.ndma = {e: 0 for e in self.ENGS}
        self.dma_idx = {e: [] for e in self.ENGS}
        self.stack = ExitStack()
        self.final = []

    def sb(self, name, shape, dt):
        return self.stack.enter_context(self.nc.sbuf_tensor(name, list(shape), dt))

    def ps(self, name, shape, dt):
        return self.stack.enter_context(self.nc.psum_tensor(name, list(shape), dt))

    def dram_in(self, name, shape, dt):
        return self.nc.dram_tensor(name, list(shape), dt, kind="ExternalInput").ap()

    def dram_out(self, name, shape, dt):
        return self.nc.dram_tensor(name, list(shape), dt, kind="ExternalOutput").ap()

    def dram_tmp(self, name, shape, dt):
        return self.nc.dram_tensor(name, list(shape), dt).ap()

    def _emit(self, eng, fn, reads, writes, dma):
        reads, writes = _flat(reads), _flat(writes)
        lst = self.ins[eng]
        idx = len(lst)
        raw, other = set(), set()
        for t in reads:
            if t.w is not None:
                raw.add(t.w)
            if t.psum:
                for e2, i2 in t.r.items():
                    if e2 != eng:
                        other.add((e2, i2))
        for t in writes:
            if t.w is not None:
                other.add(t.w)
            for e2, i2 in t.r.items():
                other.add((e2, i2))
        deps = set(raw)
        for d in other:
            if d[0] == eng and not dma and self.ins[eng][d[1]]["dma"] is None:
                continue
            deps.add(d)
        if eng == "pe":
            deps = {d for d in deps if d[0] != "pe"}
        rec = dict(fn=fn, deps=deps, dma=None)
        if dma:
            k = self.ndma[eng]
            self.ndma[eng] += 1
            rec["dma"] = k
            if k >= self.NRING:
                deps.add((eng, self.dma_idx[eng][k - self.NRING]))
            self.dma_idx[eng].append(idx)
        lst.append(rec)
        for t in reads:
            t.r[eng] = idx
        for t in writes:
            t.w = (eng, idx)
            t.r = {}
        return idx

    def op(self, eng, fn, reads=(), writes=()):
        return self._emit(eng, fn, reads, writes, False)

    def dma(self, eng, out, in_, reads=(), writes=(), final=False):
        i = self._emit(eng, lambda e: e.dma_start(out=out, in_=in_), reads, writes, True)
        if final:
            self.final.append((eng, i))
        return i

    def build(self):
        nc = self.nc
        st = self.stack
        final_deps = set(self.final)
        needed = {e: set() for e in self.ENGS}
        for e in self.ENGS:
            for rec in self.ins[e]:
                for (e2, i2) in rec["deps"]:
                    needed[e2].add(i2)
        cnt_sem = {e: st.enter_context(nc.semaphore("c_" + e)) for e in self.ENGS}
        ring = {e: [st.enter_context(nc.semaphore("r_%s%d" % (e, i))) for i in range(self.NRING)]
                for e in self.ENGS if self.ndma[e] > 0}
        sig = {e: {} for e in self.ENGS}
        for e in self.ENGS:
            c = 0
            for i, rec in enumerate(self.ins[e]):
                if rec["dma"] is not None:
                    k = rec["dma"]
                    sig[e][i] = (ring[e][k % self.NRING], 16 * (k // self.NRING + 1))
                elif i in needed[e]:
                    c += 1
                    sig[e][i] = (cnt_sem[e], c)
        self.stats = {e: (len(self.ins[e]), self.ndma[e]) for e in self.ENGS}
        block = st.enter_context(nc.Block())
        handles = {"pe": block.tensor, "act": block.scalar, "dve": block.vector,
                   "pool": block.gpsimd, "sp": block.sync}

        def make(e):
            def body(eng):
                waited = {}
                for i, rec in enumerate(self.ins[e]):
                    for d in sorted(rec["deps"]):
                        sem, val = sig[d[0]][d[1]]
                        key = id(sem)
                        if waited.get(key, 0) >= val:
                            continue
                        waited[key] = val
                        eng.wait_ge(sem, val)
                    inst = rec["fn"](eng)
                    if i in sig[e]:
                        sem, val = sig[e][i]
                        inst.then_inc(sem, 16 if rec["dma"] is not None else 1)
                if e == "sp":
                    for d in sorted(final_deps):
                        sem, val = sig[d[0]][d[1]]
                        if waited.get(id(sem), 0) >= val:
                            continue
                        waited[id(sem)] = val
                        eng.wait_ge(sem, val)
            return body

        for e in self.ENGS:
            handles[e](make(e))
        st.close()
        return nc


def load_w_bf16(P, name, w_dram, kdim, ncols, col0=0, tile=None, tr=None):
    kc = kdim // 128
    if tile is None:
        tile = P.sb(name, [128, kc, ncols], BF16)
    src = w_dram[:, col0:col0 + ncols].rearrange("(c p) n -> p c n", p=128)
    step = max(1, kc // 4)
    trs = []
    for c0 in range(0, kc, step):
        c1 = min(kc, c0 + step)
        tr = T(name + str(c0))
        trs.append(tr)
        P.dma("pool", tile[:, c0:c1, :], src[:, c0:c1, :], writes=[tr])
    return tile, trs


class NormCtx:
    def __init__(self, P, gain_dram, ident, ident_t, pst, pst_t):
        self.P = P
        self.ident, self.ident_t = ident, ident_t
        self.pst, self.pst_t = pst, pst_t
        self.g = P.sb("ng_" + gain_dram.tensor.name, [128, 8], F32)
        self.g_t = T("ng")
        P.dma("sp", self.g[:, :], gain_dram[:, :], writes=[self.g_t])
        self.junk = P.sb("nj_" + gain_dram.tensor.name, [128, 1024], BF16)
        self.junk_t = T("nj")
        self.ss = [P.sb("nss%d_" % i + gain_dram.tensor.name, [128, 2], F32) for i in range(2)]
        self.ss_t = [T("nss%d" % i) for i in range(2)]
        self.xs = [P.sb("nxs%d_" % i + gain_dram.tensor.name, [128, 1024], BF16) for i in range(2)]
        self.xs_t = [T("nxs%d" % i) for i in range(2)]
        self.k = 0

    def run(self, h_ap, h_t, dst_fn, dst_t):
        P = self.P
        k = self.k
        self.k += 1
        ss, ss_t = self.ss[k % 2], self.ss_t[k % 2]
        xs, xs_t = self.xs[k % 2], self.xs_t[k % 2]
        pst, pst_t = self.pst[k % len(self.pst)], self.pst_t[k % len(self.pst)]
        junk, junk_t = self.junk, self.junk_t
        P.op("act", lambda e: e.activation(out=junk[:, :], in_=h_ap, func=AF.Square,
                                           accum_out=ss[:, 0:1]),
             reads=[h_t], writes=[junk_t, ss_t])
        P.op("dve", lambda e: e.tensor_scalar(out=ss[:, 1:2], in0=ss[:, 0:1], scalar1=1.0 / D,
                                              scalar2=EPS, op0=ALU.mult, op1=ALU.add),
             reads=[ss_t], writes=[ss_t])
        P.op("act", lambda e: e.activation(out=ss[:, 1:2], in_=ss[:, 1:2], func=AF.Sqrt),
             reads=[ss_t], writes=[ss_t])
        P.op("dve", lambda e: e.reciprocal(out=ss[:, 1:2], in_=ss[:, 1:2]),
             reads=[ss_t], writes=[ss_t])
        P.op("dve", lambda e: e.tensor_scalar(out=xs[:, :], in0=h_ap, scalar1=ss[:, 1:2],
                                              scalar2=None, op0=ALU.mult),
             reads=[h_t, ss_t], writes=[xs_t])
        for c in range(8):
            P.op("pe", lambda e, c=c: e.transpose(out=pst[:, c, :], in_=xs[:, c * 128:(c + 1) * 128],
                                                  identity=self.ident[:, :]),
                 reads=[xs_t, self.ident_t], writes=[pst_t])
        g = self.g
        for c in range(8):
            P.op("act", lambda e, c=c: e.activation(out=dst_fn(c), in_=pst[:, c, :], func=AF.Copy,
                                                    scale=g[:, c:c + 1]),
                 reads=[pst_t, self.g_t], writes=[dst_t])


def make_ident(P, ident_dram):
    ident = P.sb("ident_sb", [128, 128], BF16)
    ident_t = T("ident")
    P.dma("sp", ident[:, :], ident_dram[:, :], writes=[ident_t])
    return ident, ident_t


FFN_PARTS = [(0, 5), (5, 5), (10, 4), (14, 4), (18, 4)]


def build_projffn(final):
    P = Prog()
    NT = 16
    hin = P.dram_in("hin", [2048, D], F32)
    aT = P.dram_in("aT", [D, 2048], BF16)
    wp = P.dram_in("wp", [D, D], F32)
    gn = P.dram_in("gn", [128, 8], F32)
    wgu = P.dram_in("wgu", [D, 2 * DFF], F32)
    wd = P.dram_in("wd", [DFF, D], F32)
    identd = P.dram_in("ident", [128, 128], BF16)
    if final:
        fn = P.dram_in("fnw", [D], F32)
    hout = P.dram_out("hout", [2048, D], F32)

    ident, ident_t = make_ident(P, identd)
    h = P.sb("h", [128, NT, D], F32)
    h_t = [T("h%d" % i) for i in range(NT)]
    xnT = P.sb("xnT", [128, 8, 2048], BF16)
    xn_t = [T("xn%d" % i) for i in range(NT)]
    wps, wps_t = load_w_bf16(P, "wps", wp, D, D)
    at = [P.sb("at%d" % i, [128, 8, 128], BF16) for i in range(2)]
    at_t = [T("at%d" % i) for i in range(2)]
    pf = [P.ps("pf%d" % i, [128, 512], F32) for i in range(6)]
    pf_t = [T("pf%d" % i, True) for i in range(6)]
    pb = [P.ps("pb%d" % i, [128, 8, 128], BF16) for i in range(2)]
    pb_t = [T("pb%d" % i, True) for i in range(2)]
    norm = NormCtx(P, gn, ident, ident_t, pb, pb_t)
    pfk = [0]

    def next_pf():
        i = pfk[0] % 6
        pfk[0] += 1
        return pf[i], pf_t[i]

    for t in range(NT):
        P.dma("sp", h[:, t, :], hin[t * 128:(t + 1) * 128, :], writes=[h_t[t]])
        a, a_t = at[t % 2], at_t[t % 2]
        P.dma("sp", a[:, :, :], aT[:, t * 128:(t + 1) * 128].rearrange("(c p) n -> p c n", p=128),
              writes=[a_t])
        for nh in range(2):
            ps, ps_t = next_pf()
            for c in range(8):
                P.op("pe", lambda e, c=c, ps=ps, a=a, nh=nh: e.matmul(
                    ps[:, :], lhsT=a[:, c, :], rhs=wps[:, c, nh * 512:(nh + 1) * 512],
                    start=(c == 0), stop=(c == 7)),
                    reads=[a_t, wps_t], writes=[ps_t])
            P.op("dve", lambda e, ps=ps, t=t, nh=nh: e.tensor_tensor(
                out=h[:, t, nh * 512:(nh + 1) * 512], in0=h[:, t, nh * 512:(nh + 1) * 512],
                in1=ps[:, :], op=ALU.add),
                reads=[ps_t, h_t[t]], writes=[h_t[t]])
        norm.run(h[:, t, :], h_t[t], lambda c, t=t: xnT[:, c, t * 128:(t + 1) * 128], xn_t[t])

    wg_b = [P.sb("wg%d" % i, [128, 8, 640], BF16) for i in range(2)]
    wu_b = [P.sb("wu%d" % i, [128, 8, 640], BF16) for i in range(2)]
    wd_b = [P.sb("wd%d" % i, [128, 5, 1024], BF16) for i in range(2)]
    wg_t = [[T("wg%d_%d" % (i, q)) for q in range(4)] for i in range(2)]
    wu_t = [[T("wu%d_%d" % (i, q)) for q in range(4)] for i in range(2)]
    wd_t = [[T("wd%d_%d" % (i, q)) for q in range(3)] for i in range(2)]
    sg = [P.sb("sg%d" % i, [128, 512], F32) for i in range(2)]
    sg_t = [T("sg%d" % i) for i in range(2)]
    aF = [P.sb("aF%d" % i, [128, 5, 512], BF16) for i in range(2)]
    aF_t = [T("aF%d" % i) for i in range(2)]
    kk = 0
    for pi, (c0, ncn) in enumerate(FFN_PARTS):
        b = pi % 2
        ncols = ncn * 128
        srcg = wgu[:, c0 * 128:c0 * 128 + ncols].rearrange("(c p) n -> p c n", p=128)
        srcu = wgu[:, DFF + c0 * 128:DFF + c0 * 128 + ncols].rearrange("(c p) n -> p c n", p=128)
        for q in range(4):
            P.dma("pool", wg_b[b][:, 2 * q:2 * q + 2, 0:ncols], srcg[:, 2 * q:2 * q + 2, :], writes=[wg_t[b][q]])
            P.dma("pool", wu_b[b][:, 2 * q:2 * q + 2, 0:ncols], srcu[:, 2 * q:2 * q + 2, :], writes=[wu_t[b][q]])
        srcd = wd[c0 * 128:(c0 + ncn) * 128, :].rearrange("(c p) n -> p c n", p=128)
        for q in range(0, ncn, 2):
            q1 = min(ncn, q + 2)
            P.dma("pool", wd_b[b][:, q:q1, :], srcd[:, q:q1, :], writes=[wd_t[b][q // 2]])
        for tg in range(4):
            af, af_t = aF[tg % 2], aF_t[tg % 2]
            for j in range(ncn):
                psg, psg_t = next_pf()
                psu, psu_t = next_pf()
                for c in range(8):
                    P.op("pe", lambda e, c=c, j=j, psg=psg, b=b, tg=tg: e.matmul(
                        psg[:, :], lhsT=wg_b[b][:, c, j * 128:(j + 1) * 128],
                        rhs=xnT[:, c, tg * 512:(tg + 1) * 512], start=(c == 0), stop=(c == 7)),
                        reads=[wg_t[b]] + xn_t[tg * 4:tg * 4 + 4], writes=[psg_t])
                for c in range(8):
                    P.op("pe", lambda e, c=c, j=j, psu=psu, b=b, tg=tg: e.matmul(
                        psu[:, :], lhsT=wu_b[b][:, c, j * 128:(j + 1) * 128],
                        rhs=xnT[:, c, tg * 512:(tg + 1) * 512], start=(c == 0), stop=(c == 7)),
                        reads=[wu_t[b]] + xn_t[tg * 4:tg * 4 + 4], writes=[psu_t])
                s, s_t = sg[kk % 2], sg_t[kk % 2]
                kk += 1
                P.op("act", lambda e, s=s, psg=psg: e.activation(out=s[:, :], in_=psg[:, :], func=AF.Silu),
                     reads=[psg_t], writes=[s_t])
                P.op("dve", lambda e, s=s, psu=psu, af=af, j=j: e.tensor_tensor(
                    out=af[:, j, :], in0=s[:, :], in1=psu[:, :], op=ALU.mult),
                    reads=[s_t, psu_t], writes=[af_t])
            for tt in range(4):
                t = tg * 4 + tt
                for nh in range(2):
                    ps, ps_t = next_pf()
                    for j in range(ncn):
                        P.op("pe", lambda e, j=j, ps=ps, af=af, tt=tt, nh=nh, b=b: e.matmul(
                            ps[:, :], lhsT=af[:, j, tt * 128:(tt + 1) * 128],
                            rhs=wd_b[b][:, j, nh * 512:(nh + 1) * 512],
                            start=(j == 0), stop=(j == ncn - 1)),
                            reads=[af_t, wd_t[b]], writes=[ps_t])
                    P.op("dve", lambda e, ps=ps, t=t, nh=nh: e.tensor_tensor(
                        out=h[:, t, nh * 512:(nh + 1) * 512], in0=h[:, t, nh * 512:(nh + 1) * 512],
                        in1=ps[:, :], op=ALU.add),
                        reads=[ps_t, h_t[t]], writes=[h_t[t]])

    if final:
        fw = P.sb("fw", [128, D], F32)
        fw_t = T("fw")
        P.dma("sp", fw[:, :], fn.partition_broadcast(128), writes=[fw_t])
        ss = P.sb("fss", [128, 2 * NT], F32)
        ss_t = T("fss")
        junk = norm.junk
        for t in range(NT):
            P.op("act", lambda e, t=t: e.activation(out=junk[:, :], in_=h[:, t, :], func=AF.Square,
                                                    accum_out=ss[:, 2 * t:2 * t + 1]),
                 reads=[h_t[t]], writes=[norm.junk_t, ss_t])
            P.op("dve", lambda e, t=t: e.tensor_scalar(out=ss[:, 2 * t + 1:2 * t + 2], in0=ss[:, 2 * t:2 * t + 1],
                                                       scalar1=1.0 / D, scalar2=EPS, op0=ALU.mult, op1=ALU.add),
                 reads=[ss_t], writes=[ss_t])
            P.op("act", lambda e, t=t: e.activation(out=ss[:, 2 * t + 1:2 * t + 2],
                                                    in_=ss[:, 2 * t + 1:2 * t + 2], func=AF.Sqrt),
                 reads=[ss_t], writes=[ss_t])
            P.op("dve", lambda e, t=t: e.reciprocal(out=ss[:, 2 * t + 1:2 * t + 2],
                                                    in_=ss[:, 2 * t + 1:2 * t + 2]),
                 reads=[ss_t], writes=[ss_t])
            P.op("dve", lambda e, t=t: e.scalar_tensor_tensor(
                out=h[:, t, :], in0=h[:, t, :], scalar=ss[:, 2 * t + 1:2 * t + 2], in1=fw[:, :],
                op0=ALU.mult, op1=ALU.mult),
                reads=[h_t[t], ss_t, fw_t], writes=[h_t[t]])
    for t in range(NT):
        P.dma("sp", hout[t * 128:(t + 1) * 128, :], h[:, t, :], reads=[h_t[t]], writes=[T("o%d" % t)], final=True)
    return P.build()


def build_mlstm():
    P = Prog()
    NCH = S // 128
    hin = P.dram_in("hin", [S, D], F32)
    gn = P.dram_in("gn", [128, 8], F32)
    wq_d = P.dram_in("wq", [D, 128], F32)
    wk_d = P.dram_in("wk", [D, 128], F32)
    wt_d = P.dram_in("wtok", [D, 644], F32)
    bg_d = P.dram_in("bg", [128, 4], F32)
    hn_d = P.dram_in("hn", [128, 256], F32)
    identd = P.dram_in("ident", [128, 128], BF16)
    U_d = P.dram_in("U", [128, 128], F32)
    ones_d = P.dram_in("ones", [128, 128], F32)
    yout = P.dram_out("y", [S, 256], BF16)

    ident, ident_t = make_ident(P, identd)
    U = P.sb("U_sb", [128, 128], F32)
    ONES = P.sb("ones_sb", [128, 128], F32)
    bg = P.sb("bg_sb", [128, 4], F32)
    hn = P.sb("hn_sb", [128, 256], F32)
    c_t = T("consts")
    P.dma("sp", U[:, :], U_d[:, :], writes=[c_t])
    c2_t = T("consts2")
    P.dma("sp", ONES[:, :], ones_d[:, :], writes=[c2_t])
    c3_t = T("consts3")
    P.dma("sp", bg[:, :], bg_d[:, :], writes=[c3_t])
    c4_t = T("consts4")
    P.dma("sp", hn[:, :], hn_d[:, :], writes=[c4_t])
    wq, wq_t = load_w_bf16(P, "wq_sb", wq_d, D, 128)
    wk, wk_t = load_w_bf16(P, "wk_sb", wk_d, D, 128)
    wt, wt_t = load_w_bf16(P, "wt_sb", wt_d, D, 644)

    def dbl(name, shape, dt):
        return [P.sb("%s%d" % (name, i), shape, dt) for i in range(2)], [T("%s%d" % (name, i)) for i in range(2)]

    xin, xin_t = dbl("xin", [128, D], F32)
    xnT, xnT_t = dbl("xnT", [128, 8, 128], BF16)
    qT, qT_t = dbl("qT", [64, 2, 128], BF16)
    kT, kT_t = dbl("kT", [64, 2, 128], BF16)
    ktok, ktok_t = dbl("ktok", [128, 128], BF16)
    og, og_t = dbl("og", [128, 256], F32)
    gt, gt_t = dbl("gt", [128, 24], F32)
    V1 = [dbl("V1_%d_" % h, [128, 129], BF16) for h in range(2)]
    V2 = [dbl("V2_%d_" % h, [128, 129], BF16) for h in range(2)]
    PT, PT_t = dbl("PT", [128, 128], BF16)
    hh, hh_t = dbl("hh", [128, 128], F32)
    sq = P.sb("sqj", [128, 128], BF16)
    sq_t = T("sqj")
    st, st_t = dbl("st", [128, 8], F32)
    yt, yt_t = dbl("yt", [128, 256], BF16)
    C = [P.sb("C%d" % h, [64, 129], F32) for h in range(2)]
    C_t = [T("C%d" % h) for h in range(2)]
    Cb = [dbl("Cb%d_" % h, [64, 129], BF16) for h in range(2)]

    pb = [P.ps("pbT", [128, 8, 128], BF16)]
    pb_t = [T("pbT", True)]
    pq = P.ps("pq", [128, 512], F32); pq_t = T("pq", True)
    p1 = P.ps("p1", [128, 512], F32); p1_t = T("p1", True)
    p2 = P.ps("p2", [128, 512], F32); p2_t = T("p2", True)
    pg = P.ps("pg", [128, 512], F32); pg_t = T("pg", True)
    pS = P.ps("pS", [128, 512], F32); pS_t = T("pS", True)
    pO = P.ps("pO", [128, 512], F32); pO_t = T("pO", True)
    pA = P.ps("pA", [128, 512], F32); pA_t = T("pA", True)
    norm = NormCtx(P, gn, ident, ident_t, pb, pb_t)

    for h in range(2):
        P.op("dve", lambda e, h=h: e.memset(C[h][:, :], 0.0), writes=[C_t[h]])
        P.op("dve", lambda e, h=h: e.memset(Cb[h][0][0][:, :], 0.0), writes=[Cb[h][1][0]])

    for j in range(NCH):
        b = j % 2
        P.dma("sp", xin[b][:, :], hin[j * 128:(j + 1) * 128, :], writes=[xin_t[b]])
        norm.run(xin[b][:, :], xin_t[b], lambda c, b=b: xnT[b][:, c, :], xnT_t[b])
        for qi, (w, w_t) in enumerate(((wq, wq_t), (wk, wk_t))):
            for h in range(2):
                for c in range(8):
                    P.op("pe", lambda e, c=c, h=h, w=w, qi=qi, b=b: e.matmul(
                        pq[0:64, (2 * qi + h) * 128:(2 * qi + h + 1) * 128],
                        lhsT=w[:, c, h * 64:(h + 1) * 64], rhs=xnT[b][:, c, :],
                        start=(c == 0), stop=(c == 7)),
                        reads=[w_t, xnT_t[b]], writes=[pq_t])
        P.op("act", lambda e, b=b: e.activation(out=qT[b][:, :, :].rearrange("p a n -> p (a n)"),
                                                in_=pq[0:64, 0:256], func=AF.Copy, scale=0.125),
             reads=[pq_t], writes=[qT_t[b]])
        P.op("dve", lambda e, b=b: e.tensor_copy(out=kT[b][:, :, :].rearrange("p a n -> p (a n)"),
                                                 in_=pq[0:64, 256:512]),
             reads=[pq_t], writes=[kT_t[b]])
        for c in range(8):
            P.op("pe", lambda e, c=c, b=b: e.matmul(p1[:, 0:384], lhsT=xnT[b][:, c, :], rhs=wt[:, c, 0:384],
                                                    start=(c == 0), stop=(c == 7)),
                 reads=[wt_t, xnT_t[b]], writes=[p1_t])
        for c in range(8):
            P.op("pe", lambda e, c=c, b=b: e.matmul(p2[:, 0:260], lhsT=xnT[b][:, c, :], rhs=wt[:, c, 384:644],
                                                    start=(c == 0), stop=(c == 7)),
                 reads=[wt_t, xnT_t[b]], writes=[p2_t])
        g = gt[b]
        g_t = gt_t[b]
        P.op("dve", lambda e, g=g: e.tensor_tensor(out=g[:, 0:4], in0=p2[:, 256:260], in1=bg[:, :], op=ALU.add),
             reads=[p2_t, c3_t], writes=[g_t])
        P.op("act", lambda e, g=g: e.activation(out=g[:, 4:8], in_=g[:, 0:4], func=AF.Tanh, scale=1.0 / 15.0),
             reads=[g_t], writes=[g_t])
        P.op("act", lambda e, g=g: e.activation(out=g[:, 8:10], in_=g[:, 6:8], func=AF.Exp, scale=-15.0),
             reads=[g_t], writes=[g_t])
        P.op("act", lambda e, g=g: e.activation(out=g[:, 10:12], in_=g[:, 8:10], func=AF.Ln, bias=1.0),
             reads=[g_t], writes=[g_t])
        P.op("pe", lambda e, g=g: e.matmul(pg[:, 0:2], lhsT=U[:, :], rhs=g[:, 10:12], start=True, stop=True),
             reads=[g_t, c_t], writes=[pg_t])
        P.op("pe", lambda e, g=g: e.matmul(pg[:, 2:4], lhsT=ONES[:, :], rhs=g[:, 10:12], start=True, stop=True),
             reads=[g_t, c2_t], writes=[pg_t])
        P.op("dve", lambda e, g=g: e.tensor_copy(out=g[:, 12:16], in_=pg[:, 0:4]), reads=[pg_t], writes=[g_t])
        P.op("dve", lambda e, g=g: e.tensor_tensor(out=g[:, 16:18], in0=g[:, 12:14], in1=g[:, 14:16],
                                                   op=ALU.subtract),
             reads=[g_t], writes=[g_t])
        s = st[b]
        s_t = st_t[b]
        for h in range(2):
            P.op("act", lambda e, g=g, h=h: e.activation(out=g[:, 18 + h:19 + h], in_=g[:, 4 + h:5 + h], func=AF.Exp,
                                                         scale=15.0, bias=g[:, 12 + h:13 + h]),
                 reads=[g_t], writes=[g_t])
            P.op("act", lambda e, g=g, h=h: e.activation(out=g[:, 20 + h:21 + h], in_=g[:, 4 + h:5 + h], func=AF.Exp,
                                                         scale=15.0, bias=g[:, 16 + h:17 + h]),
                 reads=[g_t], writes=[g_t])
        P.op("act", lambda e, g=g: e.activation(out=g[:, 22:24], in_=g[:, 12:14], func=AF.Exp, scale=-1.0),
             reads=[g_t], writes=[g_t])
        P.op("act", lambda e, g=g, s=s: e.activation(out=s[:, 6:8], in_=g[:, 14:16], func=AF.Exp, scale=-1.0),
             reads=[g_t], writes=[s_t])
        P.op("act", lambda e, b=b: e.activation(out=ktok[b][:, :], in_=p1[:, 0:128], func=AF.Copy),
             reads=[p1_t], writes=[ktok_t[b]])
        P.op("act", lambda e, b=b: e.activation(out=og[b][:, :], in_=p2[:, 0:256], func=AF.Sigmoid),
             reads=[p2_t], writes=[og_t[b]])
        for h in range(2):
            for (V, col) in ((V1[h], 18 + h), (V2[h], 20 + h)):
                Vb, Vb_t = V[0][b], V[1][b]
                P.op("dve", lambda e, Vb=Vb, g=g, col=col, h=h: e.tensor_scalar(
                    out=Vb[:, 0:128], in0=p1[:, 128 + h * 128:256 + h * 128], scalar1=g[:, col:col + 1],
                    scalar2=None, op0=ALU.mult),
                    reads=[p1_t, g_t], writes=[Vb_t])
                P.op("act", lambda e, Vb=Vb, g=g, col=col: e.activation(out=Vb[:, 128:129], in_=g[:, col:col + 1],
                                                                        func=AF.Copy),
                     reads=[g_t], writes=[Vb_t])
        for h in range(2):
            Cb_cur, Cb_cur_t = Cb[h][0][b], Cb[h][1][b]
            Cb_nxt, Cb_nxt_t = Cb[h][0][1 - b], Cb[h][1][1 - b]
            V1b, V1b_t = V1[h][0][b], V1[h][1][b]
            V2b, V2b_t = V2[h][0][b], V2[h][1][b]
            P.op("pe", lambda e, h=h, b=b: e.matmul(pS[:, 0:128], lhsT=kT[b][:, h, :], rhs=qT[b][:, h, :],
                                                    start=True, stop=True),
                 reads=[kT_t[b], qT_t[b]], writes=[pS_t])
            pt, pt_t = PT[h], PT_t[h]
            P.op("dve", lambda e, pt=pt: e.tensor_tensor(out=pt[:, :], in0=pS[:, 0:128], in1=U[:, :], op=ALU.mult),
                 reads=[pS_t, c_t], writes=[pt_t])
            P.op("pe", lambda e, h=h, b=b, Cb_cur=Cb_cur: e.matmul(pO[:, 0:129], lhsT=qT[b][:, h, :], rhs=Cb_cur[:, :],
                                                                   start=True, stop=False),
                 reads=[qT_t[b], Cb_cur_t], writes=[pO_t])
            P.op("pe", lambda e, pt=pt, V1b=V1b: e.matmul(pO[:, 0:129], lhsT=pt[:, :], rhs=V1b[:, :],
                                                          start=False, stop=True),
                 reads=[pt_t, V1b_t], writes=[pO_t])
            o = 3 * h
            P.op("dve", lambda e, s=s, g=g, h=h, o=o: e.tensor_tensor(out=s[:, o:o + 1], in0=pO[:, 128:129],
                                                                      in1=g[:, 22 + h:23 + h], op=ALU.mult),
                 reads=[pO_t, g_t], writes=[s_t])
            P.op("dve", lambda e, s=s, o=o: e.tensor_scalar(out=s[:, o + 1:o + 2], in0=s[:, o:o + 1], scalar1=-1.0,
                                                            scalar2=1.0, op0=ALU.mult, op1=ALU.max),
                 reads=[s_t], writes=[s_t])
            P.op("dve", lambda e, s=s, o=o: e.tensor_tensor(out=s[:, o:o + 1], in0=s[:, o:o + 1],
                                                            in1=s[:, o + 1:o + 2], op=ALU.max),
                 reads=[s_t], writes=[s_t])
            P.op("dve", lambda e, s=s, o=o: e.reciprocal(out=s[:, o:o + 1], in_=s[:, o:o + 1]),
                 reads=[s_t], writes=[s_t])
            P.op("dve", lambda e, s=s, g=g, h=h, o=o: e.tensor_tensor(out=s[:, o + 1:o + 2], in0=g[:, 22 + h:23 + h],
                                                                      in1=s[:, o:o + 1], op=ALU.mult),
                 reads=[s_t, g_t], writes=[s_t])
            hb, hb_t = hh[h], hh_t[h]
            P.op("dve", lambda e, hb=hb, s=s, o=o: e.tensor_scalar(out=hb[:, :], in0=pO[:, 0:128],
                                                                   scalar1=s[:, o + 1:o + 2], scalar2=None, op0=ALU.mult),
                 reads=[pO_t, s_t], writes=[hb_t])
            P.op("act", lambda e, hb=hb, s=s, o=o: e.activation(out=sq[:, :], in_=hb[:, :], func=AF.Square,
                                                                accum_out=s[:, o + 2:o + 3]),
                 reads=[hb_t], writes=[sq_t, s_t])
            P.op("dve", lambda e, s=s, o=o: e.tensor_scalar(out=s[:, o + 2:o + 3], in0=s[:, o + 2:o + 3],
                                                            scalar1=1.0 / 128.0, scalar2=EPS, op0=ALU.mult, op1=ALU.add),
                 reads=[s_t], writes=[s_t])
            P.op("act", lambda e, s=s, o=o: e.activation(out=s[:, o + 2:o + 3], in_=s[:, o + 2:o + 3], func=AF.Sqrt),
                 reads=[s_t], writes=[s_t])
            P.op("dve", lambda e, s=s, o=o: e.reciprocal(out=s[:, o + 2:o + 3], in_=s[:, o + 2:o + 3]),
                 reads=[s_t], writes=[s_t])
            P.op("dve", lambda e, hb=hb, s=s, o=o, h=h: e.scalar_tensor_tensor(
                out=hb[:, :], in0=hb[:, :], scalar=s[:, o + 2:o + 3], in1=hn[:, h * 128:(h + 1) * 128],
                op0=ALU.mult, op1=ALU.mult),
                reads=[hb_t, s_t, c4_t], writes=[hb_t])
            P.op("dve", lambda e, hb=hb, h=h, b=b: e.tensor_tensor(out=yt[b][:, h * 128:(h + 1) * 128], in0=hb[:, :],
                                                                   in1=og[b][:, h * 128:(h + 1) * 128], op=ALU.mult),
                 reads=[hb_t, og_t[b]], writes=[yt_t[b]])
            P.op("pe", lambda e, h=h, b=b, V2b=V2b: e.matmul(pA[0:64, 0:129], lhsT=ktok[b][:, h * 64:(h + 1) * 64],
                                                             rhs=V2b[:, :], start=True, stop=True),
                 reads=[ktok_t[b], V2b_t], writes=[pA_t])
            P.op("dve", lambda e, h=h, s=s: e.scalar_tensor_tensor(
                out=C[h][:, :], in0=C[h][:, :], scalar=s[0:64, 6 + h:7 + h], in1=pA[0:64, 0:129],
                op0=ALU.mult, op1=ALU.add),
                reads=[C_t[h], s_t, pA_t], writes=[C_t[h]])
            P.op("act", lambda e, h=h, Cb_nxt=Cb_nxt: e.activation(out=Cb_nxt[:, :], in_=C[h][:, :], func=AF.Copy),
                 reads=[C_t[h]], writes=[Cb_nxt_t])
        P.dma("sp", yout[j * 128:(j + 1) * 128, :], yt[b][:, :], reads=[yt_t[b]], writes=[T("yo%d" % j)], final=True)
    return P.build()


def build_attn():
    P = Prog()
    NT = S // 128
    xin_d = P.dram_in("xin", [S, D], F32)
    gn = P.dram_in("gn", [128, 8], F32)
    w_d = P.dram_in("wqkv", [D, 768], F32)
    cs_d = P.dram_in("cs", [S, 128], F32)
    identd = P.dram_in("ident", [128, 128], BF16)
    identf_d = P.dram_in("identf", [128, 128], F32)
    oneh_d = P.dram_in("onehot", [32, S], BF16)
    tri_d = P.dram_in("tri", [128, 128], BF16)
    M1_d = P.dram_in("M1", [128, 1024], F32)
    A30_d = P.dram_in("A30", [128, 1024], F32)
    Bc_d = P.dram_in("Bc", [128, 1024], F32)
    oT = P.dram_out("oT", [256, S], BF16)
    qTs = P.dram_tmp("qTs", [4, 64, S], BF16)
    kTs = P.dram_tmp("kTs", [4, 64, S], BF16)
    vs = P.dram_tmp("vs", [S, 256], BF16)

    ident, ident_t = make_ident(P, identd)
    identf = P.sb("identf_sb", [128, 128], F32)
    tri = P.sb("tri_sb", [128, 128], BF16)
    M1 = P.sb("M1_sb", [128, 1024], F32)
    A30 = P.sb("A30_sb", [128, 1024], F32)
    Bc = P.sb("Bc_sb", [128, 1024], F32)
    cst_t = []
    for dst, src in ((identf, identf_d), (tri, tri_d), (M1, M1_d), (A30, A30_d), (Bc, Bc_d)):
        t = T("c")
        cst_t.append(t)
        P.dma("sp", dst[:, :], src[:, :], writes=[t])
    w, w_t = load_w_bf16(P, "w_sb", w_d, D, 768)

    KA = P.sb("KA", [128, S], BF16)
    QA = P.sb("QA", [128, S], BF16)
    VA = P.sb("VA", [128, NT, 128], BF16)
    qabs = P.sb("qabs", [64, S], BF16)
    KAoh_t = T("KAoh")
    for q4 in range(4):
        P.dma("sp", KA[64:96, q4 * 2048:(q4 + 1) * 2048], oneh_d[:, q4 * 2048:(q4 + 1) * 2048], writes=[KAoh_t])
    VA1_t = T("VA1")
    P.op("pool", lambda e: e.memset(VA[:, :, 64:128], 1.0), writes=[VA1_t])

    def dbl(name, shape, dt):
        return [P.sb("%s%d" % (name, i), shape, dt) for i in range(2)], [T("%s%d" % (name, i)) for i in range(2)]

    xin, xin_t = dbl("xin_sb", [128, D], F32)
    cs, cs_t = dbl("cs_sb", [128, 128], F32)
    xnT, xnT_t = dbl("xnT", [128, 8, 128], BF16)
    qk32, qk32_t = dbl("qk32", [128, 512], F32)
    rt, rt_t = dbl("rt", [128, 4, 64], F32)
    qkb, qkb_t = dbl("qkb", [128, 512], BF16)
    vb, vb_t = dbl("vb", [128, 256], BF16)
    qkT, qkT_t = dbl("qkT", [64, 8, 128], BF16)
    ksum, ksum_t = dbl("ksum", [64, 4], F32)
    kms = P.sb("kms", [64, 4, 32], F32)
    kms_t = T("kms")
    kmeanb = P.sb("kmeanb", [64, 4, 32], BF16)
    kmeanb_t = T("kmeanb")

    pb = [P.ps("pbT", [128, 8, 128], BF16)]
    pb_t = [T("pbT", True)]
    pqT = P.ps("pqT", [128, 8, 128], BF16); pqT_t = T("pqT", True)
    pqk = P.ps("pqk", [128, 512], F32); pqk_t = T("pqk", True)
    pv = P.ps("pv", [128, 512], F32); pv_t = T("pv", True)
    pS = [P.ps("pS%d" % i, [128, 512], F32) for i in range(2)]
    pS_t = [T("pS%d" % i, True) for i in range(2)]
    pO = [P.ps("pO%d" % i, [128, 512], F32) for i in range(2)]
    pO_t = [T("pO%d" % i, True) for i in range(2)]
    norm = NormCtx(P, gn, ident, ident_t, pb, pb_t)

    scr_t = []
    for j in range(NT):
        b = j % 2
        P.dma("sp", xin[b][:, :], xin_d[j * 128:(j + 1) * 128, :], writes=[xin_t[b]])
        P.dma("sp", cs[b][:, :], cs_d[j * 128:(j + 1) * 128, :], writes=[cs_t[b]])
        norm.run(xin[b][:, :], xin_t[b], lambda c, b=b: xnT[b][:, c, :], xnT_t[b])
        for c in range(8):
            P.op("pe", lambda e, c=c, b=b: e.matmul(pqk[:, :], lhsT=xnT[b][:, c, :], rhs=w[:, c, 0:512],
                                                    start=(c == 0), stop=(c == 7)),
                 reads=[w_t, xnT_t[b]], writes=[pqk_t])
        for c in range(8):
            P.op("pe", lambda e, c=c, b=b: e.matmul(pv[:, 0:256], lhsT=xnT[b][:, c, :], rhs=w[:, c, 512:768],
                                                    start=(c == 0), stop=(c == 7)),
                 reads=[w_t, xnT_t[b]], writes=[pv_t])
        P.op("act", lambda e, b=b: e.activation(out=qk32[b][:, 0:256], in_=pqk[:, 0:256], func=AF.Copy, scale=0.125),
             reads=[pqk_t], writes=[qk32_t[b]])
        P.op("act", lambda e, b=b: e.activation(out=qk32[b][:, 256:512], in_=pqk[:, 256:512], func=AF.Copy),
             reads=[pqk_t], writes=[qk32_t[b]])
        P.op("dve", lambda e, b=b: e.tensor_copy(out=vb[b][:, :], in_=pv[:, 0:256]), reads=[pv_t], writes=[vb_t[b]])
        tv = T("vs%d" % j)
        scr_t.append(tv)
        P.dma("sp", vs[j * 128:(j + 1) * 128, :], vb[b][:, :], reads=[vb_t[b]], writes=[tv])
        qv = qk32[b][:, :].rearrange("p (g d) -> p g d", d=64)
        x1, x2 = qv[:, :, 0:8], qv[:, :, 8:16]
        cosv = cs[b][:, 0:64].rearrange("p (g f) -> p g f", f=8)
        sinv = cs[b][:, 64:128].rearrange("p (g f) -> p g f", f=8)
        r = rt[b]
        rv = [r[:, i, :].rearrange("p (g f) -> p g f", f=8) for i in range(4)]
        for i, (a0, a1) in enumerate(((x1, cosv), (x2, sinv), (x2, cosv), (x1, sinv))):
            P.op("dve", lambda e, i=i, a0=a0, a1=a1, rv=rv: e.tensor_tensor(out=rv[i], in0=a0, in1=a1, op=ALU.mult),
                 reads=[qk32_t[b], cs_t[b]], writes=[rt_t[b]])
        P.op("dve", lambda e, x1=x1, rv=rv: e.tensor_tensor(out=x1, in0=rv[0], in1=rv[1], op=ALU.subtract),
             reads=[rt_t[b]], writes=[qk32_t[b]])
        P.op("dve", lambda e, x2=x2, rv=rv: e.tensor_tensor(out=x2, in0=rv[2], in1=rv[3], op=ALU.add),
             reads=[rt_t[b]], writes=[qk32_t[b]])
        P.op("act", lambda e, b=b: e.activation(out=qkb[b][:, :], in_=qk32[b][:, :], func=AF.Copy),
             reads=[qk32_t[b]], writes=[qkb_t[b]])
        for g in range(8):
            P.op("pe", lambda e, g=g, b=b: e.transpose(out=pqT[0:64, g, :], in_=qkb[b][:, g * 64:(g + 1) * 64],
                                                       identity=ident[:, :]),
                 reads=[qkb_t[b], ident_t], writes=[pqT_t])
        P.op("dve", lambda e, b=b: e.tensor_copy(out=qkT[b][:, :, :], in_=pqT[0:64, :, :]),
             reads=[pqT_t], writes=[qkT_t[b]])
        tq = T("qs%d" % j)
        tk = T("ks%d" % j)
        scr_t += [tq, tk]
        P.dma("sp", qTs[:, :, j * 128:(j + 1) * 128].rearrange("h d n -> d h n"), qkT[b][:, 0:4, :],
              reads=[qkT_t[b]], writes=[tq])
        P.dma("sp", kTs[:, :, j * 128:(j + 1) * 128].rearrange("h d n -> d h n"), qkT[b][:, 4:8, :],
              reads=[qkT_t[b]], writes=[tk])
        blk = j // 2
        if j % 2 == 0:
            P.op("dve", lambda e, b=b, blk=blk: e.tensor_reduce(out=kms[:, :, blk], in_=qkT[b][:, 4:8, :],
                                                                axis=AX.X, op=ALU.add),
                 reads=[qkT_t[b]], writes=[kms_t])
        else:
            P.op("dve", lambda e, b=b: e.tensor_reduce(out=ksum[b][:, :], in_=qkT[b][:, 4:8, :],
                                                       axis=AX.X, op=ALU.add),
                 reads=[qkT_t[b]], writes=[ksum_t[b]])
            P.op("dve", lambda e, b=b, blk=blk: e.tensor_tensor(out=kms[:, :, blk], in0=kms[:, :, blk],
                                                                in1=ksum[b][:, :], op=ALU.add),
                 reads=[ksum_t[b], kms_t], writes=[kms_t])
    P.op("act", lambda e: e.activation(out=kmeanb[:, :, :], in_=kms[:, :, :], func=AF.Copy, scale=1.0 / 256.0),
         reads=[kms_t], writes=[kmeanb_t])

    KA_t, QA_t, VA_t = T("KA"), T("QA"), T("VA")
    QAb_t = [T("QAb%d" % g) for g in range(16)]
    ab = P.sb("ab", [64, 2048], BF16); ab_t = T("ab")
    km4 = P.sb("km4", [64, 8], F32); km4_t = T("km4")
    kmaxb = P.sb("kmaxb", [64, 2], BF16); kmaxb_t = T("kmaxb")
    gm, gm_t = dbl("gm", [128, 32], F32)
    sel, sel_t = dbl("sel", [128, 32], F32)
    top8, top8_t = dbl("top8", [128, 8], F32)
    mq, mq_t = dbl("mq", [128, 1], F32)
    BT, BT_t = dbl("BT", [128, 4, 96], F32)
    for i in range(2):
        P.op("pool", lambda e, i=i: e.memset(BT[i][:, :, :], 0.0), writes=[BT_t[i]])
    Pb = [P.sb("Pb%d" % i, [128, 512], BF16) for i in range(3)]
    Pb_t = [T("Pb%d" % i) for i in range(3)]
    OS, OS_t = dbl("OS", [128, 512], F32)
    DN, DN_t = dbl("DN", [64, 512], F32)
    OTs, OTs_t = dbl("OTs", [64, 512], BF16)
    pG, pG_t = pqk, pqk_t
    pBT, pBT_t = pv, pv_t
    kS = 0
    kP = 0
    ka_l = [T("kal%d" % i) for i in range(4)]
    qa_l = [T("qal%d" % i) for i in range(4)]
    va_l = [T("val%d" % i) for i in range(4)]
    qabs_l = [T("qabs%d" % i) for i in range(4)]
    for h in range(4):
        for q4 in range(4):
            sl = slice(q4 * 2048, (q4 + 1) * 2048)
            P.dma("sp", KA[0:64, sl], kTs[h, :, sl], reads=scr_t, writes=[ka_l[q4]])
            P.dma("sp", QA[0:64, sl], qTs[h, :, sl], reads=scr_t, writes=[qa_l[q4]])
            P.dma("pool", VA[:, q4 * 16:(q4 + 1) * 16, 0:64],
                  vs[q4 * 2048:(q4 + 1) * 2048, h * 64:(h + 1) * 64].rearrange("(t p) c -> p t c", p=128),
                  reads=scr_t, writes=[va_l[q4]])
        for q4 in range(4):
            sl = slice(q4 * 2048, (q4 + 1) * 2048)
            P.op("act", lambda e, sl=sl: e.activation(out=qabs[:, sl], in_=QA[0:64, sl], func=AF.Abs),
                 reads=[qa_l[q4]], writes=[qabs_l[q4]])
            P.op("act", lambda e, sl=sl: e.activation(out=ab[:, :], in_=KA[0:64, sl], func=AF.Abs),
                 reads=[ka_l[q4]], writes=[ab_t])
            P.op("dve", lambda e, q4=q4: e.tensor_reduce(out=km4[:, q4:q4 + 1], in_=ab[:, :], axis=AX.X, op=ALU.max),
                 reads=[ab_t], writes=[km4_t])
        P.op("dve", lambda e: e.tensor_reduce(out=km4[:, 4:5], in_=km4[:, 0:4], axis=AX.X, op=ALU.max),
             reads=[km4_t], writes=[km4_t])
        P.op("dve", lambda e: e.tensor_copy(out=kmaxb[:, 0:1], in_=km4[:, 4:5]), reads=[km4_t], writes=[kmaxb_t])
        for g in range(16):
            bt, bt_t = BT[g % 2], BT_t[g % 2]
            for qi in range(4):
                qt = g * 4 + qi
                cur = qt // 2
                k2 = qt % 2
                csl = slice(qt * 128, (qt + 1) * 128)
                P.op("pe", lambda e, csl=csl, h=h: e.matmul(pG[:, 0:32], lhsT=QA[0:64, csl], rhs=kmeanb[:, h, :],
                                                            start=True, stop=True),
                     reads=[qa_l, kmeanb_t], writes=[pG_t])
                P.op("pe", lambda e, csl=csl: e.matmul(pG[:, 32:33], lhsT=qabs[:, csl], rhs=kmaxb[:, 0:1],
                                                       start=True, stop=True),
                     reads=[qabs_l, kmaxb_t], writes=[pG_t])
                P.op("dve", lambda e, k2=k2, cur=cur: e.tensor_tensor(out=gm[k2][:, :], in0=pG[:, 0:32],
                                                                      in1=M1[:, cur * 32:(cur + 1) * 32], op=ALU.add),
                     reads=[pG_t, cst_t], writes=[gm_t[k2]])
                P.op("dve", lambda e, k2=k2: e.tensor_copy(out=mq[k2][:, :], in_=pG[:, 32:33]),
                     reads=[pG_t], writes=[mq_t[k2]])
                P.op("dve", lambda e, k2=k2: e.max(out=top8[k2][:, :], in_=gm[k2][:, :]),
                     reads=[gm_t[k2]], writes=[top8_t[k2]])
                P.op("dve", lambda e, k2=k2: e.tensor_scalar(out=sel[k2][:, :], in0=gm[k2][:, :],
                                                             scalar1=top8[k2][:, 2:3], scalar2=1.0,
                                                             op0=ALU.is_ge, op1=ALU.subtract),
                     reads=[gm_t[k2], top8_t[k2]], writes=[sel_t[k2]])
                P.op("dve", lambda e, k2=k2, cur=cur: e.tensor_tensor(out=sel[k2][:, :], in0=sel[k2][:, :],
                                                                      in1=A30[:, cur * 32:(cur + 1) * 32], op=ALU.mult),
                     reads=[sel_t[k2], cst_t], writes=[sel_t[k2]])
                P.op("dve", lambda e, k2=k2, cur=cur: e.tensor_tensor(out=sel[k2][:, :], in0=sel[k2][:, :],
                                                                      in1=Bc[:, cur * 32:(cur + 1) * 32], op=ALU.add),
                     reads=[sel_t[k2], cst_t], writes=[sel_t[k2]])
                P.op("dve", lambda e, k2=k2, bt=bt, qi=qi: e.tensor_scalar(out=bt[:, qi, 64:96], in0=sel[k2][:, :],
                                                                           scalar1=mq[k2][:, 0:1], scalar2=None,
                                                                           op0=ALU.subtract),
                     reads=[sel_t[k2], mq_t[k2]], writes=[bt_t])
                P.op("pe", lambda e, bt=bt, qi=qi: e.transpose(out=pBT[0:96, qi * 128:(qi + 1) * 128], in_=bt[:, qi, :],
                                                               identity=identf[:, :]),
                     reads=[bt_t, cst_t], writes=[pBT_t])
            P.op("act", lambda e, g=g: e.activation(out=QA[64:96, g * 512:(g + 1) * 512], in_=pBT[64:96, :], func=AF.Copy),
                 reads=[pBT_t], writes=[QAb_t[g]])
        for g in range(16):
            po, po_t = pO[g % 2], pO_t[g % 2]
            nk = 4 * g + 4
            for kt in range(nk):
                i = kt - 4 * g
                c0 = max(i, 0) * 128
                ps, ps_t = pS[kS % 2], pS_t[kS % 2]
                kS += 1
                pbuf, pbuf_t = Pb[kP % 3], Pb_t[kP % 3]
                kP += 1
                P.op("pe", lambda e, ps=ps, kt=kt, g=g, c0=c0, i=i: e.matmul(
                    ps[:, c0:512], lhsT=KA[0:96, kt * 128:(kt + 1) * 128], rhs=QA[0:96, g * 512 + c0:(g + 1) * 512],
                    start=True, stop=(i < 0)),
                    reads=[ka_l, KAoh_t, qa_l, QAb_t[g]], writes=[ps_t])
                if i >= 0:
                    P.op("pe", lambda e, ps=ps, c0=c0: e.matmul(ps[:, c0:c0 + 128], lhsT=ident[:, :], rhs=tri[:, :],
                                                                start=False, stop=True),
                         reads=[ident_t, cst_t], writes=[ps_t])
                P.op("act", lambda e, ps=ps, pbuf=pbuf, c0=c0: e.activation(out=pbuf[:, c0:512], in_=ps[:, c0:512],
                                                                            func=AF.Exp),
                     reads=[ps_t], writes=[pbuf_t])
                P.op("pe", lambda e, po=po, pbuf=pbuf, kt=kt, c0=c0, nk=nk: e.matmul(
                    po[:, c0:512], lhsT=VA[:, kt, :], rhs=pbuf[:, c0:512], start=(kt == 0), stop=(kt == nk - 1)),
                    reads=[va_l, VA1_t, pbuf_t], writes=[po_t])
            o = g % 2
            P.op("act", lambda e, o=o, po=po: e.activation(out=OS[o][:, :], in_=po[:, :], func=AF.Copy),
                 reads=[po_t], writes=[OS_t[o]])
            P.dma("sp", DN[o][:, :], OS[o][64:128, :], reads=[OS_t[o]], writes=[DN_t[o]])
            P.op("dve", lambda e, o=o: e.reciprocal(out=DN[o][:, :], in_=DN[o][:, :]), reads=[DN_t[o]], writes=[DN_t[o]])
            P.op("dve", lambda e, o=o: e.tensor_tensor(out=OTs[o][:, :], in0=OS[o][0:64, :], in1=DN[o][:, :], op=ALU.mult),
                 reads=[OS_t[o], DN_t[o]], writes=[OTs_t[o]])
            P.dma("sp", oT[h * 64:(h + 1) * 64, g * 512:(g + 1) * 512], OTs[o][:, :], reads=[OTs_t[o]],
                  writes=[T("oT")], final=True)
    return P.build()


_PROGS = {}


def _prog(name):
    if name not in _PROGS:
        _PROGS[name] = {"attn": build_attn, "mlstm": build_mlstm,
                        "pf0": lambda: build_projffn(False), "pf1": lambda: build_projffn(True)}[name]()
    return _PROGS[name]


def _run(name, maps):
    res = run_bass_kernel_spmd(_prog(name), maps, core_ids=list(range(NCORE)))
    return res.results


def _gain_layout(g):
    return np.ascontiguousarray(np.asarray(g, np.float32).reshape(8, 128).T)


def _consts():
    bf = ml_dtypes.bfloat16
    c = {}
    c["ident"] = np.eye(128, dtype=np.float32).astype(bf)
    c["identf"] = np.eye(128, dtype=np.float32)
    pos = np.arange(S, dtype=np.float32)
    inv = (np.float32(500000.0) ** (-np.arange(0, 16, 2, dtype=np.float32) / np.float32(16))).astype(np.float32)
    ang = (pos[:, None] * inv[None, :]).astype(np.float32)
    cos = np.cos(ang).astype(np.float32)
    sin = np.sin(ang).astype(np.float32)
    c["cs"] = np.ascontiguousarray(np.concatenate([np.tile(cos, (1, 8)), np.tile(sin, (1, 8))], axis=1))
    blk = np.arange(S) // 256
    c["onehot"] = (blk[None, :] == np.arange(32)[:, None]).astype(np.float32).astype(bf)
    kk = np.arange(128)
    c["tri"] = np.where(kk[:, None] > kk[None, :], NEG, 0.0).astype(np.float32).astype(bf)
    cur = np.arange(32)[:, None]
    n = np.arange(32)[None, :]
    m1 = np.where(n < cur, 0.0, NEG).astype(np.float32).reshape(1, 1024)
    a30 = np.where(n < cur, -NEG, 0.0).astype(np.float32).reshape(1, 1024)
    bc = np.where(n <= cur, 0.0, NEG).astype(np.float32).reshape(1, 1024)
    c["M1"] = np.ascontiguousarray(np.tile(m1, (128, 1)))
    c["A30"] = np.ascontiguousarray(np.tile(a30, (128, 1)))
    c["Bc"] = np.ascontiguousarray(np.tile(bc, (128, 1)))
    c["U"] = np.triu(np.ones((128, 128), np.float32))
    c["ones"] = np.ones((128, 128), np.float32)
    return c


def kernel(x, attn_norm, attn_w_qkv, attn_w_o, mlstm_norm, mlstm_w_in, mlstm_b_gates,
           mlstm_head_norm, mlstm_w_out, ffn_norm, ffn_w_gate_up, ffn_w_down, final_norm):
    f32 = np.float32
    x = np.asarray(x, f32)
    c = _consts()
    wqkv = np.asarray(attn_w_qkv, f32)[0]
    maps = []
    for core in range(NCORE):
        b, hg = core // 4, core % 4
        cols = np.concatenate([np.arange(hg * 256, (hg + 1) * 256) + off for off in (0, 1024, 2048)])
        maps.append({"xin": x[b], "gn": _gain_layout(attn_norm[0]), "wqkv": np.ascontiguousarray(wqkv[:, cols]),
                     "cs": c["cs"], "ident": c["ident"], "identf": c["identf"], "onehot": c["onehot"],
                     "tri": c["tri"], "M1": c["M1"], "A30": c["A30"], "Bc": c["Bc"]})
    ra = _run("attn", maps)
    oT = [np.concatenate([ra[b * 4 + hg]["oT"] for hg in range(4)], axis=0) for b in range(B)]
    maps = []
    for core in range(NCORE):
        b, q = core // 4, core % 4
        sl = slice(q * 2048, (q + 1) * 2048)
        maps.append({"hin": x[b, sl], "aT": np.ascontiguousarray(oT[b][:, sl]), "wp": np.asarray(attn_w_o, f32)[0],
                     "gn": _gain_layout(ffn_norm[0]), "wgu": np.asarray(ffn_w_gate_up, f32)[0],
                     "wd": np.asarray(ffn_w_down, f32)[0], "ident": c["ident"]})
    rb = _run("pf0", maps)
    h1 = [np.concatenate([rb[b * 4 + q]["hout"] for q in range(4)], axis=0) for b in range(B)]
    win = np.asarray(mlstm_w_in, f32)[0]
    bgv = np.asarray(mlstm_b_gates, f32)[0]
    hnv = np.asarray(mlstm_head_norm, f32)[0]
    maps = []
    for core in range(NCORE):
        b, hp = core // 4, core % 4
        h0 = 2 * hp
        wq = win[:, h0 * 64:(h0 + 2) * 64]
        wk = win[:, 512 + h0 * 64:512 + (h0 + 2) * 64]
        wv = win[:, 1024 + h0 * 128:1024 + (h0 + 2) * 128]
        wo = win[:, 2048 + h0 * 128:2048 + (h0 + 2) * 128]
        gi = win[:, 3072 + h0:3072 + h0 + 2]
        gf = win[:, 3080 + h0:3080 + h0 + 2]
        wtok = np.ascontiguousarray(np.concatenate([wk, wv, wo, gi, gf], axis=1))
        bg4 = np.concatenate([bgv[h0:h0 + 2], bgv[8 + h0:8 + h0 + 2]])
        maps.append({"hin": h1[b], "gn": _gain_layout(mlstm_norm[0]), "wq": np.ascontiguousarray(wq),
                     "wk": np.ascontiguousarray(wk), "wtok": wtok,
                     "bg": np.ascontiguousarray(np.tile(bg4[None, :], (128, 1))),
                     "hn": np.ascontiguousarray(np.tile(hnv[None, h0 * 128:(h0 + 2) * 128], (128, 1))),
                     "ident": c["ident"], "U": c["U"], "ones": c["ones"]})
    rc = _run("mlstm", maps)
    yT = [np.ascontiguousarray(np.concatenate([rc[b * 4 + hp]["y"] for hp in range(4)], axis=1).T)
          for b in range(B)]
    maps = []
    for core in range(NCORE):
        b, q = core // 4, core % 4
        sl = slice(q * 2048, (q + 1) * 2048)
        maps.append({"hin": np.ascontiguousarray(h1[b][sl]), "aT": np.ascontiguousarray(yT[b][:, sl]),
                     "wp": np.asarray(mlstm_w_out, f32)[0], "gn": _gain_layout(ffn_norm[1]),
                     "wgu": np.asarray(ffn_w_gate_up, f32)[1], "wd": np.asarray(ffn_w_down, f32)[1],
                     "ident": c["ident"], "fnw": np.asarray(final_norm, f32)})
    rd = _run("pf1", maps)
    out = np.stack([np.concatenate([rd[b * 4 + q]["hout"] for q in range(4)], axis=0) for b in range(B)])
    return out.astype(f32)
```

```python
import math
from contextlib import ExitStack

import numpy as np
import ml_dtypes

import concourse.bass as bass
import concourse.mybir as mybir
from concourse.bass_utils import run_bass_kernel_spmd

F32 = mybir.dt.float32
BF16 = mybir.dt.bfloat16
AF = mybir.ActivationFunctionType
ALU = mybir.AluOpType
AX = mybir.AxisListType

D = 1024
B = 2
S = 8192
NCORE = 8
DFF = 2816
EPS = 1e-6
NEG = -30000.0


class T:
    __slots__ = ("name", "w", "r", "psum")

    def __init__(self, name, psum=False):
        self.name = name
        self.w = None
        self.r = {}
        self.psum = psum


def _flat(x):
    out = []
    for a in x:
        if isinstance(a, (list, tuple)):
            out.extend(_flat(a))
        elif a is not None:
            out.append(a)
    return out


class Prog:
    ENGS = ("pe", "act", "dve", "pool", "sp")
    NRING = 8

    def __init__(self):
        self.nc = bass.Bass("TRN2", target_bir_lowering=False)
        self.ins = {e: [] for e in self.ENGS}
        self.ndma = {e: 0 for e in self.ENGS}
        self.dma_idx = {e: [] for e in self.ENGS}
        self.stack = ExitStack()
        self.final = []

    def sb(self, name, shape, dt):
        return self.stack.enter_context(self.nc.sbuf_tensor(name, list(shape), dt))

    def ps(self, name, shape, dt):
        return self.stack.enter_context(self.nc.psum_tensor(name, list(shape), dt))

    def dram_in(self, name, shape, dt):
        return self.nc.dram_tensor(name, list(shape), dt, kind="ExternalInput").ap()

    def dram_out(self, name, shape, dt):
        return self.nc.dram_tensor(name, list(shape), dt, kind="ExternalOutput").ap()

    def dram_tmp(self, name, shape, dt):
        return self.nc.dram_tensor(name, list(shape), dt).ap()

    COST0 = {"pe": 40.0, "act": 220.0, "dve": 120.0, "pool": 250.0, "sp": 60.0}
    COSTN = {"pe": 0.42, "act": 1.05, "dve": 0.8, "pool": 1.6, "sp": 0.0}

    def _emit(self, eng, fn, reads, writes, dma, n=128):
        reads, writes = _flat(reads), _flat(writes)
        lst = self.ins[eng]
        idx = len(lst)
        raw, other = set(), set()
        for t in reads:
            if t.w is not None:
                raw.add(t.w)
            if t.psum:
                for e2, i2 in t.r.items():
                    if e2 != eng:
                        other.add((e2, i2))
        for t in writes:
            if t.w is not None:
                other.add(t.w)
            for e2, i2 in t.r.items():
                other.add((e2, i2))
        deps, order = set(), set()

        def same_compute(d):
            return d[0] == eng and not dma and self.ins[eng][d[1]]["dma"] is None

        for d in raw:
            if same_compute(d) and eng == "pe":
                order.add(d[1])
            else:
                deps.add(d)
        for d in other:
            if same_compute(d):
                order.add(d[1])
            else:
                deps.add(d)
        deps.discard((eng, idx))
        cost = self.COST0[eng] + self.COSTN[eng] * n
        rec = dict(fn=fn, deps=deps, order=order, dma=None, cost=cost)
        if dma:
            rec["dma"] = -1
            rec["cost"] = 2000.0 + n * 0.01
        lst.append(rec)
        for t in reads:
            t.r[eng] = idx
        for t in writes:
            t.w = (eng, idx)
            t.r = {}
        return idx

    def op(self, eng, fn, reads=(), writes=(), n=128):
        return self._emit(eng, fn, reads, writes, False, n)

    def dma(self, eng, out, in_, reads=(), writes=(), final=False, n=65536):
        i = self._emit(eng, lambda e: e.dma_start(out=out, in_=in_), reads, writes, True, n)
        if final:
            self.final.append((eng, i))
        return i

    WINDOW = 48

    def schedule(self):
        ENGS = self.ENGS
        ins = self.ins
        n_tot = sum(len(ins[e]) for e in ENGS)
        done = {e: [None] * len(ins[e]) for e in ENGS}
        pend = {e: list(range(len(ins[e]))) for e in ENGS}
        free = {e: 0.0 for e in ENGS}
        neword = {e: [] for e in ENGS}
        count = 0
        while count < n_tot:
            best = None
            for e in ENGS:
                p = pend[e]
                lim = min(len(p), self.WINDOW)
                for k in range(lim):
                    i = p[k]
                    rec = ins[e][i]
                    ok = True
                    rt = free[e]
                    for o in rec["order"]:
                        if done[e][o] is None:
                            ok = False
                            break
                    if not ok:
                        continue
                    for (e2, i2) in rec["deps"]:
                        dt = done[e2][i2]
                        if dt is None:
                            ok = False
                            break
                        if dt > rt:
                            rt = dt
                    if not ok:
                        continue
                    key = (rt, k)
                    if best is None or key < best[0]:
                        best = (key, e, k, i, rt)
                    if rt <= free[e]:
                        break
            assert best is not None, "scheduler deadlock"
            _, e, k, i, rt = best
            rec = ins[e][i]
            if rec["dma"] is not None:
                free[e] = rt + 60.0
                done[e][i] = rt + rec["cost"]
            else:
                free[e] = rt + rec["cost"]
                done[e][i] = rt + rec["cost"] + 60.0
            pend[e].pop(k)
            neword[e].append(i)
            count += 1
        self.sim_time = max(max([x for x in done[e] if x is not None] + [0.0]) for e in ENGS)
        pos = {e: {old: new for new, old in enumerate(neword[e])} for e in ENGS}
        for e in ENGS:
            newl = []
            for old in neword[e]:
                rec = ins[e][old]
                rec["deps"] = {(e2, pos[e2][i2]) for (e2, i2) in rec["deps"]}
                newl.append(rec)
            ins[e] = newl
        self.final = [(e, pos[e][i]) for (e, i) in self.final]
        for e in ENGS:
            k = 0
            idxs = []
            for i, rec in enumerate(ins[e]):
                if rec["dma"] is not None:
                    rec["dma"] = k
                    if k >= self.NRING:
                        rec["deps"].add((e, idxs[k - self.NRING]))
                    idxs.append(i)
                    k += 1
            self.ndma[e] = k

    def build(self):
        nc = self.nc
        st = self.stack
        self.schedule()
        final_deps = set(self.final)
        needed = {e: set() for e in self.ENGS}
        for e in self.ENGS:
            for rec in self.ins[e]:
                for (e2, i2) in rec["deps"]:
                    needed[e2].add(i2)
        cnt_sem = {e: st.enter_context(nc.semaphore("c_" + e)) for e in self.ENGS}
        ring = {e: [st.enter_context(nc.semaphore("r_%s%d" % (e, i))) for i in range(self.NRING)]
                for e in self.ENGS if self.ndma[e] > 0}
        sig = {e: {} for e in self.ENGS}
        for e in self.ENGS:
            c = 0
            for i, rec in enumerate(self.ins[e]):
                if rec["dma"] is not None:
                    k = rec["dma"]
                    sig[e][i] = (ring[e][k % self.NRING], 16 * (k // self.NRING + 1))
                elif i in needed[e]:
                    c += 1
                    sig[e][i] = (cnt_sem[e], c)
        self.stats = {e: (len(self.ins[e]), self.ndma[e]) for e in self.ENGS}
        block = st.enter_context(nc.Block())
        handles = {"pe": block.tensor, "act": block.scalar, "dve": block.vector,
                   "pool": block.gpsimd, "sp": block.sync}

        def make(e):
            def body(eng):
                waited = {}
                for i, rec in enumerate(self.ins[e]):
                    for d in sorted(rec["deps"]):
                        sem, val = sig[d[0]][d[1]]
                        key = id(sem)
                        if waited.get(key, 0) >= val:
                            continue
                        waited[key] = val
                        eng.wait_ge(sem, val)
                    inst = rec["fn"](eng)
                    if i in sig[e]:
                        sem, val = sig[e][i]
                        inst.then_inc(sem, 16 if rec["dma"] is not None else 1)
                if e == "sp":
                    for d in sorted(final_deps):
                        sem, val = sig[d[0]][d[1]]
                        if waited.get(id(sem), 0) >= val:
                            continue
                        waited[id(sem)] = val
                        eng.wait_ge(sem, val)
            return body

        for e in self.ENGS:
            handles[e](make(e))
        st.close()
        return nc


def load_w_bf16(P, name, w_dram, kdim, ncols, col0=0, tile=None, tr=None):
    kc = kdim // 128
    if tile is None:
        tile = P.sb(name, [128, kc, ncols], BF16)
    src = w_dram[:, col0:col0 + ncols].rearrange("(c p) n -> p c n", p=128)
    step = max(1, kc // 4)
    trs = []
    for c0 in range(0, kc, step):
        c1 = min(kc, c0 + step)
        tr = T(name + str(c0))
        trs.append(tr)
        P.dma("pool", tile[:, c0:c1, :], src[:, c0:c1, :], writes=[tr])
    return tile, trs


class NormCtx:
    def __init__(self, P, gain_dram, ident, ident_t, pst, pst_t):
        self.P = P
        self.ident, self.ident_t = ident, ident_t
        self.pst, self.pst_t = pst, pst_t
        self.g = P.sb("ng_" + gain_dram.tensor.name, [128, 8], F32)
        self.g_t = T("ng")
        P.dma("sp", self.g[:, :], gain_dram[:, :], writes=[self.g_t])
        self.junk = P.sb("nj_" + gain_dram.tensor.name, [128, 1024], BF16)
        self.junk_t = T("nj")
        self.ss = [P.sb("nss%d_" % i + gain_dram.tensor.name, [128, 2], F32) for i in range(2)]
        self.ss_t = [T("nss%d" % i) for i in range(2)]
        self.xs = [P.sb("nxs%d_" % i + gain_dram.tensor.name, [128, 1024], BF16) for i in range(2)]
        self.xs_t = [T("nxs%d" % i) for i in range(2)]
        self.k = 0

    def run(self, h_ap, h_t, dst_fn, dst_t):
        P = self.P
        k = self.k
        self.k += 1
        ss, ss_t = self.ss[k % 2], self.ss_t[k % 2]
        xs, xs_t = self.xs[k % 2], self.xs_t[k % 2]
        pst, pst_t = self.pst[k % len(self.pst)], self.pst_t[k % len(self.pst)]
        junk, junk_t = self.junk, self.junk_t
        P.op("act", lambda e: e.activation(out=junk[:, :], in_=h_ap, func=AF.Square,
                                           accum_out=ss[:, 0:1]),
             reads=[h_t], writes=[junk_t, ss_t], n=1024)
        P.op("dve", lambda e: e.tensor_scalar(out=ss[:, 1:2], in0=ss[:, 0:1], scalar1=1.0 / D,
                                              scalar2=EPS, op0=ALU.mult, op1=ALU.add),
             reads=[ss_t], writes=[ss_t])
        P.op("act", lambda e: e.activation(out=ss[:, 1:2], in_=ss[:, 1:2], func=AF.Sqrt),
             reads=[ss_t], writes=[ss_t])
        P.op("dve", lambda e: e.reciprocal(out=ss[:, 1:2], in_=ss[:, 1:2]),
             reads=[ss_t], writes=[ss_t])
        P.op("dve", lambda e: e.tensor_scalar(out=xs[:, :], in0=h_ap, scalar1=ss[:, 1:2],
                                              scalar2=None, op0=ALU.mult),
             reads=[h_t, ss_t], writes=[xs_t], n=1024)
        for c in range(8):
            P.op("pe", lambda e, c=c: e.transpose(out=pst[:, c, :], in_=xs[:, c * 128:(c + 1) * 128],
                                                  identity=self.ident[:, :]),
                 reads=[xs_t, self.ident_t], writes=[pst_t])
        g = self.g
        for c in range(8):
            P.op("act", lambda e, c=c: e.activation(out=dst_fn(c), in_=pst[:, c, :], func=AF.Copy,
                                                    scale=g[:, c:c + 1]),
                 reads=[pst_t, self.g_t], writes=[dst_t])


def make_ident(P, ident_dram):
    ident = P.sb("ident_sb", [128, 128], BF16)
    ident_t = T("ident")
    P.dma("sp", ident[:, :], ident_dram[:, :], writes=[ident_t])
    return ident, ident_t


FFN_PARTS = [(0, 5), (5, 5), (10, 4), (14, 4), (18, 4)]


def build_projffn(final):
    P = Prog()
    NT = 16
    hin = P.dram_in("hin", [2048, D], F32)
    aT = P.dram_in("aT", [D, 2048], BF16)
    wp = P.dram_in("wp", [D, D], F32)
    gn = P.dram_in("gn", [128, 8], F32)
    wgu = P.dram_in("wgu", [D, 2 * DFF], F32)
    wd = P.dram_in("wd", [DFF, D], F32)
    identd = P.dram_in("ident", [128, 128], BF16)
    if final:
        fn = P.dram_in("fnw", [D], F32)
    hout = P.dram_out("hout", [2048, D], F32)

    ident, ident_t = make_ident(P, identd)
    h = P.sb("h", [128, NT, D], F32)
    h_t = [T("h%d" % i) for i in range(NT)]
    xnT = P.sb("xnT", [128, 8, 2048], BF16)
    xn_t = [T("xn%d" % i) for i in range(NT)]
    wps, wps_t = load_w_bf16(P, "wps", wp, D, D)
    at = [P.sb("at%d" % i, [128, 8, 128], BF16) for i in range(2)]
    at_t = [T("at%d" % i) for i in range(2)]
    pf = [P.ps("pf%d" % i, [128, 512], F32) for i in range(6)]
    pf_t = [T("pf%d" % i, True) for i in range(6)]
    pb = [P.ps("pb%d" % i, [128, 8, 128], BF16) for i in range(2)]
    pb_t = [T("pb%d" % i, True) for i in range(2)]
    norm = NormCtx(P, gn, ident, ident_t, pb, pb_t)
    pfk = [0]

    def next_pf():
        i = pfk[0] % 6
        pfk[0] += 1
        return pf[i], pf_t[i]

    for t in range(NT):
        P.dma("sp", h[:, t, :], hin[t * 128:(t + 1) * 128, :], writes=[h_t[t]])
        a, a_t = at[t % 2], at_t[t % 2]
        P.dma("sp", a[:, :, :], aT[:, t * 128:(t + 1) * 128].rearrange("(c p) n -> p c n", p=128),
              writes=[a_t])
        for nh in range(2):
            ps, ps_t = next_pf()
            for c in range(8):
                P.op("pe", lambda e, c=c, ps=ps, a=a, nh=nh: e.matmul(
                    ps[:, :], lhsT=a[:, c, :], rhs=wps[:, c, nh * 512:(nh + 1) * 512],
                    start=(c == 0), stop=(c == 7)),
                    reads=[a_t, wps_t], writes=[ps_t], n=512)
            P.op("dve", lambda e, ps=ps, t=t, nh=nh: e.tensor_tensor(
                out=h[:, t, nh * 512:(nh + 1) * 512], in0=h[:, t, nh * 512:(nh + 1) * 512],
                in1=ps[:, :], op=ALU.add),
                reads=[ps_t, h_t[t]], writes=[h_t[t]], n=512)
        norm.run(h[:, t, :], h_t[t], lambda c, t=t: xnT[:, c, t * 128:(t + 1) * 128], xn_t[t])

    wg_b = [P.sb("wg%d" % i, [128, 8, 640], BF16) for i in range(2)]
    wu_b = [P.sb("wu%d" % i, [128, 8, 640], BF16) for i in range(2)]
    wd_b = [P.sb("wd%d" % i, [128, 5, 1024], BF16) for i in range(2)]
    wg_t = [[T("wg%d_%d" % (i, q)) for q in range(4)] for i in range(2)]
    wu_t = [[T("wu%d_%d" % (i, q)) for q in range(4)] for i in range(2)]
    wd_t = [[T("wd%d_%d" % (i, q)) for q in range(3)] for i in range(2)]
    sg = [P.sb("sg%d" % i, [128, 512], F32) for i in range(2)]
    sg_t = [T("sg%d" % i) for i in range(2)]
    aF = [P.sb("aF%d" % i, [128, 5, 512], BF16) for i in range(2)]
    aF_t = [T("aF%d" % i) for i in range(2)]
    kk = 0
    for pi, (c0, ncn) in enumerate(FFN_PARTS):
        b = pi % 2
        ncols = ncn * 128
        srcg = wgu[:, c0 * 128:c0 * 128 + ncols].rearrange("(c p) n -> p c n", p=128)
        srcu = wgu[:, DFF + c0 * 128:DFF + c0 * 128 + ncols].rearrange("(c p) n -> p c n", p=128)
        for q in range(4):
            P.dma("pool", wg_b[b][:, 2 * q:2 * q + 2, 0:ncols], srcg[:, 2 * q:2 * q + 2, :], writes=[wg_t[b][q]])
            P.dma("pool", wu_b[b][:, 2 * q:2 * q + 2, 0:ncols], srcu[:, 2 * q:2 * q + 2, :], writes=[wu_t[b][q]])
        srcd = wd[c0 * 128:(c0 + ncn) * 128, :].rearrange("(c p) n -> p c n", p=128)
        for q in range(0, ncn, 2):
            q1 = min(ncn, q + 2)
            P.dma("pool", wd_b[b][:, q:q1, :], srcd[:, q:q1, :], writes=[wd_t[b][q // 2]])
        for tg in range(4):
            af, af_t = aF[tg % 2], aF_t[tg % 2]
            for j in range(ncn):
                psg, psg_t = next_pf()
                psu, psu_t = next_pf()
                for c in range(8):
                    P.op("pe", lambda e, c=c, j=j, psg=psg, b=b, tg=tg: e.matmul(
                        psg[:, :], lhsT=wg_b[b][:, c, j * 128:(j + 1) * 128],
                        rhs=xnT[:, c, tg * 512:(tg + 1) * 512], start=(c == 0), stop=(c == 7)),
                        reads=[wg_t[b]] + xn_t[tg * 4:tg * 4 + 4], writes=[psg_t], n=512)
                for c in range(8):
                    P.op("pe", lambda e, c=c, j=j, psu=psu, b=b, tg=tg: e.matmul(
                        psu[:, :], lhsT=wu_b[b][:, c, j * 128:(j + 1) * 128],
                        rhs=xnT[:, c, tg * 512:(tg + 1) * 512], start=(c == 0), stop=(c == 7)),
                        reads=[wu_t[b]] + xn_t[tg * 4:tg * 4 + 4], writes=[psu_t], n=512)
                s, s_t = sg[kk % 2], sg_t[kk % 2]
                kk += 1
                P.op("act", lambda e, s=s, psg=psg: e.activation(out=s[:, :], in_=psg[:, :], func=AF.Silu),
                     reads=[psg_t], writes=[s_t], n=512)
                P.op("dve", lambda e, s=s, psu=psu, af=af, j=j: e.tensor_tensor(
                    out=af[:, j, :], in0=s[:, :], in1=psu[:, :], op=ALU.mult),
                    reads=[s_t, psu_t], writes=[af_t], n=512)
            for tt in range(4):
                t = tg * 4 + tt
                for nh in range(2):
                    ps, ps_t = next_pf()
                    for j in range(ncn):
                        P.op("pe", lambda e, j=j, ps=ps, af=af, tt=tt, nh=nh, b=b: e.matmul(
                            ps[:, :], lhsT=af[:, j, tt * 128:(tt + 1) * 128],
                            rhs=wd_b[b][:, j, nh * 512:(nh + 1) * 512],
                            start=(j == 0), stop=(j == ncn - 1)),
                            reads=[af_t, wd_t[b]], writes=[ps_t], n=512)
                    P.op("dve", lambda e, ps=ps, t=t, nh=nh: e.tensor_tensor(
                        out=h[:, t, nh * 512:(nh + 1) * 512], in0=h[:, t, nh * 512:(nh + 1) * 512],
                        in1=ps[:, :], op=ALU.add),
                        reads=[ps_t, h_t[t]], writes=[h_t[t]], n=512)

    if final:
        fw = P.sb("fw", [128, D], F32)
        fw_t = T("fw")
        P.dma("sp", fw[:, :], fn.partition_broadcast(128), writes=[fw_t])
        ss = P.sb("fss", [128, 2 * NT], F32)
        ss_t = T("fss")
        junk = norm.junk
        for t in range(NT):
            P.op("act", lambda e, t=t: e.activation(out=junk[:, :], in_=h[:, t, :], func=AF.Square,
                                                    accum_out=ss[:, 2 * t:2 * t + 1]),
                 reads=[h_t[t]], writes=[norm.junk_t, ss_t])
            P.op("dve", lambda e, t=t: e.tensor_scalar(out=ss[:, 2 * t + 1:2 * t + 2], in0=ss[:, 2 * t:2 * t + 1],
                                                       scalar1=1.0 / D, scalar2=EPS, op0=ALU.mult, op1=ALU.add),
                 reads=[ss_t], writes=[ss_t])
            P.op("act", lambda e, t=t: e.activation(out=ss[:, 2 * t + 1:2 * t + 2],
                                                    in_=ss[:, 2 * t + 1:2 * t + 2], func=AF.Sqrt),
                 reads=[ss_t], writes=[ss_t])
            P.op("dve", lambda e, t=t: e.reciprocal(out=ss[:, 2 * t + 1:2 * t + 2],
                                                    in_=ss[:, 2 * t + 1:2 * t + 2]),
                 reads=[ss_t], writes=[ss_t])
            P.op("dve", lambda e, t=t: e.scalar_tensor_tensor(
                out=h[:, t, :], in0=h[:, t, :], scalar=ss[:, 2 * t + 1:2 * t + 2], in1=fw[:, :],
                op0=ALU.mult, op1=ALU.mult),
                reads=[h_t[t], ss_t, fw_t], writes=[h_t[t]])
    for t in range(NT):
        P.dma("sp", hout[t * 128:(t + 1) * 128, :], h[:, t, :], reads=[h_t[t]], writes=[T("o%d" % t)], final=True)
    return P.build()


def build_mlstm():
    P = Prog()
    NCH = S // 128
    hin = P.dram_in("hin", [S, D], F32)
    gn = P.dram_in("gn", [128, 8], F32)
    wq_d = P.dram_in("wq", [D, 128], F32)
    wk_d = P.dram_in("wk", [D, 128], F32)
    wt_d = P.dram_in("wtok", [D, 644], F32)
    bg_d = P.dram_in("bg", [128, 4], F32)
    hn_d = P.dram_in("hn", [128, 256], F32)
    identd = P.dram_in("ident", [128, 128], BF16)
    U_d = P.dram_in("U", [128, 128], F32)
    ones_d = P.dram_in("ones", [128, 128], F32)
    yout = P.dram_out("y", [S, 256], BF16)

    ident, ident_t = make_ident(P, identd)
    U = P.sb("U_sb", [128, 128], F32)
    ONES = P.sb("ones_sb", [128, 128], F32)
    bg = P.sb("bg_sb", [128, 4], F32)
    hn = P.sb("hn_sb", [128, 256], F32)
    c_t = T("consts")
    P.dma("sp", U[:, :], U_d[:, :], writes=[c_t])
    c2_t = T("consts2")
    P.dma("sp", ONES[:, :], ones_d[:, :], writes=[c2_t])
    c3_t = T("consts3")
    P.dma("sp", bg[:, :], bg_d[:, :], writes=[c3_t])
    c4_t = T("consts4")
    P.dma("sp", hn[:, :], hn_d[:, :], writes=[c4_t])
    wq, wq_t = load_w_bf16(P, "wq_sb", wq_d, D, 128)
    wk, wk_t = load_w_bf16(P, "wk_sb", wk_d, D, 128)
    wt, wt_t = load_w_bf16(P, "wt_sb", wt_d, D, 644)

    def dbl(name, shape, dt):
        return [P.sb("%s%d" % (name, i), shape, dt) for i in range(2)], [T("%s%d" % (name, i)) for i in range(2)]

    xin, xin_t = dbl("xin", [128, D], F32)
    xnT, xnT_t = dbl("xnT", [128, 8, 128], BF16)
    qT, qT_t = dbl("qT", [64, 2, 128], BF16)
    kT, kT_t = dbl("kT", [64, 2, 128], BF16)
    ktok, ktok_t = dbl("ktok", [128, 128], BF16)
    og, og_t = dbl("og", [128, 256], F32)
    gt, gt_t = dbl("gt", [128, 24], F32)
    V1 = [dbl("V1_%d_" % h, [128, 129], BF16) for h in range(2)]
    V2 = [dbl("V2_%d_" % h, [128, 129], BF16) for h in range(2)]
    PT, PT_t = dbl("PT", [128, 128], BF16)
    hh, hh_t = dbl("hh", [128, 128], F32)
    sq = P.sb("sqj", [128, 128], BF16)
    sq_t = T("sqj")
    st, st_t = dbl("st", [128, 8], F32)
    yt, yt_t = dbl("yt", [128, 256], BF16)
    C = [P.sb("C%d" % h, [64, 129], F32) for h in range(2)]
    C_t = [T("C%d" % h) for h in range(2)]
    Cb = [dbl("Cb%d_" % h, [64, 129], BF16) for h in range(2)]

    pb = [P.ps("pbT", [128, 8, 128], BF16)]
    pb_t = [T("pbT", True)]
    pq = P.ps("pq", [128, 512], F32); pq_t = T("pq", True)
    p1 = P.ps("p1", [128, 512], F32); p1_t = T("p1", True)
    p2 = P.ps("p2", [128, 512], F32); p2_t = T("p2", True)
    pg = P.ps("pg", [128, 512], F32); pg_t = T("pg", True)
    pS = P.ps("pS", [128, 512], F32); pS_t = T("pS", True)
    pO = P.ps("pO", [128, 512], F32); pO_t = T("pO", True)
    pA = P.ps("pA", [128, 512], F32); pA_t = T("pA", True)
    norm = NormCtx(P, gn, ident, ident_t, pb, pb_t)

    for h in range(2):
        P.op("dve", lambda e, h=h: e.memset(C[h][:, :], 0.0), writes=[C_t[h]])
        P.op("dve", lambda e, h=h: e.memset(Cb[h][0][0][:, :], 0.0), writes=[Cb[h][1][0]])

    for j in range(NCH):
        b = j % 2
        P.dma("sp", xin[b][:, :], hin[j * 128:(j + 1) * 128, :], writes=[xin_t[b]])
        norm.run(xin[b][:, :], xin_t[b], lambda c, b=b: xnT[b][:, c, :], xnT_t[b])
        for qi, (w, w_t) in enumerate(((wq, wq_t), (wk, wk_t))):
            for h in range(2):
                for c in range(8):
                    P.op("pe", lambda e, c=c, h=h, w=w, qi=qi, b=b: e.matmul(
                        pq[0:64, (2 * qi + h) * 128:(2 * qi + h + 1) * 128],
                        lhsT=w[:, c, h * 64:(h + 1) * 64], rhs=xnT[b][:, c, :],
                        start=(c == 0), stop=(c == 7)),
                        reads=[w_t, xnT_t[b]], writes=[pq_t])
        P.op("act", lambda e, b=b: e.activation(out=qT[b][:, :, :].rearrange("p a n -> p (a n)"),
                                                in_=pq[0:64, 0:256], func=AF.Copy, scale=0.125),
             reads=[pq_t], writes=[qT_t[b]])
        P.op("dve", lambda e, b=b: e.tensor_copy(out=kT[b][:, :, :].rearrange("p a n -> p (a n)"),
                                                 in_=pq[0:64, 256:512]),
             reads=[pq_t], writes=[kT_t[b]])
        for c in range(8):
            P.op("pe", lambda e, c=c, b=b: e.matmul(p1[:, 0:384], lhsT=xnT[b][:, c, :], rhs=wt[:, c, 0:384],
                                                    start=(c == 0), stop=(c == 7)),
                 reads=[wt_t, xnT_t[b]], writes=[p1_t])
        for c in range(8):
            P.op("pe", lambda e, c=c, b=b: e.matmul(p2[:, 0:260], lhsT=xnT[b][:, c, :], rhs=wt[:, c, 384:644],
                                                    start=(c == 0), stop=(c == 7)),
                 reads=[wt_t, xnT_t[b]], writes=[p2_t])
        g = gt[b]
        g_t = gt_t[b]
        P.op("dve", lambda e, g=g: e.tensor_tensor(out=g[:, 0:4], in0=p2[:, 256:260], in1=bg[:, :], op=ALU.add),
             reads=[p2_t, c3_t], writes=[g_t])
        P.op("act", lambda e, g=g: e.activation(out=g[:, 4:8], in_=g[:, 0:4], func=AF.Tanh, scale=1.0 / 15.0),
             reads=[g_t], writes=[g_t])
        P.op("act", lambda e, g=g: e.activation(out=g[:, 8:10], in_=g[:, 6:8], func=AF.Exp, scale=-15.0),
             reads=[g_t], writes=[g_t])
        P.op("act", lambda e, g=g: e.activation(out=g[:, 10:12], in_=g[:, 8:10], func=AF.Ln, bias=1.0),
             reads=[g_t], writes=[g_t])
        P.op("pe", lambda e, g=g: e.matmul(pg[:, 0:2], lhsT=U[:, :], rhs=g[:, 10:12], start=True, stop=True),
             reads=[g_t, c_t], writes=[pg_t])
        P.op("pe", lambda e, g=g: e.matmul(pg[:, 2:4], lhsT=ONES[:, :], rhs=g[:, 10:12], start=True, stop=True),
             reads=[g_t, c2_t], writes=[pg_t])
        P.op("dve", lambda e, g=g: e.tensor_copy(out=g[:, 12:16], in_=pg[:, 0:4]), reads=[pg_t], writes=[g_t])
        P.op("dve", lambda e, g=g: e.tensor_tensor(out=g[:, 16:18], in0=g[:, 12:14], in1=g[:, 14:16],
                                                   op=ALU.subtract),
             reads=[g_t], writes=[g_t])
        s = st[b]
        s_t = st_t[b]
        for h in range(2):
            P.op("act", lambda e, g=g, h=h: e.activation(out=g[:, 18 + h:19 + h], in_=g[:, 4 + h:5 + h], func=AF.Exp,
                                                         scale=15.0, bias=g[:, 12 + h:13 + h]),
                 reads=[g_t], writes=[g_t])
            P.op("act", lambda e, g=g, h=h: e.activation(out=g[:, 20 + h:21 + h], in_=g[:, 4 + h:5 + h], func=AF.Exp,
                                                         scale=15.0, bias=g[:, 16 + h:17 + h]),
                 reads=[g_t], writes=[g_t])
        P.op("act", lambda e, g=g: e.activation(out=g[:, 22:24], in_=g[:, 12:14], func=AF.Exp, scale=-1.0),
             reads=[g_t], writes=[g_t])
        P.op("act", lambda e, g=g, s=s: e.activation(out=s[:, 6:8], in_=g[:, 14:16], func=AF.Exp, scale=-1.0),
             reads=[g_t], writes=[s_t])
        P.op("act", lambda e, b=b: e.activation(out=ktok[b][:, :], in_=p1[:, 0:128], func=AF.Copy),
             reads=[p1_t], writes=[ktok_t[b]])
        P.op("act", lambda e, b=b: e.activation(out=og[b][:, :], in_=p2[:, 0:256], func=AF.Sigmoid),
             reads=[p2_t], writes=[og_t[b]])
        for h in range(2):
            for (V, col) in ((V1[h], 18 + h), (V2[h], 20 + h)):
                Vb, Vb_t = V[0][b], V[1][b]
                P.op("dve", lambda e, Vb=Vb, g=g, col=col, h=h: e.tensor_scalar(
                    out=Vb[:, 0:128], in0=p1[:, 128 + h * 128:256 + h * 128], scalar1=g[:, col:col + 1],
                    scalar2=None, op0=ALU.mult),
                    reads=[p1_t, g_t], writes=[Vb_t])
                P.op("act", lambda e, Vb=Vb, g=g, col=col: e.activation(out=Vb[:, 128:129], in_=g[:, col:col + 1],
                                                                        func=AF.Copy),
                     reads=[g_t], writes=[Vb_t])
        for h in range(2):
            Cb_cur, Cb_cur_t = Cb[h][0][b], Cb[h][1][b]
            Cb_nxt, Cb_nxt_t = Cb[h][0][1 - b], Cb[h][1][1 - b]
            V1b, V1b_t = V1[h][0][b], V1[h][1][b]
            V2b, V2b_t = V2[h][0][b], V2[h][1][b]
            P.op("pe", lambda e, h=h, b=b: e.matmul(pS[:, 0:128], lhsT=kT[b][:, h, :], rhs=qT[b][:, h, :],
                                                    start=True, stop=True),
                 reads=[kT_t[b], qT_t[b]], writes=[pS_t])
            pt, pt_t = PT[h], PT_t[h]
            P.op("dve", lambda e, pt=pt: e.tensor_tensor(out=pt[:, :], in0=pS[:, 0:128], in1=U[:, :], op=ALU.mult),
                 reads=[pS_t, c_t], writes=[pt_t])
            P.op("pe", lambda e, h=h, b=b, Cb_cur=Cb_cur: e.matmul(pO[:, 0:129], lhsT=qT[b][:, h, :], rhs=Cb_cur[:, :],
                                                                   start=True, stop=False),
                 reads=[qT_t[b], Cb_cur_t], writes=[pO_t])
            P.op("pe", lambda e, pt=pt, V1b=V1b: e.matmul(pO[:, 0:129], lhsT=pt[:, :], rhs=V1b[:, :],
                                                          start=False, stop=True),
                 reads=[pt_t, V1b_t], writes=[pO_t])
            o = 3 * h
            P.op("dve", lambda e, s=s, g=g, h=h, o=o: e.tensor_tensor(out=s[:, o:o + 1], in0=pO[:, 128:129],
                                                                      in1=g[:, 22 + h:23 + h], op=ALU.mult),
                 reads=[pO_t, g_t], writes=[s_t])
            P.op("dve", lambda e, s=s, o=o: e.tensor_scalar(out=s[:, o + 1:o + 2], in0=s[:, o:o + 1], scalar1=-1.0,
                                                            scalar2=1.0, op0=ALU.mult, op1=ALU.max),
                 reads=[s_t], writes=[s_t])
            P.op("dve", lambda e, s=s, o=o: e.tensor_tensor(out=s[:, o:o + 1], in0=s[:, o:o + 1],
                                                            in1=s[:, o + 1:o + 2], op=ALU.max),
                 reads=[s_t], writes=[s_t])
            P.op("dve", lambda e, s=s, o=o: e.reciprocal(out=s[:, o:o + 1], in_=s[:, o:o + 1]),
                 reads=[s_t], writes=[s_t])
            P.op("dve", lambda e, s=s, g=g, h=h, o=o: e.tensor_tensor(out=s[:, o + 1:o + 2], in0=g[:, 22 + h:23 + h],
                                                                      in1=s[:, o:o + 1], op=ALU.mult),
                 reads=[s_t, g_t], writes=[s_t])
            hb, hb_t = hh[h], hh_t[h]
            P.op("dve", lambda e, hb=hb, s=s, o=o: e.tensor_scalar(out=hb[:, :], in0=pO[:, 0:128],
                                                                   scalar1=s[:, o + 1:o + 2], scalar2=None, op0=ALU.mult),
                 reads=[pO_t, s_t], writes=[hb_t])
            P.op("act", lambda e, hb=hb, s=s, o=o: e.activation(out=sq[:, :], in_=hb[:, :], func=AF.Square,
                                                                accum_out=s[:, o + 2:o + 3]),
                 reads=[hb_t], writes=[sq_t, s_t])
            P.op("dve", lambda e, s=s, o=o: e.tensor_scalar(out=s[:, o + 2:o + 3], in0=s[:, o + 2:o + 3],
                                                            scalar1=1.0 / 128.0, scalar2=EPS, op0=ALU.mult, op1=ALU.add),
                 reads=[s_t], writes=[s_t])
            P.op("act", lambda e, s=s, o=o: e.activation(out=s[:, o + 2:o + 3], in_=s[:, o + 2:o + 3], func=AF.Sqrt),
                 reads=[s_t], writes=[s_t])
            P.op("dve", lambda e, s=s, o=o: e.reciprocal(out=s[:, o + 2:o + 3], in_=s[:, o + 2:o + 3]),
                 reads=[s_t], writes=[s_t])
            P.op("dve", lambda e, hb=hb, s=s, o=o, h=h: e.scalar_tensor_tensor(
                out=hb[:, :], in0=hb[:, :], scalar=s[:, o + 2:o + 3], in1=hn[:, h * 128:(h + 1) * 128],
                op0=ALU.mult, op1=ALU.mult),
                reads=[hb_t, s_t, c4_t], writes=[hb_t])
            P.op("dve", lambda e, hb=hb, h=h, b=b: e.tensor_tensor(out=yt[b][:, h * 128:(h + 1) * 128], in0=hb[:, :],
                                                                   in1=og[b][:, h * 128:(h + 1) * 128], op=ALU.mult),
                 reads=[hb_t, og_t[b]], writes=[yt_t[b]])
            P.op("pe", lambda e, h=h, b=b, V2b=V2b: e.matmul(pA[0:64, 0:129], lhsT=ktok[b][:, h * 64:(h + 1) * 64],
                                                             rhs=V2b[:, :], start=True, stop=True),
                 reads=[ktok_t[b], V2b_t], writes=[pA_t])
            P.op("dve", lambda e, h=h, s=s: e.scalar_tensor_tensor(
                out=C[h][:, :], in0=C[h][:, :], scalar=s[0:64, 6 + h:7 + h], in1=pA[0:64, 0:129],
                op0=ALU.mult, op1=ALU.add),
                reads=[C_t[h], s_t, pA_t], writes=[C_t[h]])
            P.op("act", lambda e, h=h, Cb_nxt=Cb_nxt: e.activation(out=Cb_nxt[:, :], in_=C[h][:, :], func=AF.Copy),
                 reads=[C_t[h]], writes=[Cb_nxt_t])
        P.dma("sp", yout[j * 128:(j + 1) * 128, :], yt[b][:, :], reads=[yt_t[b]], writes=[T("yo%d" % j)], final=True)
    return P.build()


def build_attn():
    P = Prog()
    NT = S // 128
    xin_d = P.dram_in("xin", [S, D], F32)
    gn = P.dram_in("gn", [128, 8], F32)
    w_d = P.dram_in("wqkv", [D, 768], F32)
    cs_d = P.dram_in("cs", [S, 128], F32)
    identd = P.dram_in("ident", [128, 128], BF16)
    identf_d = P.dram_in("identf", [128, 128], F32)
    oneh_d = P.dram_in("onehot", [32, S], BF16)
    tri_d = P.dram_in("tri", [128, 128], BF16)
    M1_d = P.dram_in("M1", [128, 1024], F32)
    A30_d = P.dram_in("A30", [128, 1024], F32)
    Bc_d = P.dram_in("Bc", [128, 1024], F32)
    oT = P.dram_out("oT", [256, S], BF16)
    qTs = P.dram_tmp("qTs", [4, 64, S], BF16)
    kTs = P.dram_tmp("kTs", [4, 64, S], BF16)
    vs = P.dram_tmp("vs", [S, 256], BF16)

    ident, ident_t = make_ident(P, identd)
    identf = P.sb("identf_sb", [128, 128], F32)
    tri = P.sb("tri_sb", [128, 128], BF16)
    M1 = P.sb("M1_sb", [128, 1024], F32)
    A30 = P.sb("A30_sb", [128, 1024], F32)
    Bc = P.sb("Bc_sb", [128, 1024], F32)
    cst_t = []
    for dst, src in ((identf, identf_d), (tri, tri_d), (M1, M1_d), (A30, A30_d), (Bc, Bc_d)):
        t = T("c")
        cst_t.append(t)
        P.dma("sp", dst[:, :], src[:, :], writes=[t])
    w, w_t = load_w_bf16(P, "w_sb", w_d, D, 768)

    KA = P.sb("KA", [128, S], BF16)
    QA = P.sb("QA", [128, S], BF16)
    VA = P.sb("VA", [128, NT, 128], BF16)
    qabs = P.sb("qabs", [64, S], BF16)
    KAoh_t = T("KAoh")
    for q4 in range(4):
        P.dma("sp", KA[64:96, q4 * 2048:(q4 + 1) * 2048], oneh_d[:, q4 * 2048:(q4 + 1) * 2048], writes=[KAoh_t])
    VA1_t = T("VA1")
    P.op("pool", lambda e: e.memset(VA[:, :, 64:128], 1.0), writes=[VA1_t])

    def dbl(name, shape, dt):
        return [P.sb("%s%d" % (name, i), shape, dt) for i in range(2)], [T("%s%d" % (name, i)) for i in range(2)]

    xin, xin_t = dbl("xin_sb", [128, D], F32)
    cs, cs_t = dbl("cs_sb", [128, 128], F32)
    xnT, xnT_t = dbl("xnT", [128, 8, 128], BF16)
    qk32, qk32_t = dbl("qk32", [128, 512], F32)
    rt, rt_t = dbl("rt", [128, 4, 64], F32)
    qkb, qkb_t = dbl("qkb", [128, 512], BF16)
    vb, vb_t = dbl("vb", [128, 256], BF16)
    qkT, qkT_t = dbl("qkT", [64, 8, 128], BF16)
    ksum, ksum_t = dbl("ksum", [64, 4], F32)
    kms = P.sb("kms", [64, 4, 32], F32)
    kms_t = T("kms")
    kmeanb = P.sb("kmeanb", [64, 4, 32], BF16)
    kmeanb_t = T("kmeanb")

    pb = [P.ps("pbT", [128, 8, 128], BF16)]
    pb_t = [T("pbT", True)]
    pqT = P.ps("pqT", [128, 8, 128], BF16); pqT_t = T("pqT", True)
    pqk = P.ps("pqk", [128, 512], F32); pqk_t = T("pqk", True)
    pv = P.ps("pv", [128, 512], F32); pv_t = T("pv", True)
    pS = [P.ps("pS%d" % i, [128, 512], F32) for i in range(2)]
    pS_t = [T("pS%d" % i, True) for i in range(2)]
    pO = [P.ps("pO%d" % i, [128, 512], F32) for i in range(2)]
    pO_t = [T("pO%d" % i, True) for i in range(2)]
    norm = NormCtx(P, gn, ident, ident_t, pb, pb_t)

    scr_t = []
    for j in range(NT):
        b = j % 2
        P.dma("sp", xin[b][:, :], xin_d[j * 128:(j + 1) * 128, :], writes=[xin_t[b]])
        P.dma("sp", cs[b][:, :], cs_d[j * 128:(j + 1) * 128, :], writes=[cs_t[b]])
        norm.run(xin[b][:, :], xin_t[b], lambda c, b=b: xnT[b][:, c, :], xnT_t[b])
        for c in range(8):
            P.op("pe", lambda e, c=c, b=b: e.matmul(pqk[:, :], lhsT=xnT[b][:, c, :], rhs=w[:, c, 0:512],
                                                    start=(c == 0), stop=(c == 7)),
                 reads=[w_t, xnT_t[b]], writes=[pqk_t], n=512)
        for c in range(8):
            P.op("pe", lambda e, c=c, b=b: e.matmul(pv[:, 0:256], lhsT=xnT[b][:, c, :], rhs=w[:, c, 512:768],
                                                    start=(c == 0), stop=(c == 7)),
                 reads=[w_t, xnT_t[b]], writes=[pv_t], n=256)
        P.op("act", lambda e, b=b: e.activation(out=qk32[b][:, 0:256], in_=pqk[:, 0:256], func=AF.Copy, scale=0.125),
             reads=[pqk_t], writes=[qk32_t[b]])
        P.op("act", lambda e, b=b: e.activation(out=qk32[b][:, 256:512], in_=pqk[:, 256:512], func=AF.Copy),
             reads=[pqk_t], writes=[qk32_t[b]])
        P.op("dve", lambda e, b=b: e.tensor_copy(out=vb[b][:, :], in_=pv[:, 0:256]), reads=[pv_t], writes=[vb_t[b]])
        tv = T("vs%d" % j)
        scr_t.append(tv)
        P.dma("sp", vs[j * 128:(j + 1) * 128, :], vb[b][:, :], reads=[vb_t[b]], writes=[tv])
        qv = qk32[b][:, :].rearrange("p (g d) -> p g d", d=64)
        x1, x2 = qv[:, :, 0:8], qv[:, :, 8:16]
        cosv = cs[b][:, 0:64].rearrange("p (g f) -> p g f", f=8)
        sinv = cs[b][:, 64:128].rearrange("p (g f) -> p g f", f=8)
        r = rt[b]
        rv = [r[:, i, :].rearrange("p (g f) -> p g f", f=8) for i in range(4)]
        for i, (a0, a1) in enumerate(((x1, cosv), (x2, sinv), (x2, cosv), (x1, sinv))):
            P.op("dve", lambda e, i=i, a0=a0, a1=a1, rv=rv: e.tensor_tensor(out=rv[i], in0=a0, in1=a1, op=ALU.mult),
                 reads=[qk32_t[b], cs_t[b]], writes=[rt_t[b]])
        P.op("dve", lambda e, x1=x1, rv=rv: e.tensor_tensor(out=x1, in0=rv[0], in1=rv[1], op=ALU.subtract),
             reads=[rt_t[b]], writes=[qk32_t[b]])
        P.op("dve", lambda e, x2=x2, rv=rv: e.tensor_tensor(out=x2, in0=rv[2], in1=rv[3], op=ALU.add),
             reads=[rt_t[b]], writes=[qk32_t[b]])
        P.op("act", lambda e, b=b: e.activation(out=qkb[b][:, :], in_=qk32[b][:, :], func=AF.Copy),
             reads=[qk32_t[b]], writes=[qkb_t[b]])
        for g in range(8):
            P.op("pe", lambda e, g=g, b=b: e.transpose(out=pqT[0:64, g, :], in_=qkb[b][:, g * 64:(g + 1) * 64],
                                                       identity=ident[:, :]),
                 reads=[qkb_t[b], ident_t], writes=[pqT_t])
        P.op("dve", lambda e, b=b: e.tensor_copy(out=qkT[b][:, :, :], in_=pqT[0:64, :, :]),
             reads=[pqT_t], writes=[qkT_t[b]])
        tq = T("qs%d" % j)
        tk = T("ks%d" % j)
        scr_t += [tq, tk]
        P.dma("sp", qTs[:, :, j * 128:(j + 1) * 128].rearrange("h d n -> d h n"), qkT[b][:, 0:4, :],
              reads=[qkT_t[b]], writes=[tq])
        P.dma("sp", kTs[:, :, j * 128:(j + 1) * 128].rearrange("h d n -> d h n"), qkT[b][:, 4:8, :],
              reads=[qkT_t[b]], writes=[tk])
        blk = j // 2
        if j % 2 == 0:
            P.op("dve", lambda e, b=b, blk=blk: e.tensor_reduce(out=kms[:, :, blk], in_=qkT[b][:, 4:8, :],
                                                                axis=AX.X, op=ALU.add),
                 reads=[qkT_t[b]], writes=[kms_t])
        else:
            P.op("dve", lambda e, b=b: e.tensor_reduce(out=ksum[b][:, :], in_=qkT[b][:, 4:8, :],
                                                       axis=AX.X, op=ALU.add),
                 reads=[qkT_t[b]], writes=[ksum_t[b]])
            P.op("dve", lambda e, b=b, blk=blk: e.tensor_tensor(out=kms[:, :, blk], in0=kms[:, :, blk],
                                                                in1=ksum[b][:, :], op=ALU.add),
                 reads=[ksum_t[b], kms_t], writes=[kms_t])
    P.op("act", lambda e: e.activation(out=kmeanb[:, :, :], in_=kms[:, :, :], func=AF.Copy, scale=1.0 / 256.0),
         reads=[kms_t], writes=[kmeanb_t])

    KA_t, QA_t, VA_t = T("KA"), T("QA"), T("VA")
    QAb_t = [T("QAb%d" % g) for g in range(16)]
    ab = P.sb("ab", [64, 2048], BF16); ab_t = T("ab")
    km4 = P.sb("km4", [64, 8], F32); km4_t = T("km4")
    kmaxb = P.sb("kmaxb", [64, 2], BF16); kmaxb_t = T("kmaxb")
    gm, gm_t = dbl("gm", [128, 32], F32)
    sel, sel_t = dbl("sel", [128, 32], F32)
    top8, top8_t = dbl("top8", [128, 8], F32)
    mq, mq_t = dbl("mq", [128, 1], F32)
    BT, BT_t = dbl("BT", [128, 4, 96], F32)
    for i in range(2):
        P.op("pool", lambda e, i=i: e.memset(BT[i][:, :, :], 0.0), writes=[BT_t[i]])
    Pb = [P.sb("Pb%d" % i, [128, 512], BF16) for i in range(3)]
    Pb_t = [T("Pb%d" % i) for i in range(3)]
    OS, OS_t = dbl("OS", [128, 512], F32)
    DN, DN_t = dbl("DN", [64, 512], F32)
    OTs, OTs_t = dbl("OTs", [64, 512], BF16)
    pG, pG_t = pqk, pqk_t
    pBT, pBT_t = pv, pv_t
    kS = 0
    kP = 0
    ka_l = [T("kal%d" % i) for i in range(4)]
    qa_l = [T("qal%d" % i) for i in range(4)]
    va_l = [T("val%d" % i) for i in range(4)]
    qabs_l = [T("qabs%d" % i) for i in range(4)]
    for h in range(4):
        for q4 in range(4):
            sl = slice(q4 * 2048, (q4 + 1) * 2048)
            P.dma("sp", KA[0:64, sl], kTs[h, :, sl], reads=scr_t, writes=[ka_l[q4]])
            P.dma("sp", QA[0:64, sl], qTs[h, :, sl], reads=scr_t, writes=[qa_l[q4]])
            P.dma("pool", VA[:, q4 * 16:(q4 + 1) * 16, 0:64],
                  vs[q4 * 2048:(q4 + 1) * 2048, h * 64:(h + 1) * 64].rearrange("(t p) c -> p t c", p=128),
                  reads=scr_t, writes=[va_l[q4]])
        for q4 in range(4):
            sl = slice(q4 * 2048, (q4 + 1) * 2048)
            P.op("act", lambda e, sl=sl: e.activation(out=qabs[:, sl], in_=QA[0:64, sl], func=AF.Abs),
                 reads=[qa_l[q4]], writes=[qabs_l[q4]])
            P.op("act", lambda e, sl=sl: e.activation(out=ab[:, :], in_=KA[0:64, sl], func=AF.Abs),
                 reads=[ka_l[q4]], writes=[ab_t])
            P.op("dve", lambda e, q4=q4: e.tensor_reduce(out=km4[:, q4:q4 + 1], in_=ab[:, :], axis=AX.X, op=ALU.max),
                 reads=[ab_t], writes=[km4_t])
        P.op("dve", lambda e: e.tensor_reduce(out=km4[:, 4:5], in_=km4[:, 0:4], axis=AX.X, op=ALU.max),
             reads=[km4_t], writes=[km4_t])
        P.op("dve", lambda e: e.tensor_copy(out=kmaxb[:, 0:1], in_=km4[:, 4:5]), reads=[km4_t], writes=[kmaxb_t])
        for g in range(16):
            bt, bt_t = BT[g % 2], BT_t[g % 2]
            for qi in range(4):
                qt = g * 4 + qi
                cur = qt // 2
                k2 = qt % 2
                csl = slice(qt * 128, (qt + 1) * 128)
                P.op("pe", lambda e, csl=csl, h=h: e.matmul(pG[:, 0:32], lhsT=QA[0:64, csl], rhs=kmeanb[:, h, :],
                                                            start=True, stop=True),
                     reads=[qa_l, kmeanb_t], writes=[pG_t])
                P.op("pe", lambda e, csl=csl: e.matmul(pG[:, 32:33], lhsT=qabs[:, csl], rhs=kmaxb[:, 0:1],
                                                       start=True, stop=True),
                     reads=[qabs_l, kmaxb_t], writes=[pG_t])
                P.op("dve", lambda e, k2=k2, cur=cur: e.tensor_tensor(out=gm[k2][:, :], in0=pG[:, 0:32],
                                                                      in1=M1[:, cur * 32:(cur + 1) * 32], op=ALU.add),
                     reads=[pG_t, cst_t], writes=[gm_t[k2]])
                P.op("dve", lambda e, k2=k2: e.tensor_copy(out=mq[k2][:, :], in_=pG[:, 32:33]),
                     reads=[pG_t], writes=[mq_t[k2]])
                P.op("dve", lambda e, k2=k2: e.max(out=top8[k2][:, :], in_=gm[k2][:, :]),
                     reads=[gm_t[k2]], writes=[top8_t[k2]])
                P.op("dve", lambda e, k2=k2: e.tensor_scalar(out=sel[k2][:, :], in0=gm[k2][:, :],
                                                             scalar1=top8[k2][:, 2:3], scalar2=1.0,
                                                             op0=ALU.is_ge, op1=ALU.subtract),
                     reads=[gm_t[k2], top8_t[k2]], writes=[sel_t[k2]])
                P.op("dve", lambda e, k2=k2, cur=cur: e.tensor_tensor(out=sel[k2][:, :], in0=sel[k2][:, :],
                                                                      in1=A30[:, cur * 32:(cur + 1) * 32], op=ALU.mult),
                     reads=[sel_t[k2], cst_t], writes=[sel_t[k2]])
                P.op("dve", lambda e, k2=k2, cur=cur: e.tensor_tensor(out=sel[k2][:, :], in0=sel[k2][:, :],
                                                                      in1=Bc[:, cur * 32:(cur + 1) * 32], op=ALU.add),
                     reads=[sel_t[k2], cst_t], writes=[sel_t[k2]])
                P.op("dve", lambda e, k2=k2, bt=bt, qi=qi: e.tensor_scalar(out=bt[:, qi, 64:96], in0=sel[k2][:, :],
                                                                           scalar1=mq[k2][:, 0:1], scalar2=None,
                                                                           op0=ALU.subtract),
                     reads=[sel_t[k2], mq_t[k2]], writes=[bt_t])
                P.op("pe", lambda e, bt=bt, qi=qi: e.transpose(out=pBT[0:96, qi * 128:(qi + 1) * 128], in_=bt[:, qi, :],
                                                               identity=identf[:, :]),
                     reads=[bt_t, cst_t], writes=[pBT_t])
            P.op("act", lambda e, g=g: e.activation(out=QA[64:96, g * 512:(g + 1) * 512], in_=pBT[64:96, :], func=AF.Copy),
                 reads=[pBT_t], writes=[QAb_t[g]])
        for g in range(16):
            po, po_t = pO[g % 2], pO_t[g % 2]
            nk = 4 * g + 4
            for kt in range(nk):
                i = kt - 4 * g
                c0 = max(i, 0) * 128
                ps, ps_t = pS[kS % 2], pS_t[kS % 2]
                kS += 1
                pbuf, pbuf_t = Pb[kP % 3], Pb_t[kP % 3]
                kP += 1
                P.op("pe", lambda e, ps=ps, kt=kt, g=g, c0=c0, i=i: e.matmul(
                    ps[:, c0:512], lhsT=KA[0:96, kt * 128:(kt + 1) * 128], rhs=QA[0:96, g * 512 + c0:(g + 1) * 512],
                    start=True, stop=(i < 0)),
                    reads=[ka_l, KAoh_t, qa_l, QAb_t[g]], writes=[ps_t], n=512 - c0)
                if i >= 0:
                    P.op("pe", lambda e, ps=ps, c0=c0: e.matmul(ps[:, c0:c0 + 128], lhsT=ident[:, :], rhs=tri[:, :],
                                                                start=False, stop=True),
                         reads=[ident_t, cst_t], writes=[ps_t])
                P.op("act", lambda e, ps=ps, pbuf=pbuf, c0=c0: e.activation(out=pbuf[:, c0:512], in_=ps[:, c0:512],
                                                                            func=AF.Exp),
                     reads=[ps_t], writes=[pbuf_t], n=512 - c0)
                P.op("pe", lambda e, po=po, pbuf=pbuf, kt=kt, c0=c0, nk=nk: e.matmul(
                    po[:, c0:512], lhsT=VA[:, kt, :], rhs=pbuf[:, c0:512], start=(kt == 0), stop=(kt == nk - 1)),
                    reads=[va_l, VA1_t, pbuf_t], writes=[po_t], n=512 - c0)
            o = g % 2
            P.op("act", lambda e, o=o, po=po: e.activation(out=OS[o][:, :], in_=po[:, :], func=AF.Copy),
                 reads=[po_t], writes=[OS_t[o]])
            P.dma("sp", DN[o][:, :], OS[o][64:128, :], reads=[OS_t[o]], writes=[DN_t[o]])
            P.op("dve", lambda e, o=o: e.reciprocal(out=DN[o][:, :], in_=DN[o][:, :]), reads=[DN_t[o]], writes=[DN_t[o]])
            P.op("dve", lambda e, o=o: e.tensor_tensor(out=OTs[o][:, :], in0=OS[o][0:64, :], in1=DN[o][:, :], op=ALU.mult),
                 reads=[OS_t[o], DN_t[o]], writes=[OTs_t[o]])
            P.dma("sp", oT[h * 64:(h + 1) * 64, g * 512:(g + 1) * 512], OTs[o][:, :], reads=[OTs_t[o]],
                  writes=[T("oT")], final=True)
    return P.build()


_PROGS = {}


def _prog(name):
    if name not in _PROGS:
        _PROGS[name] = {"attn": build_attn, "mlstm": build_mlstm,
                        "pf0": lambda: build_projffn(False), "pf1": lambda: build_projffn(True)}[name]()
    return _PROGS[name]


def _run(name, maps):
    res = run_bass_kernel_spmd(_prog(name), maps, core_ids=list(range(NCORE)))
    return res.results


def _gain_layout(g):
    return np.ascontiguousarray(np.asarray(g, np.float32).reshape(8, 128).T)


def _consts():
    bf = ml_dtypes.bfloat16
    c = {}
    c["ident"] = np.eye(128, dtype=np.float32).astype(bf)
    c["identf"] = np.eye(128, dtype=np.float32)
    pos = np.arange(S, dtype=np.float32)
    inv = (np.float32(500000.0) ** (-np.arange(0, 16, 2, dtype=np.float32) / np.float32(16))).astype(np.float32)
    ang = (pos[:, None] * inv[None, :]).astype(np.float32)
    cos = np.cos(ang).astype(np.float32)
    sin = np.sin(ang).astype(np.float32)
    c["cs"] = np.ascontiguousarray(np.concatenate([np.tile(cos, (1, 8)), np.tile(sin, (1, 8))], axis=1))
    blk = np.arange(S) // 256
    c["onehot"] = (blk[None, :] == np.arange(32)[:, None]).astype(np.float32).astype(bf)
    kk = np.arange(128)
    c["tri"] = np.where(kk[:, None] > kk[None, :], NEG, 0.0).astype(np.float32).astype(bf)
    cur = np.arange(32)[:, None]
    n = np.arange(32)[None, :]
    m1 = np.where(n < cur, 0.0, NEG).astype(np.float32).reshape(1, 1024)
    a30 = np.where(n < cur, -NEG, 0.0).astype(np.float32).reshape(1, 1024)
    bc = np.where(n <= cur, 0.0, NEG).astype(np.float32).reshape(1, 1024)
    c["M1"] = np.ascontiguousarray(np.tile(m1, (128, 1)))
    c["A30"] = np.ascontiguousarray(np.tile(a30, (128, 1)))
    c["Bc"] = np.ascontiguousarray(np.tile(bc, (128, 1)))
    c["U"] = np.triu(np.ones((128, 128), np.float32))
    c["ones"] = np.ones((128, 128), np.float32)
    return c


def kernel(x, attn_norm, attn_w_qkv, attn_w_o, mlstm_norm, mlstm_w_in, mlstm_b_gates,
           mlstm_head_norm, mlstm_w_out, ffn_norm, ffn_w_gate_up, ffn_w_down, final_norm):
    f32 = np.float32
    x = np.asarray(x, f32)
    c = _consts()
    wqkv = np.asarray(attn_w_qkv, f32)[0]
    maps = []
    for core in range(NCORE):
        b, hg = core // 4, core % 4
        cols = np.concatenate([np.arange(hg * 256, (hg + 1) * 256) + off for off in (0, 1024, 2048)])
        maps.append({"xin": x[b], "gn": _gain_layout(attn_norm[0]), "wqkv": np.ascontiguousarray(wqkv[:, cols]),
                     "cs": c["cs"], "ident": c["ident"], "identf": c["identf"], "onehot": c["onehot"],
                     "tri": c["tri"], "M1": c["M1"], "A30": c["A30"], "Bc": c["Bc"]})
    ra = _run("attn", maps)
    oT = [np.concatenate([ra[b * 4 + hg]["oT"] for hg in range(4)], axis=0) for b in range(B)]
    maps = []
    for core in range(NCORE):
        b, q = core // 4, core % 4
        sl = slice(q * 2048, (q + 1) * 2048)
        maps.append({"hin": x[b, sl], "aT": np.ascontiguousarray(oT[b][:, sl]), "wp": np.asarray(attn_w_o, f32)[0],
                     "gn": _gain_layout(ffn_norm[0]), "wgu": np.asarray(ffn_w_gate_up, f32)[0],
                     "wd": np.asarray(ffn_w_down, f32)[0], "ident": c["ident"]})
    rb = _run("pf0", maps)
    h1 = [np.concatenate([rb[b * 4 + q]["hout"] for q in range(4)], axis=0) for b in range(B)]
    win = np.asarray(mlstm_w_in, f32)[0]
    bgv = np.asarray(mlstm_b_gates, f32)[0]
    hnv = np.asarray(mlstm_head_norm, f32)[0]
    maps = []
    for core in range(NCORE):
        b, hp = core // 4, core % 4
        h0 = 2 * hp
        wq = win[:, h0 * 64:(h0 + 2) * 64]
        wk = win[:, 512 + h0 * 64:512 + (h0 + 2) * 64]
        wv = win[:, 1024 + h0 * 128:1024 + (h0 + 2) * 128]
        wo = win[:, 2048 + h0 * 128:2048 + (h0 + 2) * 128]
        gi = win[:, 3072 + h0:3072 + h0 + 2]
        gf = win[:, 3080 + h0:3080 + h0 + 2]
        wtok = np.ascontiguousarray(np.concatenate([wk, wv, wo, gi, gf], axis=1))
        bg4 = np.concatenate([bgv[h0:h0 + 2], bgv[8 + h0:8 + h0 + 2]])
        maps.append({"hin": h1[b], "gn": _gain_layout(mlstm_norm[0]), "wq": np.ascontiguousarray(wq),
                     "wk": np.ascontiguousarray(wk), "wtok": wtok,
                     "bg": np.ascontiguousarray(np.tile(bg4[None, :], (128, 1))),
                     "hn": np.ascontiguousarray(np.tile(hnv[None, h0 * 128:(h0 + 2) * 128], (128, 1))),
                     "ident": c["ident"], "U": c["U"], "ones": c["ones"]})
    rc = _run("mlstm", maps)
    yT = [np.ascontiguousarray(np.concatenate([rc[b * 4 + hp]["y"] for hp in range(4)], axis=1).T)
          for b in range(B)]
    maps = []
    for core in range(NCORE):
        b, q = core // 4, core % 4
        sl = slice(q * 2048, (q + 1) * 2048)
        maps.append({"hin": np.ascontiguousarray(h1[b][sl]), "aT": np.ascontiguousarray(yT[b][:, sl]),
                     "wp": np.asarray(mlstm_w_out, f32)[0], "gn": _gain_layout(ffn_norm[1]),
                     "wgu": np.asarray(ffn_w_gate_up, f32)[1], "wd": np.asarray(ffn_w_down, f32)[1],
                     "ident": c["ident"], "fnw": np.asarray(final_norm, f32)})
    rd = _run("pf1", maps)
    out = np.stack([np.concatenate([rd[b * 4 + q]["hout"] for q in range(4)], axis=0) for b in range(B)])
    return out.astype(f32)
```

```python
import math
from contextlib import ExitStack

import numpy as np
import ml_dtypes

import concourse.bass as bass
import concourse.mybir as mybir
from concourse.bass_utils import run_bass_kernel_spmd

F32 = mybir.dt.float32
BF16 = mybir.dt.bfloat16
AF = mybir.ActivationFunctionType
ALU = mybir.AluOpType
AX = mybir.AxisListType

D = 1024
B = 2
S = 8192
NCORE = 8
DFF = 2816
EPS = 1e-6
NEG = -30000.0


class T:
    __slots__ = ("name", "w", "r", "psum")

    def __init__(self, name, psum=False):
        self.name = name
        self.w = None
        self.r = {}
        self.psum = psum


def _flat(x):
    out = []
    for a in x:
        if isinstance(a, (list, tuple)):
            out.extend(_flat(a))
        elif a is not None:
            out.append(a)
    return out


class Prog:
    ENGS = ("pe", "act", "dve", "pool", "sp")
    NRING = 8

    def __init__(self):
        self.nc = bass.Bass("TRN2", target_bir_lowering=False)
        self.ins = {e: [] for e in self.ENGS}
        self.ndma = {e: 0 for e in self.ENGS}
        self.dma_idx = {e: [] for e in self.ENGS}
        self.stack = ExitStack()
        self.final = []

    def sb(self, name, shape, dt):
        return self.stack.enter_context(self.nc.sbuf_tensor(name, list(shape), dt))

    def ps(self, name, shape, dt):
        return self.stack.enter_context(self.nc.psum_tensor(name, list(shape), dt))

    def dram_in(self, name, shape, dt):
        return self.nc.dram_tensor(name, list(shape), dt, kind="ExternalInput").ap()

    def dram_out(self, name, shape, dt):
        return self.nc.dram_tensor(name, list(shape), dt, kind="ExternalOutput").ap()

    def dram_tmp(self, name, shape, dt):
        return self.nc.dram_tensor(name, list(shape), dt).ap()

    COST0 = {"pe": 40.0, "act": 220.0, "dve": 120.0, "pool": 250.0, "sp": 60.0}
    COSTN = {"pe": 0.42, "act": 1.05, "dve": 0.8, "pool": 1.6, "sp": 0.0}

    def _emit(self, eng, fn, reads, writes, dma, n=128):
        reads, writes = _flat(reads), _flat(writes)
        lst = self.ins[eng]
        idx = len(lst)
        raw, other = set(), set()
        for t in reads:
            if t.w is not None:
                raw.add(t.w)
            if t.psum:
                for e2, i2 in t.r.items():
                    if e2 != eng:
                        other.add((e2, i2))
        for t in writes:
            if t.w is not None:
                other.add(t.w)
            for e2, i2 in t.r.items():
                other.add((e2, i2))
        deps, order = set(), set()

        def same_compute(d):
            return d[0] == eng and not dma and self.ins[eng][d[1]]["dma"] is None

        for d in raw:
            if same_compute(d) and eng == "pe":
                order.add(d[1])
            else:
                deps.add(d)
        for d in other:
            if same_compute(d):
                order.add(d[1])
            else:
                deps.add(d)
        deps.discard((eng, idx))
        cost = self.COST0[eng] + self.COSTN[eng] * n
        rec = dict(fn=fn, deps=deps, order=order, dma=None, cost=cost)
        if dma:
            rec["dma"] = -1
            rec["cost"] = 2000.0 + n * 0.01
        lst.append(rec)
        for t in reads:
            t.r[eng] = idx
        for t in writes:
            t.w = (eng, idx)
            t.r = {}
        return idx

    def op(self, eng, fn, reads=(), writes=(), n=128):
        return self._emit(eng, fn, reads, writes, False, n)

    def dma(self, eng, out, in_, reads=(), writes=(), final=False, n=65536):
        i = self._emit(eng, lambda e: e.dma_start(out=out, in_=in_), reads, writes, True, n)
        if final:
            self.final.append((eng, i))
        return i

    WINDOW = 48

    def schedule(self):
        ENGS = self.ENGS
        ins = self.ins
        n_tot = sum(len(ins[e]) for e in ENGS)
        done = {e: [None] * len(ins[e]) for e in ENGS}
        pend = {e: list(range(len(ins[e]))) for e in ENGS}
        free = {e: 0.0 for e in ENGS}
        neword = {e: [] for e in ENGS}
        count = 0
        while count < n_tot:
            best = None
            for e in ENGS:
                p = pend[e]
                lim = min(len(p), self.WINDOW)
                for k in range(lim):
                    i = p[k]
                    rec = ins[e][i]
                    ok = True
                    rt = free[e]
                    for o in rec["order"]:
                        if done[e][o] is None:
                            ok = False
                            break
                    if not ok:
                        continue
                    for (e2, i2) in rec["deps"]:
                        dt = done[e2][i2]
                        if dt is None:
                            ok = False
                            break
                        if dt > rt:
                            rt = dt
                    if not ok:
                        continue
                    key = (rt, k)
                    if best is None or key < best[0]:
                        best = (key, e, k, i, rt)
                    if rt <= free[e]:
                        break
            assert best is not None, "scheduler deadlock"
            _, e, k, i, rt = best
            rec = ins[e][i]
            if rec["dma"] is not None:
                free[e] = rt + 60.0
                done[e][i] = rt + rec["cost"]
            else:
                free[e] = rt + rec["cost"]
                done[e][i] = rt + rec["cost"] + 60.0
            pend[e].pop(k)
            neword[e].append(i)
            count += 1
        self.sim_time = max(max([x for x in done[e] if x is not None] + [0.0]) for e in ENGS)
        pos = {e: {old: new for new, old in enumerate(neword[e])} for e in ENGS}
        for e in ENGS:
            newl = []
            for old in neword[e]:
                rec = ins[e][old]
                rec["deps"] = {(e2, pos[e2][i2]) for (e2, i2) in rec["deps"]}
                newl.append(rec)
            ins[e] = newl
        self.final = [(e, pos[e][i]) for (e, i) in self.final]
        for e in ENGS:
            k = 0
            idxs = []
            for i, rec in enumerate(ins[e]):
                if rec["dma"] is not None:
                    rec["dma"] = k
                    if k >= self.NRING:
                        rec["deps"].add((e, idxs[k - self.NRING]))
                    idxs.append(i)
                    k += 1
            self.ndma[e] = k

    def build(self):
        nc = self.nc
        st = self.stack
        self.schedule()
        final_deps = set(self.final)
        needed = {e: set() for e in self.ENGS}
        for e in self.ENGS:
            for rec in self.ins[e]:
                for (e2, i2) in rec["deps"]:
                    needed[e2].add(i2)
        cnt_sem = {e: st.enter_context(nc.semaphore("c_" + e)) for e in self.ENGS}
        ring = {e: [st.enter_context(nc.semaphore("r_%s%d" % (e, i))) for i in range(self.NRING)]
                for e in self.ENGS if self.ndma[e] > 0}
        sig = {e: {} for e in self.ENGS}
        for e in self.ENGS:
            c = 0
            for i, rec in enumerate(self.ins[e]):
                if rec["dma"] is not None:
                    k = rec["dma"]
                    sig[e][i] = (ring[e][k % self.NRING], 16 * (k // self.NRING + 1))
                elif i in needed[e]:
                    c += 1
                    sig[e][i] = (cnt_sem[e], c)
        self.stats = {e: (len(self.ins[e]), self.ndma[e]) for e in self.ENGS}
        block = st.enter_context(nc.Block())
        handles = {"pe": block.tensor, "act": block.scalar, "dve": block.vector,
                   "pool": block.gpsimd, "sp": block.sync}

        def make(e):
            def body(eng):
                waited = {}
                for i, rec in enumerate(self.ins[e]):
                    for d in sorted(rec["deps"]):
                        sem, val = sig[d[0]][d[1]]
                        key = id(sem)
                        if waited.get(key, 0) >= val:
                            continue
                        waited[key] = val
                        eng.wait_ge(sem, val)
                    inst = rec["fn"](eng)
                    if i in sig[e]:
                        sem, val = sig[e][i]
                        inst.then_inc(sem, 16 if rec["dma"] is not None else 1)
                if e == "sp":
                    for d in sorted(final_deps):
                        sem, val = sig[d[0]][d[1]]
                        if waited.get(id(sem), 0) >= val:
                            continue
                        waited[id(sem)] = val
                        eng.wait_ge(sem, val)
            return body

        for e in self.ENGS:
            handles[e](make(e))
        st.close()
        return nc


def load_w_bf16(P, name, w_dram, kdim, ncols, col0=0, tile=None, tr=None):
    kc = kdim // 128
    if tile is None:
        tile = P.sb(name, [128, kc, ncols], BF16)
    src = w_dram[:, col0:col0 + ncols].rearrange("(c p) n -> p c n", p=128)
    step = max(1, kc // 4)
    trs = []
    for c0 in range(0, kc, step):
        c1 = min(kc, c0 + step)
        tr = T(name + str(c0))
        trs.append(tr)
        P.dma("pool", tile[:, c0:c1, :], src[:, c0:c1, :], writes=[tr])
    return tile, trs


class NormCtx:
    def __init__(self, P, gain_dram, ident, ident_t, pst, pst_t):
        self.P = P
        self.ident, self.ident_t = ident, ident_t
        self.pst, self.pst_t = pst, pst_t
        self.g = P.sb("ng_" + gain_dram.tensor.name, [128, 8], F32)
        self.g_t = T("ng")
        P.dma("sp", self.g[:, :], gain_dram[:, :], writes=[self.g_t])
        self.junk = P.sb("nj_" + gain_dram.tensor.name, [128, 1024], BF16)
        self.junk_t = T("nj")
        self.ss = [P.sb("nss%d_" % i + gain_dram.tensor.name, [128, 2], F32) for i in range(2)]
        self.ss_t = [T("nss%d" % i) for i in range(2)]
        self.xs = [P.sb("nxs%d_" % i + gain_dram.tensor.name, [128, 1024], BF16) for i in range(2)]
        self.xs_t = [T("nxs%d" % i) for i in range(2)]
        self.k = 0
        self.dst_full = None
        self.lnexp = False

    def run(self, h_ap, h_t, dst_fn, dst_t):
        P = self.P
        k = self.k
        self.k += 1
        ss, ss_t = self.ss[k % 2], self.ss_t[k % 2]
        xs, xs_t = self.xs[k % 2], self.xs_t[k % 2]
        pst, pst_t = self.pst[k % len(self.pst)], self.pst_t[k % len(self.pst)]
        junk, junk_t = self.junk, self.junk_t
        P.op("act", lambda e: e.activation(out=junk[:, :], in_=h_ap, func=AF.Square,
                                           accum_out=ss[:, 0:1]),
             reads=[h_t], writes=[junk_t, ss_t], n=1024)
        P.op("dve", lambda e: e.tensor_scalar(out=ss[:, 1:2], in0=ss[:, 0:1], scalar1=1.0 / D,
                                              scalar2=EPS, op0=ALU.mult, op1=ALU.add),
             reads=[ss_t], writes=[ss_t])
        if self.lnexp:
            P.op("act", lambda e: e.activation(out=ss[:, 1:2], in_=ss[:, 1:2], func=AF.Ln),
                 reads=[ss_t], writes=[ss_t])
            P.op("act", lambda e: e.activation(out=ss[:, 1:2], in_=ss[:, 1:2], func=AF.Exp, scale=-0.5),
                 reads=[ss_t], writes=[ss_t])
        else:
            P.op("act", lambda e: e.activation(out=ss[:, 1:2], in_=ss[:, 1:2], func=AF.Sqrt),
                 reads=[ss_t], writes=[ss_t])
            P.op("dve", lambda e: e.reciprocal(out=ss[:, 1:2], in_=ss[:, 1:2]),
                 reads=[ss_t], writes=[ss_t])
        P.op("dve", lambda e: e.tensor_scalar(out=xs[:, :], in0=h_ap, scalar1=ss[:, 1:2],
                                              scalar2=None, op0=ALU.mult),
             reads=[h_t, ss_t], writes=[xs_t], n=1024)
        for c in range(8):
            P.op("pe", lambda e, c=c: e.transpose(out=pst[:, c, :], in_=xs[:, c * 128:(c + 1) * 128],
                                                  identity=self.ident[:, :]),
                 reads=[xs_t, self.ident_t], writes=[pst_t])
        g = self.g
        if self.dst_full is not None:
            dfull = self.dst_full(k)
            P.op("dve", lambda e: e.tensor_tensor(out=dfull, in0=pst[:, :, :],
                                                  in1=g[:, :].unsqueeze(2).to_broadcast([128, 8, 128]),
                                                  op=ALU.mult),
                 reads=[pst_t, self.g_t], writes=[dst_t], n=1024)
        else:
            for c in range(8):
                P.op("act", lambda e, c=c: e.activation(out=dst_fn(c), in_=pst[:, c, :], func=AF.Copy,
                                                        scale=g[:, c:c + 1]),
                     reads=[pst_t, self.g_t], writes=[dst_t])


def make_ident(P, ident_dram):
    ident = P.sb("ident_sb", [128, 128], BF16)
    ident_t = T("ident")
    P.dma("sp", ident[:, :], ident_dram[:, :], writes=[ident_t])
    return ident, ident_t


FFN_PARTS = [(0, 5), (5, 5), (10, 4), (14, 4), (18, 4)]


def build_projffn(final):
    P = Prog()
    NT = 16
    hin = P.dram_in("hin", [2048, D], F32)
    aT = P.dram_in("aT", [D, 2048], BF16)
    wp = P.dram_in("wp", [D, D], F32)
    gn = P.dram_in("gn", [128, 8], F32)
    wgu = P.dram_in("wgu", [D, 2 * DFF], F32)
    wd = P.dram_in("wd", [DFF, D], F32)
    identd = P.dram_in("ident", [128, 128], BF16)
    if final:
        fn = P.dram_in("fnw", [D], F32)
    hout = P.dram_out("hout", [2048, D], F32)

    ident, ident_t = make_ident(P, identd)
    h = P.sb("h", [128, NT, D], F32)
    h_t = [T("h%d" % i) for i in range(NT)]
    xnT = P.sb("xnT", [128, 8, 2048], BF16)
    xn_t = [T("xn%d" % i) for i in range(NT)]
    wps, wps_t = load_w_bf16(P, "wps", wp, D, D)
    at = [P.sb("at%d" % i, [128, 8, 128], BF16) for i in range(2)]
    at_t = [T("at%d" % i) for i in range(2)]
    pf = [P.ps("pf%d" % i, [128, 512], F32) for i in range(6)]
    pf_t = [T("pf%d" % i, True) for i in range(6)]
    pb = [P.ps("pb%d" % i, [128, 8, 128], BF16) for i in range(2)]
    pb_t = [T("pb%d" % i, True) for i in range(2)]
    norm = NormCtx(P, gn, ident, ident_t, pb, pb_t)
    norm.dst_full = lambda k: xnT[:, :, k * 128:(k + 1) * 128]
    pfk = [0]

    def next_pf():
        i = pfk[0] % 6
        pfk[0] += 1
        return pf[i], pf_t[i]

    for t in range(NT):
        P.dma("sp", h[:, t, :], hin[t * 128:(t + 1) * 128, :], writes=[h_t[t]])
        a, a_t = at[t % 2], at_t[t % 2]
        P.dma("sp", a[:, :, :], aT[:, t * 128:(t + 1) * 128].rearrange("(c p) n -> p c n", p=128),
              writes=[a_t])
        for nh in range(2):
            ps, ps_t = next_pf()
            for c in range(8):
                P.op("pe", lambda e, c=c, ps=ps, a=a, nh=nh: e.matmul(
                    ps[:, :], lhsT=a[:, c, :], rhs=wps[:, c, nh * 512:(nh + 1) * 512],
                    start=(c == 0), stop=(c == 7)),
                    reads=[a_t, wps_t], writes=[ps_t], n=512)
            P.op("dve", lambda e, ps=ps, t=t, nh=nh: e.tensor_tensor(
                out=h[:, t, nh * 512:(nh + 1) * 512], in0=h[:, t, nh * 512:(nh + 1) * 512],
                in1=ps[:, :], op=ALU.add),
                reads=[ps_t, h_t[t]], writes=[h_t[t]], n=512)
        norm.run(h[:, t, :], h_t[t], lambda c, t=t: xnT[:, c, t * 128:(t + 1) * 128], xn_t[t])

    wg_b = [P.sb("wg%d" % i, [128, 8, 640], BF16) for i in range(2)]
    wu_b = [P.sb("wu%d" % i, [128, 8, 640], BF16) for i in range(2)]
    wd_b = [P.sb("wd%d" % i, [128, 5, 1024], BF16) for i in range(2)]
    wg_t = [[T("wg%d_%d" % (i, q)) for q in range(4)] for i in range(2)]
    wu_t = [[T("wu%d_%d" % (i, q)) for q in range(4)] for i in range(2)]
    wd_t = [[T("wd%d_%d" % (i, q)) for q in range(3)] for i in range(2)]
    sg = [P.sb("sg%d" % i, [128, 512], F32) for i in range(2)]
    sg_t = [T("sg%d" % i) for i in range(2)]
    aF = [P.sb("aF%d" % i, [128, 5, 512], BF16) for i in range(2)]
    aF_t = [T("aF%d" % i) for i in range(2)]
    kk = 0
    for pi, (c0, ncn) in enumerate(FFN_PARTS):
        b = pi % 2
        ncols = ncn * 128
        srcg = wgu[:, c0 * 128:c0 * 128 + ncols].rearrange("(c p) n -> p c n", p=128)
        srcu = wgu[:, DFF + c0 * 128:DFF + c0 * 128 + ncols].rearrange("(c p) n -> p c n", p=128)
        for q in range(4):
            P.dma("pool", wg_b[b][:, 2 * q:2 * q + 2, 0:ncols], srcg[:, 2 * q:2 * q + 2, :], writes=[wg_t[b][q]])
            P.dma("pool", wu_b[b][:, 2 * q:2 * q + 2, 0:ncols], srcu[:, 2 * q:2 * q + 2, :], writes=[wu_t[b][q]])
        srcd = wd[c0 * 128:(c0 + ncn) * 128, :].rearrange("(c p) n -> p c n", p=128)
        for q in range(0, ncn, 2):
            q1 = min(ncn, q + 2)
            P.dma("pool", wd_b[b][:, q:q1, :], srcd[:, q:q1, :], writes=[wd_t[b][q // 2]])
        for tg in range(4):
            af, af_t = aF[tg % 2], aF_t[tg % 2]
            for j in range(ncn):
                psg, psg_t = next_pf()
                psu, psu_t = next_pf()
                for c in range(8):
                    P.op("pe", lambda e, c=c, j=j, psg=psg, b=b, tg=tg: e.matmul(
                        psg[:, :], lhsT=wg_b[b][:, c, j * 128:(j + 1) * 128],
                        rhs=xnT[:, c, tg * 512:(tg + 1) * 512], start=(c == 0), stop=(c == 7)),
                        reads=[wg_t[b]] + xn_t[tg * 4:tg * 4 + 4], writes=[psg_t], n=512)
                for c in range(8):
                    P.op("pe", lambda e, c=c, j=j, psu=psu, b=b, tg=tg: e.matmul(
                        psu[:, :], lhsT=wu_b[b][:, c, j * 128:(j + 1) * 128],
                        rhs=xnT[:, c, tg * 512:(tg + 1) * 512], start=(c == 0), stop=(c == 7)),
                        reads=[wu_t[b]] + xn_t[tg * 4:tg * 4 + 4], writes=[psu_t], n=512)
                s, s_t = sg[kk % 2], sg_t[kk % 2]
                kk += 1
                P.op("act", lambda e, s=s, psg=psg: e.activation(out=s[:, :], in_=psg[:, :], func=AF.Silu),
                     reads=[psg_t], writes=[s_t], n=512)
                P.op("dve", lambda e, s=s, psu=psu, af=af, j=j: e.tensor_tensor(
                    out=af[:, j, :], in0=s[:, :], in1=psu[:, :], op=ALU.mult),
                    reads=[s_t, psu_t], writes=[af_t], n=512)
            for tt in range(4):
                t = tg * 4 + tt
                for nh in range(2):
                    ps, ps_t = next_pf()
                    for j in range(ncn):
                        P.op("pe", lambda e, j=j, ps=ps, af=af, tt=tt, nh=nh, b=b: e.matmul(
                            ps[:, :], lhsT=af[:, j, tt * 128:(tt + 1) * 128],
                            rhs=wd_b[b][:, j, nh * 512:(nh + 1) * 512],
                            start=(j == 0), stop=(j == ncn - 1)),
                            reads=[af_t, wd_t[b]], writes=[ps_t], n=512)
                    P.op("dve", lambda e, ps=ps, t=t, nh=nh: e.tensor_tensor(
                        out=h[:, t, nh * 512:(nh + 1) * 512], in0=h[:, t, nh * 512:(nh + 1) * 512],
                        in1=ps[:, :], op=ALU.add),
                        reads=[ps_t, h_t[t]], writes=[h_t[t]], n=512)

    if final:
        fw = P.sb("fw", [128, D], F32)
        fw_t = T("fw")
        P.dma("sp", fw[:, :], fn.partition_broadcast(128), writes=[fw_t])
        ss = P.sb("fss", [128, 2 * NT], F32)
        ss_t = T("fss")
        junk = norm.junk
        for t in range(NT):
            P.op("act", lambda e, t=t: e.activation(out=junk[:, :], in_=h[:, t, :], func=AF.Square,
                                                    accum_out=ss[:, 2 * t:2 * t + 1]),
                 reads=[h_t[t]], writes=[norm.junk_t, ss_t])
            P.op("dve", lambda e, t=t: e.tensor_scalar(out=ss[:, 2 * t + 1:2 * t + 2], in0=ss[:, 2 * t:2 * t + 1],
                                                       scalar1=1.0 / D, scalar2=EPS, op0=ALU.mult, op1=ALU.add),
                 reads=[ss_t], writes=[ss_t])
            P.op("act", lambda e, t=t: e.activation(out=ss[:, 2 * t + 1:2 * t + 2],
                                                    in_=ss[:, 2 * t + 1:2 * t + 2], func=AF.Sqrt),
                 reads=[ss_t], writes=[ss_t])
            P.op("dve", lambda e, t=t: e.reciprocal(out=ss[:, 2 * t + 1:2 * t + 2],
                                                    in_=ss[:, 2 * t + 1:2 * t + 2]),
                 reads=[ss_t], writes=[ss_t])
            P.op("dve", lambda e, t=t: e.scalar_tensor_tensor(
                out=h[:, t, :], in0=h[:, t, :], scalar=ss[:, 2 * t + 1:2 * t + 2], in1=fw[:, :],
                op0=ALU.mult, op1=ALU.mult),
                reads=[h_t[t], ss_t, fw_t], writes=[h_t[t]])
    for t in range(NT):
        P.dma("sp", hout[t * 128:(t + 1) * 128, :], h[:, t, :], reads=[h_t[t]], writes=[T("o%d" % t)], final=True)
    return P.build()


def build_mlstm():
    P = Prog()
    NCH = S // 128
    hin = P.dram_in("hin", [S, D], F32)
    gn = P.dram_in("gn", [128, 8], F32)
    wq_d = P.dram_in("wq", [D, 128], F32)
    wk_d = P.dram_in("wk", [D, 128], F32)
    wt_d = P.dram_in("wtok", [D, 644], F32)
    bg_d = P.dram_in("bg", [128, 4], F32)
    hn_d = P.dram_in("hn", [128, 256], F32)
    identd = P.dram_in("ident", [128, 128], BF16)
    U_d = P.dram_in("U", [128, 128], F32)
    ones_d = P.dram_in("ones", [128, 128], F32)
    yout = P.dram_out("y", [S, 256], BF16)

    ident, ident_t = make_ident(P, identd)
    U = P.sb("U_sb", [128, 128], F32)
    ONES = P.sb("ones_sb", [128, 128], F32)
    bg = P.sb("bg_sb", [128, 4], F32)
    hn = P.sb("hn_sb", [128, 256], F32)
    c_t = T("consts")
    P.dma("sp", U[:, :], U_d[:, :], writes=[c_t])
    c2_t = T("consts2")
    P.dma("sp", ONES[:, :], ones_d[:, :], writes=[c2_t])
    c3_t = T("consts3")
    P.dma("sp", bg[:, :], bg_d[:, :], writes=[c3_t])
    c4_t = T("consts4")
    P.dma("sp", hn[:, :], hn_d[:, :], writes=[c4_t])
    wq, wq_t = load_w_bf16(P, "wq_sb", wq_d, D, 128)
    wk, wk_t = load_w_bf16(P, "wk_sb", wk_d, D, 128)
    wt, wt_t = load_w_bf16(P, "wt_sb", wt_d, D, 644)

    def dbl(name, shape, dt):
        return [P.sb("%s%d" % (name, i), shape, dt) for i in range(2)], [T("%s%d" % (name, i)) for i in range(2)]

    xin, xin_t = dbl("xin", [128, D], F32)
    xnT, xnT_t = dbl("xnT", [128, 8, 128], BF16)
    qT, qT_t = dbl("qT", [64, 2, 128], BF16)
    kT, kT_t = dbl("kT", [64, 2, 128], BF16)
    ktok, ktok_t = dbl("ktok", [128, 128], BF16)
    og, og_t = dbl("og", [128, 256], F32)
    gt, gt_t = dbl("gt", [128, 24], F32)
    V1 = [dbl("V1_%d_" % h, [128, 129], BF16) for h in range(2)]
    V2 = [dbl("V2_%d_" % h, [128, 129], BF16) for h in range(2)]
    PT, PT_t = dbl("PT", [128, 128], BF16)
    hh, hh_t = dbl("hh", [128, 128], F32)
    sq = P.sb("sqj", [128, 128], BF16)
    sq_t = T("sqj")
    st, st_t = dbl("st", [128, 8], F32)
    yt, yt_t = dbl("yt", [128, 256], BF16)
    C = [P.sb("C%d" % h, [64, 129], F32) for h in range(2)]
    C_t = [T("C%d" % h) for h in range(2)]
    Cb = [dbl("Cb%d_" % h, [64, 129], BF16) for h in range(2)]

    pb = [P.ps("pbT", [128, 8, 128], BF16)]
    pb_t = [T("pbT", True)]
    pq = P.ps("pq", [128, 512], F32); pq_t = T("pq", True)
    p1 = P.ps("p1", [128, 512], F32); p1_t = T("p1", True)
    p2 = P.ps("p2", [128, 512], F32); p2_t = T("p2", True)
    pg = P.ps("pg", [128, 512], F32); pg_t = T("pg", True)
    pS = P.ps("pS", [128, 512], F32); pS_t = T("pS", True)
    pO = P.ps("pO", [128, 512], F32); pO_t = T("pO", True)
    pA = P.ps("pA", [128, 512], F32); pA_t = T("pA", True)
    norm = NormCtx(P, gn, ident, ident_t, pb, pb_t)
    norm.lnexp = True
    norm.dst_full = lambda k: xnT[k % 2][:, :, :]

    for h in range(2):
        P.op("dve", lambda e, h=h: e.memset(C[h][:, :], 0.0), writes=[C_t[h]])
        P.op("dve", lambda e, h=h: e.memset(Cb[h][0][0][:, :], 0.0), writes=[Cb[h][1][0]])

    for j in range(NCH):
        b = j % 2
        P.dma("sp", xin[b][:, :], hin[j * 128:(j + 1) * 128, :], writes=[xin_t[b]])
        norm.run(xin[b][:, :], xin_t[b], lambda c, b=b: xnT[b][:, c, :], xnT_t[b])
        for qi, (w, w_t) in enumerate(((wq, wq_t), (wk, wk_t))):
            for h in range(2):
                for c in range(8):
                    P.op("pe", lambda e, c=c, h=h, w=w, qi=qi, b=b: e.matmul(
                        pq[0:64, (2 * qi + h) * 128:(2 * qi + h + 1) * 128],
                        lhsT=w[:, c, h * 64:(h + 1) * 64], rhs=xnT[b][:, c, :],
                        start=(c == 0), stop=(c == 7)),
                        reads=[w_t, xnT_t[b]], writes=[pq_t])
        P.op("act", lambda e, b=b: e.activation(out=qT[b][:, :, :].rearrange("p a n -> p (a n)"),
                                                in_=pq[0:64, 0:256], func=AF.Copy, scale=0.125),
             reads=[pq_t], writes=[qT_t[b]])
        P.op("dve", lambda e, b=b: e.tensor_copy(out=kT[b][:, :, :].rearrange("p a n -> p (a n)"),
                                                 in_=pq[0:64, 256:512]),
             reads=[pq_t], writes=[kT_t[b]])
        for c in range(8):
            P.op("pe", lambda e, c=c, b=b: e.matmul(p1[:, 0:384], lhsT=xnT[b][:, c, :], rhs=wt[:, c, 0:384],
                                                    start=(c == 0), stop=(c == 7)),
                 reads=[wt_t, xnT_t[b]], writes=[p1_t])
        for c in range(8):
            P.op("pe", lambda e, c=c, b=b: e.matmul(p2[:, 0:260], lhsT=xnT[b][:, c, :], rhs=wt[:, c, 384:644],
                                                    start=(c == 0), stop=(c == 7)),
                 reads=[wt_t, xnT_t[b]], writes=[p2_t])
        g = gt[b]
        g_t = gt_t[b]
        P.op("dve", lambda e, g=g: e.tensor_tensor(out=g[:, 0:4], in0=p2[:, 256:260], in1=bg[:, :], op=ALU.add),
             reads=[p2_t, c3_t], writes=[g_t])
        P.op("act", lambda e, g=g: e.activation(out=g[:, 4:8], in_=g[:, 0:4], func=AF.Tanh, scale=1.0 / 15.0),
             reads=[g_t], writes=[g_t])
        P.op("act", lambda e, g=g: e.activation(out=g[:, 8:10], in_=g[:, 6:8], func=AF.Exp, scale=-15.0),
             reads=[g_t], writes=[g_t])
        P.op("act", lambda e, g=g: e.activation(out=g[:, 10:12], in_=g[:, 8:10], func=AF.Ln, bias=1.0),
             reads=[g_t], writes=[g_t])
        P.op("pe", lambda e, g=g: e.matmul(pg[:, 0:2], lhsT=U[:, :], rhs=g[:, 10:12], start=True, stop=True),
             reads=[g_t, c_t], writes=[pg_t])
        P.op("pe", lambda e, g=g: e.matmul(pg[:, 2:4], lhsT=ONES[:, :], rhs=g[:, 10:12], start=True, stop=True),
             reads=[g_t, c2_t], writes=[pg_t])
        P.op("dve", lambda e, g=g: e.tensor_copy(out=g[:, 12:16], in_=pg[:, 0:4]), reads=[pg_t], writes=[g_t])
        P.op("dve", lambda e, g=g: e.tensor_tensor(out=g[:, 16:18], in0=g[:, 12:14], in1=g[:, 14:16],
                                                   op=ALU.subtract),
             reads=[g_t], writes=[g_t])
        s = st[b]
        s_t = st_t[b]
        for h in range(2):
            P.op("act", lambda e, g=g, h=h: e.activation(out=g[:, 18 + h:19 + h], in_=g[:, 4 + h:5 + h], func=AF.Exp,
                                                         scale=15.0, bias=g[:, 12 + h:13 + h]),
                 reads=[g_t], writes=[g_t])
            P.op("act", lambda e, g=g, h=h: e.activation(out=g[:, 20 + h:21 + h], in_=g[:, 4 + h:5 + h], func=AF.Exp,
                                                         scale=15.0, bias=g[:, 16 + h:17 + h]),
                 reads=[g_t], writes=[g_t])
        P.op("act", lambda e, g=g: e.activation(out=g[:, 22:24], in_=g[:, 12:14], func=AF.Exp, scale=-1.0),
             reads=[g_t], writes=[g_t])
        P.op("act", lambda e, g=g, s=s: e.activation(out=s[:, 6:8], in_=g[:, 14:16], func=AF.Exp, scale=-1.0),
             reads=[g_t], writes=[s_t])
        P.op("act", lambda e, b=b: e.activation(out=ktok[b][:, :], in_=p1[:, 0:128], func=AF.Copy),
             reads=[p1_t], writes=[ktok_t[b]])
        P.op("act", lambda e, b=b: e.activation(out=og[b][:, :], in_=p2[:, 0:256], func=AF.Exp, scale=-1.0),
             reads=[p2_t], writes=[og_t[b]], n=256)
        P.op("pool", lambda e, b=b: e.tensor_scalar_add(out=og[b][:, :], in0=og[b][:, :], scalar1=1.0),
             reads=[og_t[b]], writes=[og_t[b]], n=256)
        P.op("dve", lambda e, b=b: e.reciprocal(out=og[b][:, :], in_=og[b][:, :]),
             reads=[og_t[b]], writes=[og_t[b]], n=256)
        for h in range(2):
            for (V, col) in ((V1[h], 18 + h), (V2[h], 20 + h)):
                Vb, Vb_t = V[0][b], V[1][b]
                P.op("dve", lambda e, Vb=Vb, g=g, col=col, h=h: e.tensor_scalar(
                    out=Vb[:, 0:128], in0=p1[:, 128 + h * 128:256 + h * 128], scalar1=g[:, col:col + 1],
                    scalar2=None, op0=ALU.mult),
                    reads=[p1_t, g_t], writes=[Vb_t])
                P.op("pool", lambda e, Vb=Vb, g=g, col=col: e.tensor_copy(out=Vb[:, 128:129], in_=g[:, col:col + 1]),
                     reads=[g_t], writes=[Vb_t], n=1)
        for h in range(2):
            Cb_cur, Cb_cur_t = Cb[h][0][b], Cb[h][1][b]
            Cb_nxt, Cb_nxt_t = Cb[h][0][1 - b], Cb[h][1][1 - b]
            V1b, V1b_t = V1[h][0][b], V1[h][1][b]
            V2b, V2b_t = V2[h][0][b], V2[h][1][b]
            P.op("pe", lambda e, h=h, b=b: e.matmul(pS[:, 0:128], lhsT=kT[b][:, h, :], rhs=qT[b][:, h, :],
                                                    start=True, stop=True),
                 reads=[kT_t[b], qT_t[b]], writes=[pS_t])
            pt, pt_t = PT[h], PT_t[h]
            P.op("dve", lambda e, pt=pt: e.tensor_tensor(out=pt[:, :], in0=pS[:, 0:128], in1=U[:, :], op=ALU.mult),
                 reads=[pS_t, c_t], writes=[pt_t])
            P.op("pe", lambda e, h=h, b=b, Cb_cur=Cb_cur: e.matmul(pO[:, 0:129], lhsT=qT[b][:, h, :], rhs=Cb_cur[:, :],
                                                                   start=True, stop=False),
                 reads=[qT_t[b], Cb_cur_t], writes=[pO_t])
            P.op("pe", lambda e, pt=pt, V1b=V1b: e.matmul(pO[:, 0:129], lhsT=pt[:, :], rhs=V1b[:, :],
                                                          start=False, stop=True),
                 reads=[pt_t, V1b_t], writes=[pO_t])
            o = 3 * h
            P.op("dve", lambda e, s=s, g=g, h=h, o=o: e.tensor_tensor(out=s[:, o:o + 1], in0=pO[:, 128:129],
                                                                      in1=g[:, 22 + h:23 + h], op=ALU.mult),
                 reads=[pO_t, g_t], writes=[s_t])
            P.op("dve", lambda e, s=s, o=o: e.tensor_scalar(out=s[:, o + 1:o + 2], in0=s[:, o:o + 1], scalar1=-1.0,
                                                            scalar2=1.0, op0=ALU.mult, op1=ALU.max),
                 reads=[s_t], writes=[s_t])
            P.op("dve", lambda e, s=s, o=o: e.tensor_tensor(out=s[:, o:o + 1], in0=s[:, o:o + 1],
                                                            in1=s[:, o + 1:o + 2], op=ALU.max),
                 reads=[s_t], writes=[s_t])
            P.op("dve", lambda e, s=s, o=o: e.reciprocal(out=s[:, o:o + 1], in_=s[:, o:o + 1]),
                 reads=[s_t], writes=[s_t])
            P.op("dve", lambda e, s=s, g=g, h=h, o=o: e.tensor_tensor(out=s[:, o + 1:o + 2], in0=g[:, 22 + h:23 + h],
                                                                      in1=s[:, o:o + 1], op=ALU.mult),
                 reads=[s_t, g_t], writes=[s_t])
            hb, hb_t = hh[h], hh_t[h]
            P.op("dve", lambda e, hb=hb, s=s, o=o: e.tensor_scalar(out=hb[:, :], in0=pO[:, 0:128],
                                                                   scalar1=s[:, o + 1:o + 2], scalar2=None, op0=ALU.mult),
                 reads=[pO_t, s_t], writes=[hb_t])
            P.op("act", lambda e, hb=hb, s=s, o=o: e.activation(out=sq[:, :], in_=hb[:, :], func=AF.Square,
                                                                accum_out=s[:, o + 2:o + 3]),
                 reads=[hb_t], writes=[sq_t, s_t])
            P.op("dve", lambda e, s=s, o=o: e.tensor_scalar(out=s[:, o + 2:o + 3], in0=s[:, o + 2:o + 3],
                                                            scalar1=1.0 / 128.0, scalar2=EPS, op0=ALU.mult, op1=ALU.add),
                 reads=[s_t], writes=[s_t])
            P.op("act", lambda e, s=s, o=o: e.activation(out=s[:, o + 2:o + 3], in_=s[:, o + 2:o + 3], func=AF.Ln),
                 reads=[s_t], writes=[s_t])
            P.op("act", lambda e, s=s, o=o: e.activation(out=s[:, o + 2:o + 3], in_=s[:, o + 2:o + 3], func=AF.Exp,
                                                         scale=-0.5),
                 reads=[s_t], writes=[s_t])
            P.op("dve", lambda e, hb=hb, s=s, o=o, h=h: e.scalar_tensor_tensor(
                out=hb[:, :], in0=hb[:, :], scalar=s[:, o + 2:o + 3], in1=hn[:, h * 128:(h + 1) * 128],
                op0=ALU.mult, op1=ALU.mult),
                reads=[hb_t, s_t, c4_t], writes=[hb_t])
            P.op("dve", lambda e, hb=hb, h=h, b=b: e.tensor_tensor(out=yt[b][:, h * 128:(h + 1) * 128], in0=hb[:, :],
                                                                   in1=og[b][:, h * 128:(h + 1) * 128], op=ALU.mult),
                 reads=[hb_t, og_t[b]], writes=[yt_t[b]])
            P.op("pe", lambda e, h=h, b=b, V2b=V2b: e.matmul(pA[0:64, 0:129], lhsT=ktok[b][:, h * 64:(h + 1) * 64],
                                                             rhs=V2b[:, :], start=True, stop=True),
                 reads=[ktok_t[b], V2b_t], writes=[pA_t])
            P.op("dve", lambda e, h=h, s=s: e.scalar_tensor_tensor(
                out=C[h][:, :], in0=C[h][:, :], scalar=s[0:64, 6 + h:7 + h], in1=pA[0:64, 0:129],
                op0=ALU.mult, op1=ALU.add),
                reads=[C_t[h], s_t, pA_t], writes=[C_t[h]])
            P.op("pool", lambda e, h=h, Cb_nxt=Cb_nxt: e.tensor_copy(out=Cb_nxt[:, :], in_=C[h][:, :]),
                 reads=[C_t[h]], writes=[Cb_nxt_t], n=129)
        P.dma("sp", yout[j * 128:(j + 1) * 128, :], yt[b][:, :], reads=[yt_t[b]], writes=[T("yo%d" % j)], final=True)
    return P.build()


def build_attn():
    P = Prog()
    NT = S // 128
    xin_d = P.dram_in("xin", [S, D], F32)
    gn = P.dram_in("gn", [128, 8], F32)
    w_d = P.dram_in("wqkv", [D, 768], F32)
    cs_d = P.dram_in("cs", [S, 128], F32)
    identd = P.dram_in("ident", [128, 128], BF16)
    identf_d = P.dram_in("identf", [128, 128], F32)
    oneh_d = P.dram_in("onehot", [32, S], BF16)
    tri_d = P.dram_in("tri", [128, 128], BF16)
    M1_d = P.dram_in("M1", [128, 1024], F32)
    A30_d = P.dram_in("A30", [128, 1024], F32)
    Bc_d = P.dram_in("Bc", [128, 1024], F32)
    oT = P.dram_out("oT", [256, S], BF16)
    qTs = P.dram_tmp("qTs", [4, 64, S], BF16)
    kTs = P.dram_tmp("kTs", [4, 64, S], BF16)
    vs = P.dram_tmp("vs", [S, 256], BF16)

    ident, ident_t = make_ident(P, identd)
    identf = P.sb("identf_sb", [128, 128], F32)
    tri = P.sb("tri_sb", [128, 128], BF16)
    M1 = P.sb("M1_sb", [128, 1024], F32)
    A30 = P.sb("A30_sb", [128, 1024], F32)
    Bc = P.sb("Bc_sb", [128, 1024], F32)
    cst_t = []
    for dst, src in ((identf, identf_d), (tri, tri_d), (M1, M1_d), (A30, A30_d), (Bc, Bc_d)):
        t = T("c")
        cst_t.append(t)
        P.dma("sp", dst[:, :], src[:, :], writes=[t])
    w, w_t = load_w_bf16(P, "w_sb", w_d, D, 768)

    KA = P.sb("KA", [128, S], BF16)
    QA = P.sb("QA", [128, S], BF16)
    VA = P.sb("VA", [128, NT, 128], BF16)
    qabs = P.sb("qabs", [64, S], BF16)
    KAoh_t = T("KAoh")
    for q4 in range(4):
        P.dma("sp", KA[64:96, q4 * 2048:(q4 + 1) * 2048], oneh_d[:, q4 * 2048:(q4 + 1) * 2048], writes=[KAoh_t])
    VA1_t = T("VA1")
    P.op("pool", lambda e: e.memset(VA[:, :, 64:128], 1.0), writes=[VA1_t])

    def dbl(name, shape, dt):
        return [P.sb("%s%d" % (name, i), shape, dt) for i in range(2)], [T("%s%d" % (name, i)) for i in range(2)]

    xin, xin_t = dbl("xin_sb", [128, D], F32)
    cs, cs_t = dbl("cs_sb", [128, 128], F32)
    xnT, xnT_t = dbl("xnT", [128, 8, 128], BF16)
    qk32, qk32_t = dbl("qk32", [128, 512], F32)
    rt, rt_t = dbl("rt", [128, 4, 64], F32)
    qkb, qkb_t = dbl("qkb", [128, 512], BF16)
    vb, vb_t = dbl("vb", [128, 256], BF16)
    qkT, qkT_t = dbl("qkT", [64, 8, 128], BF16)
    ksum, ksum_t = dbl("ksum", [64, 4], F32)
    kms = P.sb("kms", [64, 4, 32], F32)
    kms_t = T("kms")
    kmeanb = P.sb("kmeanb", [64, 4, 32], BF16)
    kmeanb_t = T("kmeanb")

    pb = [P.ps("pbT", [128, 8, 128], BF16)]
    pb_t = [T("pbT", True)]
    pqT = P.ps("pqT", [128, 8, 128], BF16); pqT_t = T("pqT", True)
    pqk = P.ps("pqk", [128, 512], F32); pqk_t = T("pqk", True)
    pv = P.ps("pv", [128, 512], F32); pv_t = T("pv", True)
    pS = [P.ps("pS%d" % i, [128, 2, 512], F32) for i in range(2)]
    pS_t = [T("pS%d" % i, True) for i in range(2)]
    pO = [pqk, pqk]
    pO_t = [pqk_t, pqk_t]
    norm = NormCtx(P, gn, ident, ident_t, pb, pb_t)
    norm.dst_full = lambda k: xnT[k % 2][:, :, :]

    scr_t = []
    for j in range(NT):
        b = j % 2
        P.dma("sp", xin[b][:, :], xin_d[j * 128:(j + 1) * 128, :], writes=[xin_t[b]])
        P.dma("sp", cs[b][:, :], cs_d[j * 128:(j + 1) * 128, :], writes=[cs_t[b]])
        norm.run(xin[b][:, :], xin_t[b], lambda c, b=b: xnT[b][:, c, :], xnT_t[b])
        for c in range(8):
            P.op("pe", lambda e, c=c, b=b: e.matmul(pqk[:, :], lhsT=xnT[b][:, c, :], rhs=w[:, c, 0:512],
                                                    start=(c == 0), stop=(c == 7)),
                 reads=[w_t, xnT_t[b]], writes=[pqk_t], n=512)
        for c in range(8):
            P.op("pe", lambda e, c=c, b=b: e.matmul(pv[:, 0:256], lhsT=xnT[b][:, c, :], rhs=w[:, c, 512:768],
                                                    start=(c == 0), stop=(c == 7)),
                 reads=[w_t, xnT_t[b]], writes=[pv_t], n=256)
        P.op("act", lambda e, b=b: e.activation(out=qk32[b][:, 0:256], in_=pqk[:, 0:256], func=AF.Copy, scale=0.125),
             reads=[pqk_t], writes=[qk32_t[b]])
        P.op("act", lambda e, b=b: e.activation(out=qk32[b][:, 256:512], in_=pqk[:, 256:512], func=AF.Copy),
             reads=[pqk_t], writes=[qk32_t[b]])
        P.op("dve", lambda e, b=b: e.tensor_copy(out=vb[b][:, :], in_=pv[:, 0:256]), reads=[pv_t], writes=[vb_t[b]])
        tv = T("vs%d" % j)
        scr_t.append(tv)
        P.dma("sp", vs[j * 128:(j + 1) * 128, :], vb[b][:, :], reads=[vb_t[b]], writes=[tv])
        qv = qk32[b][:, :].rearrange("p (g d) -> p g d", d=64)
        x1, x2 = qv[:, :, 0:8], qv[:, :, 8:16]
        cosv = cs[b][:, 0:64].rearrange("p (g f) -> p g f", f=8)
        sinv = cs[b][:, 64:128].rearrange("p (g f) -> p g f", f=8)
        r = rt[b]
        rv = [r[:, i, :].rearrange("p (g f) -> p g f", f=8) for i in range(4)]
        for i, (a0, a1) in enumerate(((x1, cosv), (x2, sinv), (x2, cosv), (x1, sinv))):
            P.op("dve", lambda e, i=i, a0=a0, a1=a1, rv=rv: e.tensor_tensor(out=rv[i], in0=a0, in1=a1, op=ALU.mult),
                 reads=[qk32_t[b], cs_t[b]], writes=[rt_t[b]])
        P.op("dve", lambda e, x1=x1, rv=rv: e.tensor_tensor(out=x1, in0=rv[0], in1=rv[1], op=ALU.subtract),
             reads=[rt_t[b]], writes=[qk32_t[b]])
        P.op("dve", lambda e, x2=x2, rv=rv: e.tensor_tensor(out=x2, in0=rv[2], in1=rv[3], op=ALU.add),
             reads=[rt_t[b]], writes=[qk32_t[b]])
        P.op("act", lambda e, b=b: e.activation(out=qkb[b][:, :], in_=qk32[b][:, :], func=AF.Copy),
             reads=[qk32_t[b]], writes=[qkb_t[b]])
        for g in range(8):
            P.op("pe", lambda e, g=g, b=b: e.transpose(out=pqT[0:64, g, :], in_=qkb[b][:, g * 64:(g + 1) * 64],
                                                       identity=ident[:, :]),
                 reads=[qkb_t[b], ident_t], writes=[pqT_t])
        P.op("dve", lambda e, b=b: e.tensor_copy(out=qkT[b][:, :, :], in_=pqT[0:64, :, :]),
             reads=[pqT_t], writes=[qkT_t[b]])
        tq = T("qs%d" % j)
        tk = T("ks%d" % j)
        scr_t += [tq, tk]
        P.dma("sp", qTs[:, :, j * 128:(j + 1) * 128].rearrange("h d n -> d h n"), qkT[b][:, 0:4, :],
              reads=[qkT_t[b]], writes=[tq])
        P.dma("sp", kTs[:, :, j * 128:(j + 1) * 128].rearrange("h d n -> d h n"), qkT[b][:, 4:8, :],
              reads=[qkT_t[b]], writes=[tk])
        blk = j // 2
        if j % 2 == 0:
            P.op("dve", lambda e, b=b, blk=blk: e.tensor_reduce(out=kms[:, :, blk], in_=qkT[b][:, 4:8, :],
                                                                axis=AX.X, op=ALU.add),
                 reads=[qkT_t[b]], writes=[kms_t])
        else:
            P.op("dve", lambda e, b=b: e.tensor_reduce(out=ksum[b][:, :], in_=qkT[b][:, 4:8, :],
                                                       axis=AX.X, op=ALU.add),
                 reads=[qkT_t[b]], writes=[ksum_t[b]])
            P.op("dve", lambda e, b=b, blk=blk: e.tensor_tensor(out=kms[:, :, blk], in0=kms[:, :, blk],
                                                                in1=ksum[b][:, :], op=ALU.add),
                 reads=[ksum_t[b], kms_t], writes=[kms_t])
    P.op("act", lambda e: e.activation(out=kmeanb[:, :, :], in_=kms[:, :, :], func=AF.Copy, scale=1.0 / 256.0),
         reads=[kms_t], writes=[kmeanb_t])

    KA_t, QA_t, VA_t = T("KA"), T("QA"), T("VA")
    QAb_t = [T("QAb%d" % g) for g in range(16)]
    ab = P.sb("ab", [64, 2048], BF16); ab_t = T("ab")
    km4 = P.sb("km4", [64, 8], F32); km4_t = T("km4")
    kmaxb = P.sb("kmaxb", [64, 2], BF16); kmaxb_t = T("kmaxb")
    gm, gm_t = dbl("gm", [128, 32], F32)
    sel, sel_t = dbl("sel", [128, 32], F32)
    top8, top8_t = dbl("top8", [128, 8], F32)
    mq, mq_t = dbl("mq", [128, 1], F32)
    BT, BT_t = dbl("BT", [128, 4, 96], F32)
    for i in range(2):
        P.op("pool", lambda e, i=i: e.memset(BT[i][:, :, :], 0.0), writes=[BT_t[i]])
    Pb = [P.sb("Pb%d" % i, [128, 2, 512], BF16) for i in range(3)]
    Pb_t = [T("Pb%d" % i) for i in range(3)]
    OS, OS_t = dbl("OS", [128, 512], F32)
    DN, DN_t = dbl("DN", [64, 512], F32)
    OTs, OTs_t = dbl("OTs", [64, 512], BF16)
    pBT, pBT_t = pv, pv_t
    pG = P.ps("pG", [128, 512], F32) if False else None
    pG, pG_t = pqk, pqk_t
    kS = 0
    kP = 0
    ka_l = [T("kal%d" % i) for i in range(4)]
    qa_l = [T("qal%d" % i) for i in range(4)]
    va_l = [T("val%d" % i) for i in range(4)]
    qabs_l = [T("qabs%d" % i) for i in range(4)]
    for h in range(4):
        for q4 in range(4):
            sl = slice(q4 * 2048, (q4 + 1) * 2048)
            P.dma("sp", KA[0:64, sl], kTs[h, :, sl], reads=scr_t, writes=[ka_l[q4]])
            P.dma("sp", QA[0:64, sl], qTs[h, :, sl], reads=scr_t, writes=[qa_l[q4]])
            P.dma("pool", VA[:, q4 * 16:(q4 + 1) * 16, 0:64],
                  vs[q4 * 2048:(q4 + 1) * 2048, h * 64:(h + 1) * 64].rearrange("(t p) c -> p t c", p=128),
                  reads=scr_t, writes=[va_l[q4]])
        for q4 in range(4):
            sl = slice(q4 * 2048, (q4 + 1) * 2048)
            P.op("act", lambda e, sl=sl: e.activation(out=qabs[:, sl], in_=QA[0:64, sl], func=AF.Abs),
                 reads=[qa_l[q4]], writes=[qabs_l[q4]])
            P.op("act", lambda e, sl=sl: e.activation(out=ab[:, :], in_=KA[0:64, sl], func=AF.Abs),
                 reads=[ka_l[q4]], writes=[ab_t])
            P.op("dve", lambda e, q4=q4: e.tensor_reduce(out=km4[:, q4:q4 + 1], in_=ab[:, :], axis=AX.X, op=ALU.max),
                 reads=[ab_t], writes=[km4_t])
        P.op("dve", lambda e: e.tensor_reduce(out=km4[:, 4:5], in_=km4[:, 0:4], axis=AX.X, op=ALU.max),
             reads=[km4_t], writes=[km4_t])
        P.op("dve", lambda e: e.tensor_copy(out=kmaxb[:, 0:1], in_=km4[:, 4:5]), reads=[km4_t], writes=[kmaxb_t])
        for g in range(16):
            bt, bt_t = BT[g % 2], BT_t[g % 2]
            for qi in range(4):
                qt = g * 4 + qi
                cur = qt // 2
                k2 = qt % 2
                csl = slice(qt * 128, (qt + 1) * 128)
                P.op("pe", lambda e, csl=csl, h=h: e.matmul(pG[:, 0:32], lhsT=QA[0:64, csl], rhs=kmeanb[:, h, :],
                                                            start=True, stop=True),
                     reads=[qa_l, kmeanb_t], writes=[pG_t])
                P.op("pe", lambda e, csl=csl: e.matmul(pG[:, 32:33], lhsT=qabs[:, csl], rhs=kmaxb[:, 0:1],
                                                       start=True, stop=True),
                     reads=[qabs_l, kmaxb_t], writes=[pG_t])
                P.op("dve", lambda e, k2=k2, cur=cur: e.tensor_tensor(out=gm[k2][:, :], in0=pG[:, 0:32],
                                                                      in1=M1[:, cur * 32:(cur + 1) * 32], op=ALU.add),
                     reads=[pG_t, cst_t], writes=[gm_t[k2]])
                P.op("dve", lambda e, k2=k2: e.tensor_copy(out=mq[k2][:, :], in_=pG[:, 32:33]),
                     reads=[pG_t], writes=[mq_t[k2]])
                P.op("dve", lambda e, k2=k2: e.max(out=top8[k2][:, :], in_=gm[k2][:, :]),
                     reads=[gm_t[k2]], writes=[top8_t[k2]])
                P.op("dve", lambda e, k2=k2: e.tensor_scalar(out=sel[k2][:, :], in0=gm[k2][:, :],
                                                             scalar1=top8[k2][:, 2:3], scalar2=1.0,
                                                             op0=ALU.is_ge, op1=ALU.subtract),
                     reads=[gm_t[k2], top8_t[k2]], writes=[sel_t[k2]])
                P.op("dve", lambda e, k2=k2, cur=cur: e.tensor_tensor(out=sel[k2][:, :], in0=sel[k2][:, :],
                                                                      in1=A30[:, cur * 32:(cur + 1) * 32], op=ALU.mult),
                     reads=[sel_t[k2], cst_t], writes=[sel_t[k2]])
                P.op("dve", lambda e, k2=k2, cur=cur: e.tensor_tensor(out=sel[k2][:, :], in0=sel[k2][:, :],
                                                                      in1=Bc[:, cur * 32:(cur + 1) * 32], op=ALU.add),
                     reads=[sel_t[k2], cst_t], writes=[sel_t[k2]])
                P.op("dve", lambda e, k2=k2, bt=bt, qi=qi: e.tensor_scalar(out=bt[:, qi, 64:96], in0=sel[k2][:, :],
                                                                           scalar1=mq[k2][:, 0:1], scalar2=None,
                                                                           op0=ALU.subtract),
                     reads=[sel_t[k2], mq_t[k2]], writes=[bt_t])
                P.op("pe", lambda e, bt=bt, qi=qi: e.transpose(out=pBT[0:96, qi * 128:(qi + 1) * 128], in_=bt[:, qi, :],
                                                               identity=identf[:, :]),
                     reads=[bt_t, cst_t], writes=[pBT_t])
            P.op("act", lambda e, g=g: e.activation(out=QA[64:96, g * 512:(g + 1) * 512], in_=pBT[64:96, :], func=AF.Copy),
                 reads=[pBT_t], writes=[QAb_t[g]])
        for g in range(16):
            po, po_t = pO[g % 2], pO_t[g % 2]
            nk = 4 * g + 4
            for kp in range(nk // 2):
                ps, ps_t = pS[kS % 2], pS_t[kS % 2]
                kS += 1
                pbuf, pbuf_t = Pb[kP % 3], Pb_t[kP % 3]
                kP += 1
                c0s = []
                for u in range(2):
                    kt = 2 * kp + u
                    i = kt - 4 * g
                    c0 = max(i, 0) * 128
                    c0s.append(c0)
                    P.op("pe", lambda e, ps=ps, kt=kt, g=g, c0=c0, i=i, u=u: e.matmul(
                        ps[:, u, c0:512], lhsT=KA[0:96, kt * 128:(kt + 1) * 128],
                        rhs=QA[0:96, g * 512 + c0:(g + 1) * 512], start=True, stop=(i < 0)),
                        reads=[ka_l, KAoh_t, qa_l, QAb_t[g]], writes=[ps_t], n=512 - c0)
                    if i >= 0:
                        P.op("pe", lambda e, ps=ps, c0=c0, u=u: e.matmul(ps[:, u, c0:c0 + 128], lhsT=ident[:, :],
                                                                         rhs=tri[:, :], start=False, stop=True),
                             reads=[ident_t, cst_t], writes=[ps_t])
                cm = c0s[0]
                P.op("act", lambda e, ps=ps, pbuf=pbuf, cm=cm: e.activation(out=pbuf[:, :, cm:512], in_=ps[:, :, cm:512],
                                                                            func=AF.Exp),
                     reads=[ps_t], writes=[pbuf_t], n=2 * (512 - cm))
                for u in range(2):
                    kt = 2 * kp + u
                    c0 = c0s[u]
                    P.op("pe", lambda e, po=po, pbuf=pbuf, kt=kt, c0=c0, nk=nk, u=u: e.matmul(
                        po[:, c0:512], lhsT=VA[:, kt, :], rhs=pbuf[:, u, c0:512], start=(kt == 0), stop=(kt == nk - 1)),
                        reads=[va_l, VA1_t, pbuf_t], writes=[po_t], n=512 - c0)
            o = g % 2
            P.op("act", lambda e, o=o, po=po: e.activation(out=OS[o][:, :], in_=po[:, :], func=AF.Copy),
                 reads=[po_t], writes=[OS_t[o]])
            P.dma("sp", DN[o][:, :], OS[o][64:128, :], reads=[OS_t[o]], writes=[DN_t[o]])
            P.op("dve", lambda e, o=o: e.reciprocal(out=DN[o][:, :], in_=DN[o][:, :]), reads=[DN_t[o]], writes=[DN_t[o]])
            P.op("dve", lambda e, o=o: e.tensor_tensor(out=OTs[o][:, :], in0=OS[o][0:64, :], in1=DN[o][:, :], op=ALU.mult),
                 reads=[OS_t[o], DN_t[o]], writes=[OTs_t[o]])
            P.dma("sp", oT[h * 64:(h + 1) * 64, g * 512:(g + 1) * 512], OTs[o][:, :], reads=[OTs_t[o]],
                  writes=[T("oT")], final=True)
    return P.build()


_PROGS = {}


def _prog(name):
    if name not in _PROGS:
        _PROGS[name] = {"attn": build_attn, "mlstm": build_mlstm,
                        "pf0": lambda: build_projffn(False), "pf1": lambda: build_projffn(True)}[name]()
    return _PROGS[name]


def _run(name, maps):
    res = run_bass_kernel_spmd(_prog(name), maps, core_ids=list(range(NCORE)))
    return res.results


def _gain_layout(g):
    return np.ascontiguousarray(np.asarray(g, np.float32).reshape(8, 128).T)


def _consts():
    bf = ml_dtypes.bfloat16
    c = {}
    c["ident"] = np.eye(128, dtype=np.float32).astype(bf)
    c["identf"] = np.eye(128, dtype=np.float32)
    pos = np.arange(S, dtype=np.float32)
    inv = (np.float32(500000.0) ** (-np.arange(0, 16, 2, dtype=np.float32) / np.float32(16))).astype(np.float32)
    ang = (pos[:, None] * inv[None, :]).astype(np.float32)
    cos = np.cos(ang).astype(np.float32)
    sin = np.sin(ang).astype(np.float32)
    c["cs"] = np.ascontiguousarray(np.concatenate([np.tile(cos, (1, 8)), np.tile(sin, (1, 8))], axis=1))
    blk = np.arange(S) // 256
    c["onehot"] = (blk[None, :] == np.arange(32)[:, None]).astype(np.float32).astype(bf)
    kk = np.arange(128)
    c["tri"] = np.where(kk[:, None] > kk[None, :], NEG, 0.0).astype(np.float32).astype(bf)
    cur = np.arange(32)[:, None]
    n = np.arange(32)[None, :]
    m1 = np.where(n < cur, 0.0, NEG).astype(np.float32).reshape(1, 1024)
    a30 = np.where(n < cur, -NEG, 0.0).astype(np.float32).reshape(1, 1024)
    bc = np.where(n <= cur, 0.0, NEG).astype(np.float32).reshape(1, 1024)
    c["M1"] = np.ascontiguousarray(np.tile(m1, (128, 1)))
    c["A30"] = np.ascontiguousarray(np.tile(a30, (128, 1)))
    c["Bc"] = np.ascontiguousarray(np.tile(bc, (128, 1)))
    c["U"] = np.triu(np.ones((128, 128), np.float32))
    c["ones"] = np.ones((128, 128), np.float32)
    return c


def kernel(x, attn_norm, attn_w_qkv, attn_w_o, mlstm_norm, mlstm_w_in, mlstm_b_gates,
           mlstm_head_norm, mlstm_w_out, ffn_norm, ffn_w_gate_up, ffn_w_down, final_norm):
    f32 = np.float32
    x = np.asarray(x, f32)
    c = _consts()
    wqkv = np.asarray(attn_w_qkv, f32)[0]
    maps = []
    for core in range(NCORE):
        b, hg = core // 4, core % 4
        cols = np.concatenate([np.arange(hg * 256, (hg + 1) * 256) + off for off in (0, 1024, 2048)])
        maps.append({"xin": x[b], "gn": _gain_layout(attn_norm[0]), "wqkv": np.ascontiguousarray(wqkv[:, cols]),
                     "cs": c["cs"], "ident": c["ident"], "identf": c["identf"], "onehot": c["onehot"],
                     "tri": c["tri"], "M1": c["M1"], "A30": c["A30"], "Bc": c["Bc"]})
    ra = _run("attn", maps)
    oT = [np.concatenate([ra[b * 4 + hg]["oT"] for hg in range(4)], axis=0) for b in range(B)]
    maps = []
    for core in range(NCORE):
        b, q = core // 4, core % 4
        sl = slice(q * 2048, (q + 1) * 2048)
        maps.append({"hin": x[b, sl], "aT": np.ascontiguousarray(oT[b][:, sl]), "wp": np.asarray(attn_w_o, f32)[0],
                     "gn": _gain_layout(ffn_norm[0]), "wgu": np.asarray(ffn_w_gate_up, f32)[0],
                     "wd": np.asarray(ffn_w_down, f32)[0], "ident": c["ident"]})
    rb = _run("pf0", maps)
    h1 = [np.concatenate([rb[b * 4 + q]["hout"] for q in range(4)], axis=0) for b in range(B)]
    win = np.asarray(mlstm_w_in, f32)[0]
    bgv = np.asarray(mlstm_b_gates, f32)[0]
    hnv = np.asarray(mlstm_head_norm, f32)[0]
    maps = []
    for core in range(NCORE):
        b, hp = core // 4, core % 4
        h0 = 2 * hp
        wq = win[:, h0 * 64:(h0 + 2) * 64]
        wk = win[:, 512 + h0 * 64:512 + (h0 + 2) * 64]
        wv = win[:, 1024 + h0 * 128:1024 + (h0 + 2) * 128]
        wo = win[:, 2048 + h0 * 128:2048 + (h0 + 2) * 128]
        gi = win[:, 3072 + h0:3072 + h0 + 2]
        gf = win[:, 3080 + h0:3080 + h0 + 2]
        wtok = np.ascontiguousarray(np.concatenate([wk, wv, wo, gi, gf], axis=1))
        bg4 = np.concatenate([bgv[h0:h0 + 2], bgv[8 + h0:8 + h0 + 2]])
        maps.append({"hin": h1[b], "gn": _gain_layout(mlstm_norm[0]), "wq": np.ascontiguousarray(wq),
                     "wk": np.ascontiguousarray(wk), "wtok": wtok,
                     "bg": np.ascontiguousarray(np.tile(bg4[None, :], (128, 1))),
                     "hn": np.ascontiguousarray(np.tile(hnv[None, h0 * 128:(h0 + 2) * 128], (128, 1))),
                     "ident": c["ident"], "U": c["U"], "ones": c["ones"]})
    rc = _run("mlstm", maps)
    yT = [np.ascontiguousarray(np.concatenate([rc[b * 4 + hp]["y"] for hp in range(4)], axis=1).T)
          for b in range(B)]
    maps = []
    for core in range(NCORE):
        b, q = core // 4, core % 4
        sl = slice(q * 2048, (q + 1) * 2048)
        maps.append({"hin": np.ascontiguousarray(h1[b][sl]), "aT": np.ascontiguousarray(yT[b][:, sl]),
                     "wp": np.asarray(mlstm_w_out, f32)[0], "gn": _gain_layout(ffn_norm[1]),
                     "wgu": np.asarray(ffn_w_gate_up, f32)[1], "wd": np.asarray(ffn_w_down, f32)[1],
                     "ident": c["ident"], "fnw": np.asarray(final_norm, f32)})
    rd = _run("pf1", maps)
    out = np.stack([np.concatenate([rd[b * 4 + q]["hout"] for q in range(4)], axis=0) for b in range(B)])
    return out.astype(f32)
```

```python
import math
from contextlib import ExitStack

import numpy as np
import ml_dtypes

import concourse.bass as bass
import concourse.mybir as mybir
from concourse.bass_utils import run_bass_kernel_spmd

F32 = mybir.dt.float32
BF16 = mybir.dt.bfloat16
AF = mybir.ActivationFunctionType
ALU = mybir.AluOpType
AX = mybir.AxisListType

D = 1024
B = 2
S = 8192
NCORE = 8
DFF = 2816
EPS = 1e-6
NEG = -30000.0


class T:
    __slots__ = ("name", "w", "r", "psum")

    def __init__(self, name, psum=False):
        self.name = name
        self.w = None
        self.r = {}
        self.psum = psum


def _flat(x):
    out = []
    for a in x:
        if isinstance(a, (list, tuple)):
            out.extend(_flat(a))
        elif a is not None:
            out.append(a)
    return out


class Prog:
    ENGS = ("pe", "act", "dve", "pool", "sp")
    NRING = 8

    def __init__(self):
        self.nc = bass.Bass("TRN2", target_bir_lowering=False)
        self.ins = {e: [] for e in self.ENGS}
        self.ndma = {e: 0 for e in self.ENGS}
        self.dma_idx = {e: [] for e in self.ENGS}
        self.stack = ExitStack()
        self.final = []

    def sb(self, name, shape, dt):
        return self.stack.enter_context(self.nc.sbuf_tensor(name, list(shape), dt))

    def ps(self, name, shape, dt):
        return self.stack.enter_context(self.nc.psum_tensor(name, list(shape), dt))

    def dram_in(self, name, shape, dt):
        return self.nc.dram_tensor(name, list(shape), dt, kind="ExternalInput").ap()

    def dram_out(self, name, shape, dt):
        return self.nc.dram_tensor(name, list(shape), dt, kind="ExternalOutput").ap()

    def dram_tmp(self, name, shape, dt):
        return self.nc.dram_tensor(name, list(shape), dt).ap()

    COST0 = {"pe": 40.0, "act": 220.0, "dve": 120.0, "pool": 250.0, "sp": 60.0}
    COSTN = {"pe": 0.42, "act": 1.05, "dve": 0.8, "pool": 1.6, "sp": 0.0}

    def _emit(self, eng, fn, reads, writes, dma, n=128):
        reads, writes = _flat(reads), _flat(writes)
        lst = self.ins[eng]
        idx = len(lst)
        raw, other = set(), set()
        for t in reads:
            if t.w is not None:
                raw.add(t.w)
            if t.psum:
                for e2, i2 in t.r.items():
                    if e2 != eng:
                        other.add((e2, i2))
        for t in writes:
            if t.w is not None:
                other.add(t.w)
            for e2, i2 in t.r.items():
                other.add((e2, i2))
        deps, order = set(), set()

        def same_compute(d):
            return d[0] == eng and not dma and self.ins[eng][d[1]]["dma"] is None

        for d in raw:
            if same_compute(d) and eng == "pe":
                order.add(d[1])
            else:
                deps.add(d)
        for d in other:
            if same_compute(d):
                order.add(d[1])
            else:
                deps.add(d)
        deps.discard((eng, idx))
        cost = self.COST0[eng] + self.COSTN[eng] * n
        rec = dict(fn=fn, deps=deps, order=order, dma=None, cost=cost)
        if dma:
            rec["dma"] = -1
            rec["cost"] = 2000.0 + n * 0.01
        lst.append(rec)
        for t in reads:
            t.r[eng] = idx
        for t in writes:
            t.w = (eng, idx)
            t.r = {}
        return idx

    def op(self, eng, fn, reads=(), writes=(), n=128):
        return self._emit(eng, fn, reads, writes, False, n)

    def dma(self, eng, out, in_, reads=(), writes=(), final=False, n=65536):
        i = self._emit(eng, lambda e: e.dma_start(out=out, in_=in_), reads, writes, True, n)
        if final:
            self.final.append((eng, i))
        return i

    WINDOW = 48

    def schedule(self):
        ENGS = self.ENGS
        ins = self.ins
        n_tot = sum(len(ins[e]) for e in ENGS)
        done = {e: [None] * len(ins[e]) for e in ENGS}
        pend = {e: list(range(len(ins[e]))) for e in ENGS}
        free = {e: 0.0 for e in ENGS}
        neword = {e: [] for e in ENGS}
        count = 0
        while count < n_tot:
            best = None
            for e in ENGS:
                p = pend[e]
                lim = min(len(p), self.WINDOW)
                for k in range(lim):
                    i = p[k]
                    rec = ins[e][i]
                    ok = True
                    rt = free[e]
                    for o in rec["order"]:
                        if done[e][o] is None:
                            ok = False
                            break
                    if not ok:
                        continue
                    for (e2, i2) in rec["deps"]:
                        dt = done[e2][i2]
                        if dt is None:
                            ok = False
                            break
                        if dt > rt:
                            rt = dt
                    if not ok:
                        continue
                    key = (rt, k)
                    if best is None or key < best[0]:
                        best = (key, e, k, i, rt)
                    if rt <= free[e]:
                        break
            assert best is not None, "scheduler deadlock"
            _, e, k, i, rt = best
            rec = ins[e][i]
            if rec["dma"] is not None:
                free[e] = rt + 60.0
                done[e][i] = rt + rec["cost"]
            else:
                free[e] = rt + rec["cost"]
                done[e][i] = rt + rec["cost"] + 60.0
            pend[e].pop(k)
            neword[e].append(i)
            count += 1
        self.sim_time = max(max([x for x in done[e] if x is not None] + [0.0]) for e in ENGS)
        pos = {e: {old: new for new, old in enumerate(neword[e])} for e in ENGS}
        for e in ENGS:
            newl = []
            for old in neword[e]:
                rec = ins[e][old]
                rec["deps"] = {(e2, pos[e2][i2]) for (e2, i2) in rec["deps"]}
                newl.append(rec)
            ins[e] = newl
        self.final = [(e, pos[e][i]) for (e, i) in self.final]
        for e in ENGS:
            k = 0
            idxs = []
            for i, rec in enumerate(ins[e]):
                if rec["dma"] is not None:
                    rec["dma"] = k
                    if k >= self.NRING:
                        rec["deps"].add((e, idxs[k - self.NRING]))
                    idxs.append(i)
                    k += 1
            self.ndma[e] = k

    def build(self):
        nc = self.nc
        st = self.stack
        self.schedule()
        final_deps = set(self.final)
        needed = {e: set() for e in self.ENGS}
        for e in self.ENGS:
            for rec in self.ins[e]:
                for (e2, i2) in rec["deps"]:
                    needed[e2].add(i2)
        cnt_sem = {e: st.enter_context(nc.semaphore("c_" + e)) for e in self.ENGS}
        ring = {e: [st.enter_context(nc.semaphore("r_%s%d" % (e, i))) for i in range(self.NRING)]
                for e in self.ENGS if self.ndma[e] > 0}
        sig = {e: {} for e in self.ENGS}
        for e in self.ENGS:
            c = 0
            for i, rec in enumerate(self.ins[e]):
                if rec["dma"] is not None:
                    k = rec["dma"]
                    sig[e][i] = (ring[e][k % self.NRING], 16 * (k // self.NRING + 1))
                elif i in needed[e]:
                    c += 1
                    sig[e][i] = (cnt_sem[e], c)
        self.stats = {e: (len(self.ins[e]), self.ndma[e]) for e in self.ENGS}
        block = st.enter_context(nc.Block())
        handles = {"pe": block.tensor, "act": block.scalar, "dve": block.vector,
                   "pool": block.gpsimd, "sp": block.sync}

        def make(e):
            def body(eng):
                waited = {}
                for i, rec in enumerate(self.ins[e]):
                    for d in sorted(rec["deps"]):
                        sem, val = sig[d[0]][d[1]]
                        key = id(sem)
                        if waited.get(key, 0) >= val:
                            continue
                        waited[key] = val
                        eng.wait_ge(sem, val)
                    inst = rec["fn"](eng)
                    if i in sig[e]:
                        sem, val = sig[e][i]
                        inst.then_inc(sem, 16 if rec["dma"] is not None else 1)
                if e == "sp":
                    for d in sorted(final_deps):
                        sem, val = sig[d[0]][d[1]]
                        if waited.get(id(sem), 0) >= val:
                            continue
                        waited[id(sem)] = val
                        eng.wait_ge(sem, val)
            return body

        for e in self.ENGS:
            handles[e](make(e))
        st.close()
        return nc


def load_w_bf16(P, name, w_dram, kdim, ncols, col0=0, tile=None, tr=None):
    kc = kdim // 128
    if tile is None:
        tile = P.sb(name, [128, kc, ncols], BF16)
    src = w_dram[:, col0:col0 + ncols].rearrange("(c p) n -> p c n", p=128)
    step = max(1, kc // 4)
    trs = []
    for c0 in range(0, kc, step):
        c1 = min(kc, c0 + step)
        tr = T(name + str(c0))
        trs.append(tr)
        P.dma("pool", tile[:, c0:c1, :], src[:, c0:c1, :], writes=[tr])
    return tile, trs


class NormCtx:
    def __init__(self, P, gain_dram, ident, ident_t, pst, pst_t):
        self.P = P
        self.ident, self.ident_t = ident, ident_t
        self.pst, self.pst_t = pst, pst_t
        self.g = P.sb("ng_" + gain_dram.tensor.name, [128, 8], F32)
        self.g_t = T("ng")
        P.dma("sp", self.g[:, :], gain_dram[:, :], writes=[self.g_t])
        self.junk = P.sb("nj_" + gain_dram.tensor.name, [128, 1024], BF16)
        self.junk_t = T("nj")
        self.ss = [P.sb("nss%d_" % i + gain_dram.tensor.name, [128, 2], F32) for i in range(2)]
        self.ss_t = [T("nss%d" % i) for i in range(2)]
        self.xs = [P.sb("nxs%d_" % i + gain_dram.tensor.name, [128, 1024], BF16) for i in range(2)]
        self.xs_t = [T("nxs%d" % i) for i in range(2)]
        self.k = 0
        self.dst_full = None
        self.lnexp = False
        self.act_scale = False

    def run(self, h_ap, h_t, dst_fn, dst_t):
        P = self.P
        k = self.k
        self.k += 1
        ss, ss_t = self.ss[k % 2], self.ss_t[k % 2]
        xs, xs_t = self.xs[k % 2], self.xs_t[k % 2]
        pst, pst_t = self.pst[k % len(self.pst)], self.pst_t[k % len(self.pst)]
        junk, junk_t = self.junk, self.junk_t
        P.op("act", lambda e: e.activation(out=junk[:, :], in_=h_ap, func=AF.Square,
                                           accum_out=ss[:, 0:1]),
             reads=[h_t], writes=[junk_t, ss_t], n=1024)
        P.op("dve", lambda e: e.tensor_scalar(out=ss[:, 1:2], in0=ss[:, 0:1], scalar1=1.0 / D,
                                              scalar2=EPS, op0=ALU.mult, op1=ALU.add),
             reads=[ss_t], writes=[ss_t])
        if self.lnexp:
            P.op("act", lambda e: e.activation(out=ss[:, 1:2], in_=ss[:, 1:2], func=AF.Ln),
                 reads=[ss_t], writes=[ss_t])
            P.op("act", lambda e: e.activation(out=ss[:, 1:2], in_=ss[:, 1:2], func=AF.Exp, scale=-0.5),
                 reads=[ss_t], writes=[ss_t])
        else:
            P.op("act", lambda e: e.activation(out=ss[:, 1:2], in_=ss[:, 1:2], func=AF.Sqrt),
                 reads=[ss_t], writes=[ss_t])
            P.op("dve", lambda e: e.reciprocal(out=ss[:, 1:2], in_=ss[:, 1:2]),
                 reads=[ss_t], writes=[ss_t])
        if self.act_scale:
            P.op("act", lambda e: e.activation(out=xs[:, :], in_=h_ap, func=AF.Copy, scale=ss[:, 1:2]),
                 reads=[h_t, ss_t], writes=[xs_t], n=1024)
        else:
            P.op("dve", lambda e: e.tensor_scalar(out=xs[:, :], in0=h_ap, scalar1=ss[:, 1:2],
                                                  scalar2=None, op0=ALU.mult),
                 reads=[h_t, ss_t], writes=[xs_t], n=1024)
        for c in range(8):
            P.op("pe", lambda e, c=c: e.transpose(out=pst[:, c, :], in_=xs[:, c * 128:(c + 1) * 128],
                                                  identity=self.ident[:, :]),
                 reads=[xs_t, self.ident_t], writes=[pst_t])
        g = self.g
        if self.dst_full is not None:
            dfull = self.dst_full(k)
            P.op("dve", lambda e: e.tensor_tensor(out=dfull, in0=pst[:, :, :],
                                                  in1=g[:, :].unsqueeze(2).to_broadcast([128, 8, 128]),
                                                  op=ALU.mult),
                 reads=[pst_t, self.g_t], writes=[dst_t], n=1024)
        else:
            for c in range(8):
                P.op("act", lambda e, c=c: e.activation(out=dst_fn(c), in_=pst[:, c, :], func=AF.Copy,
                                                        scale=g[:, c:c + 1]),
                     reads=[pst_t, self.g_t], writes=[dst_t])


def make_ident(P, ident_dram):
    ident = P.sb("ident_sb", [128, 128], BF16)
    ident_t = T("ident")
    P.dma("sp", ident[:, :], ident_dram[:, :], writes=[ident_t])
    return ident, ident_t


FFN_PARTS = [(0, 5), (5, 5), (10, 4), (14, 4), (18, 4)]


def build_projffn(final):
    P = Prog()
    NT = 16
    hin = P.dram_in("hin", [2048, D], F32)
    aT = P.dram_in("aT", [D, 2048], BF16)
    wp = P.dram_in("wp", [D, D], F32)
    gn = P.dram_in("gn", [128, 8], F32)
    wgu = P.dram_in("wgu", [D, 2 * DFF], F32)
    wd = P.dram_in("wd", [DFF, D], F32)
    identd = P.dram_in("ident", [128, 128], BF16)
    if final:
        fn = P.dram_in("fnw", [D], F32)
    hout = P.dram_out("hout", [2048, D], F32)

    ident, ident_t = make_ident(P, identd)
    h = P.sb("h", [128, NT, D], F32)
    h_t = [T("h%d" % i) for i in range(NT)]
    xnT = P.sb("xnT", [128, 8, 2048], BF16)
    xn_t = [T("xn%d" % i) for i in range(NT)]
    wps, wps_t = load_w_bf16(P, "wps", wp, D, D)
    at = [P.sb("at%d" % i, [128, 8, 128], BF16) for i in range(2)]
    at_t = [T("at%d" % i) for i in range(2)]
    pf = [P.ps("pf%d" % i, [128, 512], F32) for i in range(6)]
    pf_t = [T("pf%d" % i, True) for i in range(6)]
    pb = [P.ps("pb%d" % i, [128, 8, 128], BF16) for i in range(2)]
    pb_t = [T("pb%d" % i, True) for i in range(2)]
    norm = NormCtx(P, gn, ident, ident_t, pb, pb_t)
    norm.dst_full = lambda k: xnT[:, :, k * 128:(k + 1) * 128]
    pfk = [0]

    def next_pf():
        i = pfk[0] % 6
        pfk[0] += 1
        return pf[i], pf_t[i]

    for t in range(NT):
        P.dma("sp", h[:, t, :], hin[t * 128:(t + 1) * 128, :], writes=[h_t[t]])
        a, a_t = at[t % 2], at_t[t % 2]
        P.dma("sp", a[:, :, :], aT[:, t * 128:(t + 1) * 128].rearrange("(c p) n -> p c n", p=128),
              writes=[a_t])
        for nh in range(2):
            ps, ps_t = next_pf()
            for c in range(8):
                P.op("pe", lambda e, c=c, ps=ps, a=a, nh=nh: e.matmul(
                    ps[:, :], lhsT=a[:, c, :], rhs=wps[:, c, nh * 512:(nh + 1) * 512],
                    start=(c == 0), stop=(c == 7)),
                    reads=[a_t, wps_t], writes=[ps_t], n=512)
            P.op("dve", lambda e, ps=ps, t=t, nh=nh: e.tensor_tensor(
                out=h[:, t, nh * 512:(nh + 1) * 512], in0=h[:, t, nh * 512:(nh + 1) * 512],
                in1=ps[:, :], op=ALU.add),
                reads=[ps_t, h_t[t]], writes=[h_t[t]], n=512)
        norm.run(h[:, t, :], h_t[t], lambda c, t=t: xnT[:, c, t * 128:(t + 1) * 128], xn_t[t])

    wg_b = [P.sb("wg%d" % i, [128, 8, 640], BF16) for i in range(2)]
    wu_b = [P.sb("wu%d" % i, [128, 8, 640], BF16) for i in range(2)]
    wd_b = [P.sb("wd%d" % i, [128, 5, 1024], BF16) for i in range(2)]
    wg_t = [[T("wg%d_%d" % (i, q)) for q in range(4)] for i in range(2)]
    wu_t = [[T("wu%d_%d" % (i, q)) for q in range(4)] for i in range(2)]
    wd_t = [[T("wd%d_%d" % (i, q)) for q in range(3)] for i in range(2)]
    sg = [P.sb("sg%d" % i, [128, 512], F32) for i in range(2)]
    sg_t = [T("sg%d" % i) for i in range(2)]
    aF = [P.sb("aF%d" % i, [128, 5, 512], BF16) for i in range(2)]
    aF_t = [T("aF%d" % i) for i in range(2)]
    kk = 0
    for pi, (c0, ncn) in enumerate(FFN_PARTS):
        b = pi % 2
        ncols = ncn * 128
        srcg = wgu[:, c0 * 128:c0 * 128 + ncols].rearrange("(c p) n -> p c n", p=128)
        srcu = wgu[:, DFF + c0 * 128:DFF + c0 * 128 + ncols].rearrange("(c p) n -> p c n", p=128)
        for q in range(4):
            P.dma("pool", wg_b[b][:, 2 * q:2 * q + 2, 0:ncols], srcg[:, 2 * q:2 * q + 2, :], writes=[wg_t[b][q]])
            P.dma("pool", wu_b[b][:, 2 * q:2 * q + 2, 0:ncols], srcu[:, 2 * q:2 * q + 2, :], writes=[wu_t[b][q]])
        srcd = wd[c0 * 128:(c0 + ncn) * 128, :].rearrange("(c p) n -> p c n", p=128)
        for q in range(0, ncn, 2):
            q1 = min(ncn, q + 2)
            P.dma("pool", wd_b[b][:, q:q1, :], srcd[:, q:q1, :], writes=[wd_t[b][q // 2]])
        for tg in range(4):
            af, af_t = aF[tg % 2], aF_t[tg % 2]
            for j in range(ncn):
                psg, psg_t = next_pf()
                psu, psu_t = next_pf()
                for c in range(8):
                    P.op("pe", lambda e, c=c, j=j, psg=psg, b=b, tg=tg: e.matmul(
                        psg[:, :], lhsT=wg_b[b][:, c, j * 128:(j + 1) * 128],
                        rhs=xnT[:, c, tg * 512:(tg + 1) * 512], start=(c == 0), stop=(c == 7)),
                        reads=[wg_t[b]] + xn_t[tg * 4:tg * 4 + 4], writes=[psg_t], n=512)
                for c in range(8):
                    P.op("pe", lambda e, c=c, j=j, psu=psu, b=b, tg=tg: e.matmul(
                        psu[:, :], lhsT=wu_b[b][:, c, j * 128:(j + 1) * 128],
                        rhs=xnT[:, c, tg * 512:(tg + 1) * 512], start=(c == 0), stop=(c == 7)),
                        reads=[wu_t[b]] + xn_t[tg * 4:tg * 4 + 4], writes=[psu_t], n=512)
                s, s_t = sg[kk % 2], sg_t[kk % 2]
                kk += 1
                P.op("act", lambda e, s=s, psg=psg: e.activation(out=s[:, :], in_=psg[:, :], func=AF.Silu),
                     reads=[psg_t], writes=[s_t], n=512)
                P.op("dve", lambda e, s=s, psu=psu, af=af, j=j: e.tensor_tensor(
                    out=af[:, j, :], in0=s[:, :], in1=psu[:, :], op=ALU.mult),
                    reads=[s_t, psu_t], writes=[af_t], n=512)
            for tt in range(4):
                t = tg * 4 + tt
                for nh in range(2):
                    ps, ps_t = next_pf()
                    for j in range(ncn):
                        P.op("pe", lambda e, j=j, ps=ps, af=af, tt=tt, nh=nh, b=b: e.matmul(
                            ps[:, :], lhsT=af[:, j, tt * 128:(tt + 1) * 128],
                            rhs=wd_b[b][:, j, nh * 512:(nh + 1) * 512],
                            start=(j == 0), stop=(j == ncn - 1)),
                            reads=[af_t, wd_t[b]], writes=[ps_t], n=512)
                    P.op("dve", lambda e, ps=ps, t=t, nh=nh: e.tensor_tensor(
                        out=h[:, t, nh * 512:(nh + 1) * 512], in0=h[:, t, nh * 512:(nh + 1) * 512],
                        in1=ps[:, :], op=ALU.add),
                        reads=[ps_t, h_t[t]], writes=[h_t[t]], n=512)

    if final:
        fw = P.sb("fw", [128, D], F32)
        fw_t = T("fw")
        P.dma("sp", fw[:, :], fn.partition_broadcast(128), writes=[fw_t])
        ss = P.sb("fss", [128, 2 * NT], F32)
        ss_t = T("fss")
        junk = norm.junk
        for t in range(NT):
            P.op("act", lambda e, t=t: e.activation(out=junk[:, :], in_=h[:, t, :], func=AF.Square,
                                                    accum_out=ss[:, 2 * t:2 * t + 1]),
                 reads=[h_t[t]], writes=[norm.junk_t, ss_t])
            P.op("dve", lambda e, t=t: e.tensor_scalar(out=ss[:, 2 * t + 1:2 * t + 2], in0=ss[:, 2 * t:2 * t + 1],
                                                       scalar1=1.0 / D, scalar2=EPS, op0=ALU.mult, op1=ALU.add),
                 reads=[ss_t], writes=[ss_t])
            P.op("act", lambda e, t=t: e.activation(out=ss[:, 2 * t + 1:2 * t + 2],
                                                    in_=ss[:, 2 * t + 1:2 * t + 2], func=AF.Sqrt),
                 reads=[ss_t], writes=[ss_t])
            P.op("dve", lambda e, t=t: e.reciprocal(out=ss[:, 2 * t + 1:2 * t + 2],
                                                    in_=ss[:, 2 * t + 1:2 * t + 2]),
                 reads=[ss_t], writes=[ss_t])
            P.op("dve", lambda e, t=t: e.scalar_tensor_tensor(
                out=h[:, t, :], in0=h[:, t, :], scalar=ss[:, 2 * t + 1:2 * t + 2], in1=fw[:, :],
                op0=ALU.mult, op1=ALU.mult),
                reads=[h_t[t], ss_t, fw_t], writes=[h_t[t]])
    for t in range(NT):
        P.dma("sp", hout[t * 128:(t + 1) * 128, :], h[:, t, :], reads=[h_t[t]], writes=[T("o%d" % t)], final=True)
    return P.build()


def build_mlstm():
    P = Prog()
    NCH = S // 128
    hin = P.dram_in("hin", [S, D], F32)
    gn = P.dram_in("gn", [128, 8], F32)
    wt_d = P.dram_in("wtok", [D, 772], F32)
    bg_d = P.dram_in("bg", [128, 4], F32)
    hn_d = P.dram_in("hn", [128, 256], F32)
    identd = P.dram_in("ident", [128, 128], BF16)
    U_d = P.dram_in("U", [128, 128], F32)
    ones_d = P.dram_in("ones", [128, 128], F32)
    yout = P.dram_out("y", [S, 256], BF16)

    ident, ident_t = make_ident(P, identd)
    U = P.sb("U_sb", [128, 128], F32)
    ONES = P.sb("ones_sb", [128, 128], F32)
    bg = P.sb("bg_sb", [128, 4], F32)
    hn = P.sb("hn_sb", [128, 256], F32)
    c_t = T("consts")
    P.dma("sp", U[:, :], U_d[:, :], writes=[c_t])
    c2_t = T("consts2")
    P.dma("sp", ONES[:, :], ones_d[:, :], writes=[c2_t])
    c3_t = T("consts3")
    P.dma("sp", bg[:, :], bg_d[:, :], writes=[c3_t])
    c4_t = T("consts4")
    P.dma("sp", hn[:, :], hn_d[:, :], writes=[c4_t])
    wt, wt_t = load_w_bf16(P, "wt_sb", wt_d, D, 772)

    def dbl(name, shape, dt):
        return [P.sb("%s%d" % (name, i), shape, dt) for i in range(2)], [T("%s%d" % (name, i)) for i in range(2)]

    xin, xin_t = dbl("xin", [128, D], F32)
    xnT, xnT_t = dbl("xnT", [128, 8, 128], BF16)
    qT, qT_t = dbl("qT", [64, 2, 128], BF16)
    kT, kT_t = dbl("kT", [64, 2, 128], BF16)
    ktok, ktok_t = dbl("ktok", [128, 128], BF16)
    og, og_t = dbl("og", [128, 256], F32)
    gt, gt_t = dbl("gt", [128, 24], F32)
    V1 = [dbl("V1_%d_" % h, [128, 129], BF16) for h in range(2)]
    V2 = [dbl("V2_%d_" % h, [128, 129], BF16) for h in range(2)]
    PT, PT_t = dbl("PT", [128, 128], BF16)
    hh, hh_t = dbl("hh", [128, 128], F32)
    sq = P.sb("sqj", [128, 128], BF16)
    sq_t = T("sqj")
    st, st_t = dbl("st", [128, 8], F32)
    yt, yt_t = dbl("yt", [128, 256], BF16)
    C = [P.sb("C%d" % h, [64, 129], F32) for h in range(2)]
    C_t = [T("C%d" % h) for h in range(2)]
    Cb = [dbl("Cb%d_" % h, [64, 129], BF16) for h in range(2)]

    pb = [P.ps("pbT", [128, 8, 128], BF16)]
    pb_t = [T("pbT", True)]
    pq = P.ps("pq", [128, 8, 128], BF16); pq_t = T("pq", True)
    qtok, qtok_t = dbl("qtok", [128, 128], BF16)
    p1 = P.ps("p1", [128, 512], F32); p1_t = T("p1", True)
    p2 = P.ps("p2", [128, 512], F32); p2_t = T("p2", True)
    pg = P.ps("pg", [128, 512], F32); pg_t = T("pg", True)
    pS = P.ps("pS", [128, 512], F32); pS_t = T("pS", True)
    pO = P.ps("pO", [128, 512], F32); pO_t = T("pO", True)
    pA = P.ps("pA", [128, 512], F32); pA_t = T("pA", True)
    norm = NormCtx(P, gn, ident, ident_t, pb, pb_t)
    norm.lnexp = True
    norm.dst_full = lambda k: xnT[k % 2][:, :, :]

    for h in range(2):
        P.op("dve", lambda e, h=h: e.memset(C[h][:, :], 0.0), writes=[C_t[h]])
        P.op("dve", lambda e, h=h: e.memset(Cb[h][0][0][:, :], 0.0), writes=[Cb[h][1][0]])

    for j in range(NCH):
        b = j % 2
        P.dma("sp", xin[b][:, :], hin[j * 128:(j + 1) * 128, :], writes=[xin_t[b]])
        norm.run(xin[b][:, :], xin_t[b], lambda c, b=b: xnT[b][:, c, :], xnT_t[b])
        for c in range(8):
            P.op("pe", lambda e, c=c, b=b: e.matmul(p1[:, 0:512], lhsT=xnT[b][:, c, :], rhs=wt[:, c, 0:512],
                                                    start=(c == 0), stop=(c == 7)),
                 reads=[wt_t, xnT_t[b]], writes=[p1_t], n=512)
        for c in range(8):
            P.op("pe", lambda e, c=c, b=b: e.matmul(p2[:, 0:260], lhsT=xnT[b][:, c, :], rhs=wt[:, c, 512:772],
                                                    start=(c == 0), stop=(c == 7)),
                 reads=[wt_t, xnT_t[b]], writes=[p2_t], n=260)
        P.op("act", lambda e, b=b: e.activation(out=ktok[b][:, :], in_=p1[:, 0:128], func=AF.Copy),
             reads=[p1_t], writes=[ktok_t[b]])
        P.op("act", lambda e, b=b: e.activation(out=qtok[b][:, :], in_=p1[:, 384:512], func=AF.Copy, scale=0.125),
             reads=[p1_t], writes=[qtok_t[b]])
        for h in range(2):
            P.op("pe", lambda e, h=h, b=b: e.transpose(out=pq[0:64, h, :], in_=qtok[b][:, h * 64:(h + 1) * 64],
                                                       identity=ident[:, :]),
                 reads=[qtok_t[b], ident_t], writes=[pq_t], n=64)
            P.op("pe", lambda e, h=h, b=b: e.transpose(out=pq[0:64, 2 + h, :], in_=ktok[b][:, h * 64:(h + 1) * 64],
                                                       identity=ident[:, :]),
                 reads=[ktok_t[b], ident_t], writes=[pq_t], n=64)
        P.op("act", lambda e, b=b: e.activation(out=qT[b][:, :, :], in_=pq[0:64, 0:2, :], func=AF.Copy),
             reads=[pq_t], writes=[qT_t[b]], n=256)
        P.op("dve", lambda e, b=b: e.tensor_copy(out=kT[b][:, :, :], in_=pq[0:64, 2:4, :]),
             reads=[pq_t], writes=[kT_t[b]], n=256)
        g = gt[b]
        g_t = gt_t[b]
        P.op("dve", lambda e, g=g: e.tensor_tensor(out=g[:, 0:4], in0=p2[:, 256:260], in1=bg[:, :], op=ALU.add),
             reads=[p2_t, c3_t], writes=[g_t])
        P.op("act", lambda e, g=g: e.activation(out=g[:, 4:8], in_=g[:, 0:4], func=AF.Tanh, scale=1.0 / 15.0),
             reads=[g_t], writes=[g_t])
        P.op("act", lambda e, g=g: e.activation(out=g[:, 8:10], in_=g[:, 6:8], func=AF.Exp, scale=-15.0),
             reads=[g_t], writes=[g_t])
        P.op("act", lambda e, g=g: e.activation(out=g[:, 10:12], in_=g[:, 8:10], func=AF.Ln, bias=1.0),
             reads=[g_t], writes=[g_t])
        P.op("pe", lambda e, g=g: e.matmul(pg[:, 0:2], lhsT=U[:, :], rhs=g[:, 10:12], start=True, stop=True),
             reads=[g_t, c_t], writes=[pg_t])
        P.op("pe", lambda e, g=g: e.matmul(pg[:, 2:4], lhsT=ONES[:, :], rhs=g[:, 10:12], start=True, stop=True),
             reads=[g_t, c2_t], writes=[pg_t])
        P.op("dve", lambda e, g=g: e.tensor_copy(out=g[:, 12:16], in_=pg[:, 0:4]), reads=[pg_t], writes=[g_t])
        P.op("dve", lambda e, g=g: e.tensor_tensor(out=g[:, 16:18], in0=g[:, 12:14], in1=g[:, 14:16],
                                                   op=ALU.subtract),
             reads=[g_t], writes=[g_t])
        s = st[b]
        s_t = st_t[b]
        for h in range(2):
            P.op("act", lambda e, g=g, h=h: e.activation(out=g[:, 18 + h:19 + h], in_=g[:, 4 + h:5 + h], func=AF.Exp,
                                                         scale=15.0, bias=g[:, 12 + h:13 + h]),
                 reads=[g_t], writes=[g_t])
            P.op("act", lambda e, g=g, h=h: e.activation(out=g[:, 20 + h:21 + h], in_=g[:, 4 + h:5 + h], func=AF.Exp,
                                                         scale=15.0, bias=g[:, 16 + h:17 + h]),
                 reads=[g_t], writes=[g_t])
        P.op("act", lambda e, g=g: e.activation(out=g[:, 22:24], in_=g[:, 12:14], func=AF.Exp, scale=-1.0),
             reads=[g_t], writes=[g_t])
        P.op("act", lambda e, g=g, s=s: e.activation(out=s[:, 6:8], in_=g[:, 14:16], func=AF.Exp, scale=-1.0),
             reads=[g_t], writes=[s_t])
        P.op("act", lambda e, b=b: e.activation(out=og[b][:, :], in_=p2[:, 0:256], func=AF.Exp, scale=-1.0),
             reads=[p2_t], writes=[og_t[b]], n=256)
        P.op("pool", lambda e, b=b: e.tensor_scalar_add(out=og[b][:, :], in0=og[b][:, :], scalar1=1.0),
             reads=[og_t[b]], writes=[og_t[b]], n=256)
        P.op("dve", lambda e, b=b: e.reciprocal(out=og[b][:, :], in_=og[b][:, :]),
             reads=[og_t[b]], writes=[og_t[b]], n=256)
        for h in range(2):
            for (V, col) in ((V1[h], 18 + h), (V2[h], 20 + h)):
                Vb, Vb_t = V[0][b], V[1][b]
                P.op("dve", lambda e, Vb=Vb, g=g, col=col, h=h: e.tensor_scalar(
                    out=Vb[:, 0:128], in0=p1[:, 128 + h * 128:256 + h * 128], scalar1=g[:, col:col + 1],
                    scalar2=None, op0=ALU.mult),
                    reads=[p1_t, g_t], writes=[Vb_t])
                P.op("pool", lambda e, Vb=Vb, g=g, col=col: e.tensor_copy(out=Vb[:, 128:129], in_=g[:, col:col + 1]),
                     reads=[g_t], writes=[Vb_t], n=1)
        for h in range(2):
            Cb_cur, Cb_cur_t = Cb[h][0][b], Cb[h][1][b]
            Cb_nxt, Cb_nxt_t = Cb[h][0][1 - b], Cb[h][1][1 - b]
            V1b, V1b_t = V1[h][0][b], V1[h][1][b]
            V2b, V2b_t = V2[h][0][b], V2[h][1][b]
            P.op("pe", lambda e, h=h, b=b: e.matmul(pS[:, 0:128], lhsT=kT[b][:, h, :], rhs=qT[b][:, h, :],
                                                    start=True, stop=True),
                 reads=[kT_t[b], qT_t[b]], writes=[pS_t])
            pt, pt_t = PT[h], PT_t[h]
            P.op("dve", lambda e, pt=pt: e.tensor_tensor(out=pt[:, :], in0=pS[:, 0:128], in1=U[:, :], op=ALU.mult),
                 reads=[pS_t, c_t], writes=[pt_t])
            P.op("pe", lambda e, h=h, b=b, Cb_cur=Cb_cur: e.matmul(pO[:, 0:129], lhsT=qT[b][:, h, :], rhs=Cb_cur[:, :],
                                                                   start=True, stop=False),
                 reads=[qT_t[b], Cb_cur_t], writes=[pO_t])
            P.op("pe", lambda e, pt=pt, V1b=V1b: e.matmul(pO[:, 0:129], lhsT=pt[:, :], rhs=V1b[:, :],
                                                          start=False, stop=True),
                 reads=[pt_t, V1b_t], writes=[pO_t])
            o = 3 * h
            P.op("dve", lambda e, s=s, g=g, h=h, o=o: e.tensor_tensor(out=s[:, o:o + 1], in0=pO[:, 128:129],
                                                                      in1=g[:, 22 + h:23 + h], op=ALU.mult),
                 reads=[pO_t, g_t], writes=[s_t])
            P.op("dve", lambda e, s=s, o=o: e.tensor_scalar(out=s[:, o + 1:o + 2], in0=s[:, o:o + 1], scalar1=-1.0,
                                                            scalar2=1.0, op0=ALU.mult, op1=ALU.max),
                 reads=[s_t], writes=[s_t])
            P.op("dve", lambda e, s=s, o=o: e.tensor_tensor(out=s[:, o:o + 1], in0=s[:, o:o + 1],
                                                            in1=s[:, o + 1:o + 2], op=ALU.max),
                 reads=[s_t], writes=[s_t])
            P.op("dve", lambda e, s=s, o=o: e.reciprocal(out=s[:, o:o + 1], in_=s[:, o:o + 1]),
                 reads=[s_t], writes=[s_t])
            P.op("dve", lambda e, s=s, g=g, h=h, o=o: e.tensor_tensor(out=s[:, o + 1:o + 2], in0=g[:, 22 + h:23 + h],
                                                                      in1=s[:, o:o + 1], op=ALU.mult),
                 reads=[s_t, g_t], writes=[s_t])
            hb, hb_t = hh[h], hh_t[h]
            P.op("dve", lambda e, hb=hb, s=s, o=o: e.tensor_scalar(out=hb[:, :], in0=pO[:, 0:128],
                                                                   scalar1=s[:, o + 1:o + 2], scalar2=None, op0=ALU.mult),
                 reads=[pO_t, s_t], writes=[hb_t])
            P.op("act", lambda e, hb=hb, s=s, o=o: e.activation(out=sq[:, :], in_=hb[:, :], func=AF.Square,
                                                                accum_out=s[:, o + 2:o + 3]),
                 reads=[hb_t], writes=[sq_t, s_t])
            P.op("dve", lambda e, s=s, o=o: e.tensor_scalar(out=s[:, o + 2:o + 3], in0=s[:, o + 2:o + 3],
                                                            scalar1=1.0 / 128.0, scalar2=EPS, op0=ALU.mult, op1=ALU.add),
                 reads=[s_t], writes=[s_t])
            P.op("act", lambda e, s=s, o=o: e.activation(out=s[:, o + 2:o + 3], in_=s[:, o + 2:o + 3], func=AF.Ln),
                 reads=[s_t], writes=[s_t])
            P.op("act", lambda e, s=s, o=o: e.activation(out=s[:, o + 2:o + 3], in_=s[:, o + 2:o + 3], func=AF.Exp,
                                                         scale=-0.5),
                 reads=[s_t], writes=[s_t])
            P.op("dve", lambda e, hb=hb, s=s, o=o, h=h: e.scalar_tensor_tensor(
                out=hb[:, :], in0=hb[:, :], scalar=s[:, o + 2:o + 3], in1=hn[:, h * 128:(h + 1) * 128],
                op0=ALU.mult, op1=ALU.mult),
                reads=[hb_t, s_t, c4_t], writes=[hb_t])
            P.op("dve", lambda e, hb=hb, h=h, b=b: e.tensor_tensor(out=yt[b][:, h * 128:(h + 1) * 128], in0=hb[:, :],
                                                                   in1=og[b][:, h * 128:(h + 1) * 128], op=ALU.mult),
                 reads=[hb_t, og_t[b]], writes=[yt_t[b]])
            P.op("pe", lambda e, h=h, b=b, V2b=V2b: e.matmul(pA[0:64, 0:129], lhsT=ktok[b][:, h * 64:(h + 1) * 64],
                                                             rhs=V2b[:, :], start=True, stop=True),
                 reads=[ktok_t[b], V2b_t], writes=[pA_t])
            P.op("dve", lambda e, h=h, s=s: e.scalar_tensor_tensor(
                out=C[h][:, :], in0=C[h][:, :], scalar=s[0:64, 6 + h:7 + h], in1=pA[0:64, 0:129],
                op0=ALU.mult, op1=ALU.add),
                reads=[C_t[h], s_t, pA_t], writes=[C_t[h]])
            P.op("pool", lambda e, h=h, Cb_nxt=Cb_nxt: e.tensor_copy(out=Cb_nxt[:, :], in_=C[h][:, :]),
                 reads=[C_t[h]], writes=[Cb_nxt_t], n=129)
        P.dma("sp", yout[j * 128:(j + 1) * 128, :], yt[b][:, :], reads=[yt_t[b]], writes=[T("yo%d" % j)], final=True)
    return P.build()


def build_attn():
    P = Prog()
    NT = S // 128
    xin_d = P.dram_in("xin", [S, D], F32)
    gn = P.dram_in("gn", [128, 8], F32)
    w_d = P.dram_in("wqkv", [D, 768], F32)
    cs_d = P.dram_in("cs", [S, 128], F32)
    identd = P.dram_in("ident", [128, 128], BF16)
    identf_d = P.dram_in("identf", [128, 128], F32)
    oneh_d = P.dram_in("onehot", [32, S], BF16)
    tri_d = P.dram_in("tri", [128, 128], BF16)
    M1_d = P.dram_in("M1", [128, 1024], F32)
    A30_d = P.dram_in("A30", [128, 1024], F32)
    Bc_d = P.dram_in("Bc", [128, 1024], F32)
    oT = P.dram_out("oT", [256, S], BF16)
    qTs = P.dram_tmp("qTs", [4, 64, S], BF16)
    kTs = P.dram_tmp("kTs", [4, 64, S], BF16)
    vs = P.dram_tmp("vs", [S, 256], BF16)

    ident, ident_t = make_ident(P, identd)
    identf = P.sb("identf_sb", [128, 128], F32)
    tri = P.sb("tri_sb", [128, 128], BF16)
    M1 = P.sb("M1_sb", [128, 1024], F32)
    A30 = P.sb("A30_sb", [128, 1024], F32)
    Bc = P.sb("Bc_sb", [128, 1024], F32)
    cst_t = []
    for dst, src in ((identf, identf_d), (tri, tri_d), (M1, M1_d), (A30, A30_d), (Bc, Bc_d)):
        t = T("c")
        cst_t.append(t)
        P.dma("sp", dst[:, :], src[:, :], writes=[t])
    w, w_t = load_w_bf16(P, "w_sb", w_d, D, 768)

    KA = P.sb("KA", [128, S], BF16)
    QA = P.sb("QA", [128, S], BF16)
    VA = P.sb("VA", [128, NT, 128], BF16)
    qabs = P.sb("qabs", [64, S], BF16)
    KAoh_t = T("KAoh")
    for q4 in range(4):
        P.dma("sp", KA[64:96, q4 * 2048:(q4 + 1) * 2048], oneh_d[:, q4 * 2048:(q4 + 1) * 2048], writes=[KAoh_t])
    VA1_t = T("VA1")
    P.op("pool", lambda e: e.memset(VA[:, :, 64:128], 1.0), writes=[VA1_t])

    def dbl(name, shape, dt):
        return [P.sb("%s%d" % (name, i), shape, dt) for i in range(2)], [T("%s%d" % (name, i)) for i in range(2)]

    xin, xin_t = dbl("xin_sb", [128, D], F32)
    cs, cs_t = dbl("cs_sb", [128, 128], F32)
    xnT, xnT_t = dbl("xnT", [128, 8, 128], BF16)
    qk32, qk32_t = dbl("qk32", [128, 512], F32)
    rt, rt_t = dbl("rt", [128, 4, 64], F32)
    qkb, qkb_t = dbl("qkb", [128, 512], BF16)
    vb, vb_t = dbl("vb", [128, 256], BF16)
    qkT, qkT_t = dbl("qkT", [64, 8, 128], BF16)
    ksum, ksum_t = dbl("ksum", [64, 4], F32)
    kms = P.sb("kms", [64, 4, 32], F32)
    kms_t = T("kms")
    kmeanb = P.sb("kmeanb", [64, 4, 32], BF16)
    kmeanb_t = T("kmeanb")

    pb = [P.ps("pbT", [128, 8, 128], BF16)]
    pb_t = [T("pbT", True)]
    pqT = P.ps("pqT", [128, 8, 128], BF16); pqT_t = T("pqT", True)
    pqk = P.ps("pqk", [128, 512], F32); pqk_t = T("pqk", True)
    pv = P.ps("pv", [128, 512], F32); pv_t = T("pv", True)
    pS = [P.ps("pS%d" % i, [128, 2, 512], F32) for i in range(2)]
    pS_t = [T("pS%d" % i, True) for i in range(2)]
    pO = [pqk, pqk]
    pO_t = [pqk_t, pqk_t]
    norm = NormCtx(P, gn, ident, ident_t, pb, pb_t)
    norm.dst_full = lambda k: xnT[k % 2][:, :, :]
    norm.act_scale = True
    kmx = P.sb("kmx", [64, 4], F32)
    kmx_t = T("kmx")
    kmt, kmt_t = dbl("kmt", [64, 4], F32)

    scr_t = []
    for j in range(NT):
        b = j % 2
        P.dma("sp", xin[b][:, :], xin_d[j * 128:(j + 1) * 128, :], writes=[xin_t[b]])
        P.dma("sp", cs[b][:, :], cs_d[j * 128:(j + 1) * 128, :], writes=[cs_t[b]])
        norm.run(xin[b][:, :], xin_t[b], lambda c, b=b: xnT[b][:, c, :], xnT_t[b])
        for c in range(8):
            P.op("pe", lambda e, c=c, b=b: e.matmul(pqk[:, :], lhsT=xnT[b][:, c, :], rhs=w[:, c, 0:512],
                                                    start=(c == 0), stop=(c == 7)),
                 reads=[w_t, xnT_t[b]], writes=[pqk_t], n=512)
        for c in range(8):
            P.op("pe", lambda e, c=c, b=b: e.matmul(pv[:, 0:256], lhsT=xnT[b][:, c, :], rhs=w[:, c, 512:768],
                                                    start=(c == 0), stop=(c == 7)),
                 reads=[w_t, xnT_t[b]], writes=[pv_t], n=256)
        P.op("act", lambda e, b=b: e.activation(out=qk32[b][:, 0:256], in_=pqk[:, 0:256], func=AF.Copy, scale=0.125),
             reads=[pqk_t], writes=[qk32_t[b]])
        P.op("act", lambda e, b=b: e.activation(out=qk32[b][:, 256:512], in_=pqk[:, 256:512], func=AF.Copy),
             reads=[pqk_t], writes=[qk32_t[b]])
        P.op("dve", lambda e, b=b: e.tensor_copy(out=vb[b][:, :], in_=pv[:, 0:256]), reads=[pv_t], writes=[vb_t[b]])
        tv = T("vs%d" % j)
        scr_t.append(tv)
        P.dma("sp", vs[j * 128:(j + 1) * 128, :], vb[b][:, :], reads=[vb_t[b]], writes=[tv])
        qv = qk32[b][:, :].rearrange("p (g d) -> p g d", d=64)
        x1, x2 = qv[:, :, 0:8], qv[:, :, 8:16]
        cosv = cs[b][:, 0:64].rearrange("p (g f) -> p g f", f=8)
        sinv = cs[b][:, 64:128].rearrange("p (g f) -> p g f", f=8)
        r = rt[b]
        rv = [r[:, i, :].rearrange("p (g f) -> p g f", f=8) for i in range(4)]
        for i, (a0, a1) in enumerate(((x1, cosv), (x2, sinv), (x2, cosv), (x1, sinv))):
            P.op("pool" if i % 2 else "dve",
                 lambda e, i=i, a0=a0, a1=a1, rv=rv: e.tensor_tensor(out=rv[i], in0=a0, in1=a1, op=ALU.mult),
                 reads=[qk32_t[b], cs_t[b]], writes=[rt_t[b]], n=64)
        P.op("dve", lambda e, x1=x1, rv=rv: e.tensor_tensor(out=x1, in0=rv[0], in1=rv[1], op=ALU.subtract),
             reads=[rt_t[b]], writes=[qk32_t[b]])
        P.op("dve", lambda e, x2=x2, rv=rv: e.tensor_tensor(out=x2, in0=rv[2], in1=rv[3], op=ALU.add),
             reads=[rt_t[b]], writes=[qk32_t[b]])
        P.op("act", lambda e, b=b: e.activation(out=qkb[b][:, :], in_=qk32[b][:, :], func=AF.Copy),
             reads=[qk32_t[b]], writes=[qkb_t[b]])
        for g in range(8):
            P.op("pe", lambda e, g=g, b=b: e.transpose(out=pqT[0:64, g, :], in_=qkb[b][:, g * 64:(g + 1) * 64],
                                                       identity=ident[:, :]),
                 reads=[qkb_t[b], ident_t], writes=[pqT_t])
        P.op("dve", lambda e, b=b: e.tensor_copy(out=qkT[b][:, :, :], in_=pqT[0:64, :, :]),
             reads=[pqT_t], writes=[qkT_t[b]])
        tq = T("qs%d" % j)
        tk = T("ks%d" % j)
        scr_t += [tq, tk]
        P.dma("sp", qTs[:, :, j * 128:(j + 1) * 128].rearrange("h d n -> d h n"), qkT[b][:, 0:4, :],
              reads=[qkT_t[b]], writes=[tq])
        P.dma("sp", kTs[:, :, j * 128:(j + 1) * 128].rearrange("h d n -> d h n"), qkT[b][:, 4:8, :],
              reads=[qkT_t[b]], writes=[tk])
        if j == 0:
            P.op("dve", lambda e, b=b: e.tensor_reduce(out=kmx[:, :], in_=qkT[b][:, 4:8, :], axis=AX.X, op=ALU.max,
                                                       apply_absolute_value=True),
                 reads=[qkT_t[b]], writes=[kmx_t])
        else:
            P.op("dve", lambda e, b=b: e.tensor_reduce(out=kmt[b][:, :], in_=qkT[b][:, 4:8, :], axis=AX.X, op=ALU.max,
                                                       apply_absolute_value=True),
                 reads=[qkT_t[b]], writes=[kmt_t[b]])
            P.op("dve", lambda e, b=b: e.tensor_tensor(out=kmx[:, :], in0=kmx[:, :], in1=kmt[b][:, :], op=ALU.max),
                 reads=[kmt_t[b], kmx_t], writes=[kmx_t])
        blk = j // 2
        if j % 2 == 0:
            P.op("dve", lambda e, b=b, blk=blk: e.tensor_reduce(out=kms[:, :, blk], in_=qkT[b][:, 4:8, :],
                                                                axis=AX.X, op=ALU.add),
                 reads=[qkT_t[b]], writes=[kms_t])
        else:
            P.op("dve", lambda e, b=b: e.tensor_reduce(out=ksum[b][:, :], in_=qkT[b][:, 4:8, :],
                                                       axis=AX.X, op=ALU.add),
                 reads=[qkT_t[b]], writes=[ksum_t[b]])
            P.op("dve", lambda e, b=b, blk=blk: e.tensor_tensor(out=kms[:, :, blk], in0=kms[:, :, blk],
                                                                in1=ksum[b][:, :], op=ALU.add),
                 reads=[ksum_t[b], kms_t], writes=[kms_t])
    P.op("act", lambda e: e.activation(out=kmeanb[:, :, :], in_=kms[:, :, :], func=AF.Copy, scale=1.0 / 256.0),
         reads=[kms_t], writes=[kmeanb_t])

    kmaxb = P.sb("kmaxb", [64, 4], BF16); kmaxb_t = T("kmaxb")
    P.op("dve", lambda e: e.tensor_copy(out=kmaxb[:, :], in_=kmx[:, :]), reads=[kmx_t], writes=[kmaxb_t])
    QA2 = [QA, P.sb("QA_b", [128, S], BF16)]
    qabs2 = [qabs, P.sb("qabs_b", [64, S], BF16)]
    qa_l = [[T("qal%d_%d" % (u, i)) for i in range(4)] for u in range(2)]
    qabs_l = [[T("qabs%d_%d" % (u, i)) for i in range(4)] for u in range(2)]
    QAb_t = [[T("QAb%d_%d" % (u, g)) for g in range(16)] for u in range(2)]
    ka_l = [T("kal%d" % i) for i in range(4)]
    va_l = [T("val%d" % i) for i in range(4)]
    gm, gm_t = dbl("gm", [128, 32], F32)
    sel, sel_t = dbl("sel", [128, 32], F32)
    top8, top8_t = dbl("top8", [128, 8], F32)
    mq, mq_t = dbl("mq", [128, 1], F32)
    BT, BT_t = dbl("BT", [128, 4, 96], F32)
    for i in range(2):
        P.op("pool", lambda e, i=i: e.memset(BT[i][:, :, :], 0.0), writes=[BT_t[i]])
    Pb = [P.sb("Pb%d" % i, [128, 2, 512], BF16) for i in range(3)]
    Pb_t = [T("Pb%d" % i) for i in range(3)]
    OS, OS_t = dbl("OS", [128, 512], F32)
    DN, DN_t = dbl("DN", [64, 512], F32)
    OTs, OTs_t = dbl("OTs", [64, 512], BF16)
    pG = pqT[:, :, :].rearrange("p g n -> p (g n)").bitcast(F32)
    pG_t = pqT_t
    pBT, pBT_t = pv, pv_t
    po, po_t = pqk, pqk_t
    cnt = {"kS": 0, "kP": 0, "gk": 0}

    def load_q(h, u):
        for q4 in range(4):
            sl = slice(q4 * 2048, (q4 + 1) * 2048)
            P.dma("sp", QA2[u][0:64, sl], qTs[h, :, sl], reads=scr_t, writes=[qa_l[u][q4]])
            P.op("act", lambda e, sl=sl, u=u: e.activation(out=qabs2[u][:, sl], in_=QA2[u][0:64, sl], func=AF.Abs),
                 reads=[qa_l[u][q4]], writes=[qabs_l[u][q4]], n=2048)
        if h == 0:
            pass

    def load_kv(h):
        for q4 in range(4):
            sl = slice(q4 * 2048, (q4 + 1) * 2048)
            P.dma("sp", KA[0:64, sl], kTs[h, :, sl], reads=scr_t, writes=[ka_l[q4]])
            P.dma("pool", VA[:, q4 * 16:(q4 + 1) * 16, 0:64],
                  vs[q4 * 2048:(q4 + 1) * 2048, h * 64:(h + 1) * 64].rearrange("(t p) c -> p t c", p=128),
                  reads=scr_t, writes=[va_l[q4]])

    def gating(h, u, g):
        QAu, qabsu = QA2[u], qabs2[u]
        bt, bt_t = BT[g % 2], BT_t[g % 2]
        for qi in range(4):
            qt = g * 4 + qi
            cur = qt // 2
            k2 = cnt["gk"] % 2
            cnt["gk"] += 1
            csl = slice(qt * 128, (qt + 1) * 128)
            P.op("pe", lambda e, csl=csl: e.matmul(pG[:, 0:32], lhsT=QAu[0:64, csl], rhs=kmeanb[:, h, :],
                                                   start=True, stop=True),
                 reads=[qa_l[u], kmeanb_t], writes=[pG_t], n=32)
            P.op("pe", lambda e, csl=csl: e.matmul(pG[:, 32:33], lhsT=qabsu[:, csl], rhs=kmaxb[:, h:h + 1],
                                                   start=True, stop=True),
                 reads=[qabs_l[u], kmaxb_t], writes=[pG_t], n=8)
            P.op("dve", lambda e, k2=k2, cur=cur: e.tensor_tensor(out=gm[k2][:, :], in0=pG[:, 0:32],
                                                                  in1=M1[:, cur * 32:(cur + 1) * 32], op=ALU.add),
                 reads=[pG_t, cst_t], writes=[gm_t[k2]], n=32)
            P.op("dve", lambda e, k2=k2: e.tensor_copy(out=mq[k2][:, :], in_=pG[:, 32:33]),
                 reads=[pG_t], writes=[mq_t[k2]], n=1)
            P.op("dve", lambda e, k2=k2: e.max(out=top8[k2][:, :], in_=gm[k2][:, :]),
                 reads=[gm_t[k2]], writes=[top8_t[k2]], n=32)
            P.op("dve", lambda e, k2=k2: e.tensor_scalar(out=sel[k2][:, :], in0=gm[k2][:, :],
                                                         scalar1=top8[k2][:, 2:3], scalar2=1.0,
                                                         op0=ALU.is_ge, op1=ALU.subtract),
                 reads=[gm_t[k2], top8_t[k2]], writes=[sel_t[k2]], n=32)
            P.op("dve", lambda e, k2=k2, cur=cur: e.tensor_tensor(out=sel[k2][:, :], in0=sel[k2][:, :],
                                                                  in1=A30[:, cur * 32:(cur + 1) * 32], op=ALU.mult),
                 reads=[sel_t[k2], cst_t], writes=[sel_t[k2]], n=32)
            P.op("dve", lambda e, k2=k2, cur=cur: e.tensor_tensor(out=sel[k2][:, :], in0=sel[k2][:, :],
                                                                  in1=Bc[:, cur * 32:(cur + 1) * 32], op=ALU.add),
                 reads=[sel_t[k2], cst_t], writes=[sel_t[k2]], n=32)
            P.op("dve", lambda e, k2=k2, qi=qi: e.tensor_scalar(out=bt[:, qi, 64:96], in0=sel[k2][:, :],
                                                                scalar1=mq[k2][:, 0:1], scalar2=None,
                                                                op0=ALU.subtract),
                 reads=[sel_t[k2], mq_t[k2]], writes=[bt_t], n=32)
            P.op("pe", lambda e, qi=qi: e.transpose(out=pBT[0:96, qi * 128:(qi + 1) * 128], in_=bt[:, qi, :],
                                                    identity=identf[:, :]),
                 reads=[bt_t, cst_t], writes=[pBT_t], n=512)
        P.op("act", lambda e: e.activation(out=QAu[64:96, g * 512:(g + 1) * 512], in_=pBT[64:96, :], func=AF.Copy),
             reads=[pBT_t], writes=[QAb_t[u][g]], n=512)

    def attention(h, u, g):
        QAu = QA2[u]
        nk = 4 * g + 4
        for kp in range(nk // 2):
            ps, ps_t = pS[cnt["kS"] % 2], pS_t[cnt["kS"] % 2]
            cnt["kS"] += 1
            pbuf, pbuf_t = Pb[cnt["kP"] % 3], Pb_t[cnt["kP"] % 3]
            cnt["kP"] += 1
            c0s = []
            for uu in range(2):
                kt = 2 * kp + uu
                i = kt - 4 * g
                c0 = max(i, 0) * 128
                c0s.append(c0)
                P.op("pe", lambda e, ps=ps, kt=kt, c0=c0, i=i, uu=uu: e.matmul(
                    ps[:, uu, c0:512], lhsT=KA[0:96, kt * 128:(kt + 1) * 128],
                    rhs=QAu[0:96, g * 512 + c0:(g + 1) * 512], start=True, stop=(i < 0)),
                    reads=[ka_l, KAoh_t, qa_l[u], QAb_t[u][g]], writes=[ps_t], n=512 - c0)
                if i >= 0:
                    P.op("pe", lambda e, ps=ps, c0=c0, uu=uu: e.matmul(ps[:, uu, c0:c0 + 128], lhsT=ident[:, :],
                                                                       rhs=tri[:, :], start=False, stop=True),
                         reads=[ident_t, cst_t], writes=[ps_t])
            cm = c0s[0]
            P.op("act", lambda e, ps=ps, pbuf=pbuf, cm=cm: e.activation(out=pbuf[:, :, cm:512], in_=ps[:, :, cm:512],
                                                                        func=AF.Exp),
                 reads=[ps_t], writes=[pbuf_t], n=2 * (512 - cm))
            for uu in range(2):
                kt = 2 * kp + uu
                c0 = c0s[uu]
                P.op("pe", lambda e, pbuf=pbuf, kt=kt, c0=c0, uu=uu: e.matmul(
                    po[:, c0:512], lhsT=VA[:, kt, :], rhs=pbuf[:, uu, c0:512], start=(kt == 0), stop=(kt == nk - 1)),
                    reads=[va_l, VA1_t, pbuf_t], writes=[po_t], n=512 - c0)
        o = g % 2
        P.op("act", lambda e, o=o: e.activation(out=OS[o][:, :], in_=po[:, :], func=AF.Copy),
             reads=[po_t], writes=[OS_t[o]], n=512)
        P.dma("sp", DN[o][:, :], OS[o][64:128, :], reads=[OS_t[o]], writes=[DN_t[o]])
        P.op("dve", lambda e, o=o: e.reciprocal(out=DN[o][:, :], in_=DN[o][:, :]), reads=[DN_t[o]], writes=[DN_t[o]],
             n=512)
        P.op("dve", lambda e, o=o: e.tensor_tensor(out=OTs[o][:, :], in0=OS[o][0:64, :], in1=DN[o][:, :], op=ALU.mult),
             reads=[OS_t[o], DN_t[o]], writes=[OTs_t[o]], n=512)
        P.dma("sp", oT[h * 64:(h + 1) * 64, g * 512:(g + 1) * 512], OTs[o][:, :], reads=[OTs_t[o]],
              writes=[T("oT")], final=True)

    load_q(0, 0)
    for g in range(16):
        gating(0, 0, g)
    for h in range(4):
        u = h % 2
        load_kv(h)
        if h < 3:
            load_q(h + 1, 1 - u)
        for g in range(16):
            attention(h, u, g)
            if h < 3:
                gating(h + 1, 1 - u, g)
    return P.build()


_PROGS = {}


def _prog(name):
    if name not in _PROGS:
        _PROGS[name] = {"attn": build_attn, "mlstm": build_mlstm,
                        "pf0": lambda: build_projffn(False), "pf1": lambda: build_projffn(True)}[name]()
    return _PROGS[name]


def _run(name, maps):
    res = run_bass_kernel_spmd(_prog(name), maps, core_ids=list(range(NCORE)))
    return res.results


def _gain_layout(g):
    return np.ascontiguousarray(np.asarray(g, np.float32).reshape(8, 128).T)


def _consts():
    bf = ml_dtypes.bfloat16
    c = {}
    c["ident"] = np.eye(128, dtype=np.float32).astype(bf)
    c["identf"] = np.eye(128, dtype=np.float32)
    pos = np.arange(S, dtype=np.float32)
    inv = (np.float32(500000.0) ** (-np.arange(0, 16, 2, dtype=np.float32) / np.float32(16))).astype(np.float32)
    ang = (pos[:, None] * inv[None, :]).astype(np.float32)
    cos = np.cos(ang).astype(np.float32)
    sin = np.sin(ang).astype(np.float32)
    c["cs"] = np.ascontiguousarray(np.concatenate([np.tile(cos, (1, 8)), np.tile(sin, (1, 8))], axis=1))
    blk = np.arange(S) // 256
    c["onehot"] = (blk[None, :] == np.arange(32)[:, None]).astype(np.float32).astype(bf)
    kk = np.arange(128)
    c["tri"] = np.where(kk[:, None] > kk[None, :], NEG, 0.0).astype(np.float32).astype(bf)
    cur = np.arange(32)[:, None]
    n = np.arange(32)[None, :]
    m1 = np.where(n < cur, 0.0, NEG).astype(np.float32).reshape(1, 1024)
    a30 = np.where(n < cur, -NEG, 0.0).astype(np.float32).reshape(1, 1024)
    bc = np.where(n <= cur, 0.0, NEG).astype(np.float32).reshape(1, 1024)
    c["M1"] = np.ascontiguousarray(np.tile(m1, (128, 1)))
    c["A30"] = np.ascontiguousarray(np.tile(a30, (128, 1)))
    c["Bc"] = np.ascontiguousarray(np.tile(bc, (128, 1)))
    c["U"] = np.triu(np.ones((128, 128), np.float32))
    c["ones"] = np.ones((128, 128), np.float32)
    return c


def kernel(x, attn_norm, attn_w_qkv, attn_w_o, mlstm_norm, mlstm_w_in, mlstm_b_gates,
           mlstm_head_norm, mlstm_w_out, ffn_norm, ffn_w_gate_up, ffn_w_down, final_norm):
    f32 = np.float32
    x = np.asarray(x, f32)
    c = _consts()
    wqkv = np.asarray(attn_w_qkv, f32)[0]
    maps = []
    for core in range(NCORE):
        b, hg = core // 4, core % 4
        cols = np.concatenate([np.arange(hg * 256, (hg + 1) * 256) + off for off in (0, 1024, 2048)])
        maps.append({"xin": x[b], "gn": _gain_layout(attn_norm[0]), "wqkv": np.ascontiguousarray(wqkv[:, cols]),
                     "cs": c["cs"], "ident": c["ident"], "identf": c["identf"], "onehot": c["onehot"],
                     "tri": c["tri"], "M1": c["M1"], "A30": c["A30"], "Bc": c["Bc"]})
    ra = _run("attn", maps)
    oT = [np.concatenate([ra[b * 4 + hg]["oT"] for hg in range(4)], axis=0) for b in range(B)]
    maps = []
    for core in range(NCORE):
        b, q = core // 4, core % 4
        sl = slice(q * 2048, (q + 1) * 2048)
        maps.append({"hin": x[b, sl], "aT": np.ascontiguousarray(oT[b][:, sl]), "wp": np.asarray(attn_w_o, f32)[0],
                     "gn": _gain_layout(ffn_norm[0]), "wgu": np.asarray(ffn_w_gate_up, f32)[0],
                     "wd": np.asarray(ffn_w_down, f32)[0], "ident": c["ident"]})
    rb = _run("pf0", maps)
    h1 = [np.concatenate([rb[b * 4 + q]["hout"] for q in range(4)], axis=0) for b in range(B)]
    win = np.asarray(mlstm_w_in, f32)[0]
    bgv = np.asarray(mlstm_b_gates, f32)[0]
    hnv = np.asarray(mlstm_head_norm, f32)[0]
    maps = []
    for core in range(NCORE):
        b, hp = core // 4, core % 4
        h0 = 2 * hp
        wq = win[:, h0 * 64:(h0 + 2) * 64]
        wk = win[:, 512 + h0 * 64:512 + (h0 + 2) * 64]
        wv = win[:, 1024 + h0 * 128:1024 + (h0 + 2) * 128]
        wo = win[:, 2048 + h0 * 128:2048 + (h0 + 2) * 128]
        gi = win[:, 3072 + h0:3072 + h0 + 2]
        gf = win[:, 3080 + h0:3080 + h0 + 2]
        wtok = np.ascontiguousarray(np.concatenate([wk, wv, wq, wo, gi, gf], axis=1))
        bg4 = np.concatenate([bgv[h0:h0 + 2], bgv[8 + h0:8 + h0 + 2]])
        maps.append({"hin": h1[b], "gn": _gain_layout(mlstm_norm[0]), "wtok": wtok,
                     "bg": np.ascontiguousarray(np.tile(bg4[None, :], (128, 1))),
                     "hn": np.ascontiguousarray(np.tile(hnv[None, h0 * 128:(h0 + 2) * 128], (128, 1))),
                     "ident": c["ident"], "U": c["U"], "ones": c["ones"]})
    rc = _run("mlstm", maps)
    yT = [np.ascontiguousarray(np.concatenate([rc[b * 4 + hp]["y"] for hp in range(4)], axis=1).T)
          for b in range(B)]
    maps = []
    for core in range(NCORE):
        b, q = core // 4, core % 4
        sl = slice(q * 2048, (q + 1) * 2048)
        maps.append({"hin": np.ascontiguousarray(h1[b][sl]), "aT": np.ascontiguousarray(yT[b][:, sl]),
                     "wp": np.asarray(mlstm_w_out, f32)[0], "gn": _gain_layout(ffn_norm[1]),
                     "wgu": np.asarray(ffn_w_gate_up, f32)[1], "wd": np.asarray(ffn_w_down, f32)[1],
                     "ident": c["ident"], "fnw": np.asarray(final_norm, f32)})
    rd = _run("pf1", maps)
    out = np.stack([np.concatenate([rd[b * 4 + q]["hout"] for q in range(4)], axis=0) for b in range(B)])
    return out.astype(f32)
```

```python
import math
from contextlib import ExitStack

import numpy as np
import ml_dtypes

import concourse.bass as bass
import concourse.mybir as mybir
from concourse.bass_utils import run_bass_kernel_spmd

F32 = mybir.dt.float32
BF16 = mybir.dt.bfloat16
AF = mybir.ActivationFunctionType
ALU = mybir.AluOpType
AX = mybir.AxisListType

D = 1024
B = 2
S = 8192
NCORE = 8
DFF = 2816
EPS = 1e-6
NEG = -30000.0


class T:
    __slots__ = ("name", "w", "r", "psum")

    def __init__(self, name, psum=False):
        self.name = name
        self.w = None
        self.r = {}
        self.psum = psum


def _flat(x):
    out = []
    for a in x:
        if isinstance(a, (list, tuple)):
            out.extend(_flat(a))
        elif a is not None:
            out.append(a)
    return out


class Prog:
    ENGS = ("pe", "act", "dve", "pool", "sp")
    NRING = 8

    def __init__(self):
        self.nc = bass.Bass("TRN2", target_bir_lowering=False)
        self.ins = {e: [] for e in self.ENGS}
        self.ndma = {e: 0 for e in self.ENGS}
        self.dma_idx = {e: [] for e in self.ENGS}
        self.stack = ExitStack()
        self.final = []

    def sb(self, name, shape, dt):
        return self.stack.enter_context(self.nc.sbuf_tensor(name, list(shape), dt))

    def ps(self, name, shape, dt):
        return self.stack.enter_context(self.nc.psum_tensor(name, list(shape), dt))

    def dram_in(self, name, shape, dt):
        return self.nc.dram_tensor(name, list(shape), dt, kind="ExternalInput").ap()

    def dram_out(self, name, shape, dt):
        return self.nc.dram_tensor(name, list(shape), dt, kind="ExternalOutput").ap()

    def dram_tmp(self, name, shape, dt):
        return self.nc.dram_tensor(name, list(shape), dt).ap()

    COST0 = {"pe": 40.0, "act": 220.0, "dve": 120.0, "pool": 250.0, "sp": 60.0}
    COSTN = {"pe": 0.42, "act": 1.05, "dve": 0.8, "pool": 1.6, "sp": 0.0}

    def _emit(self, eng, fn, reads, writes, dma, n=128):
        reads, writes = _flat(reads), _flat(writes)
        lst = self.ins[eng]
        idx = len(lst)
        raw, other = set(), set()
        for t in reads:
            if t.w is not None:
                raw.add(t.w)
            if t.psum:
                for e2, l2 in t.r.items():
                    if e2 != eng:
                        for i2 in l2:
                            other.add((e2, i2))
        for t in writes:
            if t.w is not None:
                other.add(t.w)
            for e2, l2 in t.r.items():
                for i2 in l2:
                    other.add((e2, i2))
        deps, order = set(), set()

        def same_compute(d):
            return d[0] == eng and not dma and self.ins[eng][d[1]]["dma"] is None

        for d in raw:
            if same_compute(d) and eng == "pe":
                order.add(d[1])
            else:
                deps.add(d)
        for d in other:
            if same_compute(d):
                order.add(d[1])
            else:
                deps.add(d)
        deps.discard((eng, idx))
        cost = self.COST0[eng] + self.COSTN[eng] * n
        rec = dict(fn=fn, deps=deps, order=order, dma=None, cost=cost)
        if dma:
            rec["dma"] = -1
            rec["cost"] = 2000.0 + n * 0.01
        lst.append(rec)
        for t in reads:
            t.r.setdefault(eng, []).append(idx)
        for t in writes:
            t.w = (eng, idx)
            t.r = {}
        return idx

    def op(self, eng, fn, reads=(), writes=(), n=128):
        return self._emit(eng, fn, reads, writes, False, n)

    def dma(self, eng, out, in_, reads=(), writes=(), final=False, n=65536):
        i = self._emit(eng, lambda e: e.dma_start(out=out, in_=in_), reads, writes, True, n)
        if final:
            self.final.append((eng, i))
        return i

    WINDOW = 160

    def schedule(self):
        ENGS = self.ENGS
        ins = self.ins
        n_tot = sum(len(ins[e]) for e in ENGS)
        done = {e: [None] * len(ins[e]) for e in ENGS}
        pend = {e: list(range(len(ins[e]))) for e in ENGS}
        free = {e: 0.0 for e in ENGS}
        neword = {e: [] for e in ENGS}
        count = 0
        while count < n_tot:
            best = None
            for e in ENGS:
                p = pend[e]
                lim = min(len(p), self.WINDOW)
                for k in range(lim):
                    i = p[k]
                    rec = ins[e][i]
                    ok = True
                    rt = free[e]
                    for o in rec["order"]:
                        if done[e][o] is None:
                            ok = False
                            break
                    if not ok:
                        continue
                    for (e2, i2) in rec["deps"]:
                        dt = done[e2][i2]
                        if dt is None:
                            ok = False
                            break
                        if dt > rt:
                            rt = dt
                    if not ok:
                        continue
                    key = (rt, k)
                    if best is None or key < best[0]:
                        best = (key, e, k, i, rt)
                    if rt <= free[e]:
                        break
            assert best is not None, "scheduler deadlock"
            _, e, k, i, rt = best
            rec = ins[e][i]
            if rec["dma"] is not None:
                free[e] = rt + 60.0
                done[e][i] = rt + rec["cost"]
            else:
                free[e] = rt + rec["cost"]
                done[e][i] = rt + rec["cost"] + 60.0
            pend[e].pop(k)
            neword[e].append(i)
            count += 1
        self.sim_time = max(max([x for x in done[e] if x is not None] + [0.0]) for e in ENGS)
        pos = {e: {old: new for new, old in enumerate(neword[e])} for e in ENGS}
        for e in ENGS:
            newl = []
            for old in neword[e]:
                rec = ins[e][old]
                rec["deps"] = {(e2, pos[e2][i2]) for (e2, i2) in rec["deps"]}
                newl.append(rec)
            ins[e] = newl
        self.final = [(e, pos[e][i]) for (e, i) in self.final]
        for e in ENGS:
            k = 0
            idxs = []
            for i, rec in enumerate(ins[e]):
                if rec["dma"] is not None:
                    rec["dma"] = k
                    if k >= self.NRING:
                        rec["deps"].add((e, idxs[k - self.NRING]))
                    idxs.append(i)
                    k += 1
            self.ndma[e] = k

    def build(self):
        nc = self.nc
        st = self.stack
        self.schedule()
        final_deps = set(self.final)
        needed = {e: set() for e in self.ENGS}
        for e in self.ENGS:
            for rec in self.ins[e]:
                for (e2, i2) in rec["deps"]:
                    needed[e2].add(i2)
        cnt_sem = {e: st.enter_context(nc.semaphore("c_" + e)) for e in self.ENGS}
        ring = {e: [st.enter_context(nc.semaphore("r_%s%d" % (e, i))) for i in range(self.NRING)]
                for e in self.ENGS if self.ndma[e] > 0}
        sig = {e: {} for e in self.ENGS}
        for e in self.ENGS:
            c = 0
            for i, rec in enumerate(self.ins[e]):
                if rec["dma"] is not None:
                    k = rec["dma"]
                    sig[e][i] = (ring[e][k % self.NRING], 16 * (k // self.NRING + 1))
                elif i in needed[e]:
                    c += 1
                    sig[e][i] = (cnt_sem[e], c)
        self.stats = {e: (len(self.ins[e]), self.ndma[e]) for e in self.ENGS}
        block = st.enter_context(nc.Block())
        handles = {"pe": block.tensor, "act": block.scalar, "dve": block.vector,
                   "pool": block.gpsimd, "sp": block.sync}

        def make(e):
            def body(eng):
                waited = {}
                for i, rec in enumerate(self.ins[e]):
                    for d in sorted(rec["deps"]):
                        sem, val = sig[d[0]][d[1]]
                        key = id(sem)
                        if waited.get(key, 0) >= val:
                            continue
                        waited[key] = val
                        eng.wait_ge(sem, val)
                    inst = rec["fn"](eng)
                    if i in sig[e]:
                        sem, val = sig[e][i]
                        inst.then_inc(sem, 16 if rec["dma"] is not None else 1)
                if e == "sp":
                    for d in sorted(final_deps):
                        sem, val = sig[d[0]][d[1]]
                        if waited.get(id(sem), 0) >= val:
                            continue
                        waited[id(sem)] = val
                        eng.wait_ge(sem, val)
            return body

        for e in self.ENGS:
            handles[e](make(e))
        st.close()
        return nc


def load_w_bf16(P, name, w_dram, kdim, ncols, col0=0, tile=None, tr=None):
    kc = kdim // 128
    if tile is None:
        tile = P.sb(name, [128, kc, ncols], BF16)
    src = w_dram[:, col0:col0 + ncols].rearrange("(c p) n -> p c n", p=128)
    step = max(1, kc // 4)
    trs = []
    for c0 in range(0, kc, step):
        c1 = min(kc, c0 + step)
        tr = T(name + str(c0))
        trs.append(tr)
        P.dma("pool", tile[:, c0:c1, :], src[:, c0:c1, :], writes=[tr])
    return tile, trs


class NormCtx:
    def __init__(self, P, gain_dram, ident, ident_t, pst, pst_t):
        self.P = P
        self.ident, self.ident_t = ident, ident_t
        self.pst, self.pst_t = pst, pst_t
        self.g = P.sb("ng_" + gain_dram.tensor.name, [128, 8], F32)
        self.g_t = T("ng")
        P.dma("sp", self.g[:, :], gain_dram[:, :], writes=[self.g_t])
        self.junk = P.sb("nj_" + gain_dram.tensor.name, [128, 1024], BF16)
        self.junk_t = T("nj")
        self.ss = [P.sb("nss%d_" % i + gain_dram.tensor.name, [128, 2], F32) for i in range(2)]
        self.ss_t = [T("nss%d" % i) for i in range(2)]
        self.xs = [P.sb("nxs%d_" % i + gain_dram.tensor.name, [128, 1024], BF16) for i in range(2)]
        self.xs_t = [T("nxs%d" % i) for i in range(2)]
        self.k = 0
        self.dst_full = None
        self.lnexp = False
        self.act_scale = False

    def run(self, h_ap, h_t, dst_fn, dst_t):
        P = self.P
        k = self.k
        self.k += 1
        ss, ss_t = self.ss[k % 2], self.ss_t[k % 2]
        xs, xs_t = self.xs[k % 2], self.xs_t[k % 2]
        pst, pst_t = self.pst[k % len(self.pst)], self.pst_t[k % len(self.pst)]
        junk, junk_t = self.junk, self.junk_t
        P.op("act", lambda e: e.activation(out=junk[:, :], in_=h_ap, func=AF.Square,
                                           accum_out=ss[:, 0:1]),
             reads=[h_t], writes=[junk_t, ss_t], n=1024)
        P.op("dve", lambda e: e.tensor_scalar(out=ss[:, 1:2], in0=ss[:, 0:1], scalar1=1.0 / D,
                                              scalar2=EPS, op0=ALU.mult, op1=ALU.add),
             reads=[ss_t], writes=[ss_t])
        if self.lnexp:
            P.op("act", lambda e: e.activation(out=ss[:, 1:2], in_=ss[:, 1:2], func=AF.Ln),
                 reads=[ss_t], writes=[ss_t])
            P.op("act", lambda e: e.activation(out=ss[:, 1:2], in_=ss[:, 1:2], func=AF.Exp, scale=-0.5),
                 reads=[ss_t], writes=[ss_t])
        else:
            P.op("act", lambda e: e.activation(out=ss[:, 1:2], in_=ss[:, 1:2], func=AF.Sqrt),
                 reads=[ss_t], writes=[ss_t])
            P.op("dve", lambda e: e.reciprocal(out=ss[:, 1:2], in_=ss[:, 1:2]),
                 reads=[ss_t], writes=[ss_t])
        if self.act_scale:
            P.op("act", lambda e: e.activation(out=xs[:, :], in_=h_ap, func=AF.Copy, scale=ss[:, 1:2]),
                 reads=[h_t, ss_t], writes=[xs_t], n=1024)
        else:
            P.op("dve", lambda e: e.tensor_scalar(out=xs[:, :], in0=h_ap, scalar1=ss[:, 1:2],
                                                  scalar2=None, op0=ALU.mult),
                 reads=[h_t, ss_t], writes=[xs_t], n=1024)
        for c in range(8):
            P.op("pe", lambda e, c=c: e.transpose(out=pst[:, c, :], in_=xs[:, c * 128:(c + 1) * 128],
                                                  identity=self.ident[:, :]),
                 reads=[xs_t, self.ident_t], writes=[pst_t])
        g = self.g
        if self.dst_full is not None:
            dfull = self.dst_full(k)
            P.op("dve", lambda e: e.tensor_tensor(out=dfull, in0=pst[:, :, :],
                                                  in1=g[:, :].unsqueeze(2).to_broadcast([128, 8, 128]),
                                                  op=ALU.mult),
                 reads=[pst_t, self.g_t], writes=[dst_t], n=1024)
        else:
            for c in range(8):
                P.op("act", lambda e, c=c: e.activation(out=dst_fn(c), in_=pst[:, c, :], func=AF.Copy,
                                                        scale=g[:, c:c + 1]),
                     reads=[pst_t, self.g_t], writes=[dst_t])


def make_ident(P, ident_dram):
    ident = P.sb("ident_sb", [128, 128], BF16)
    ident_t = T("ident")
    P.dma("sp", ident[:, :], ident_dram[:, :], writes=[ident_t])
    return ident, ident_t


FFN_PARTS = [(0, 5), (5, 5), (10, 4), (14, 4), (18, 4)]


def build_projffn(final):
    P = Prog()
    NT = 16
    hin = P.dram_in("hin", [2048, D], F32)
    aT = P.dram_in("aT", [D, 2048], BF16)
    wp = P.dram_in("wp", [D, D], F32)
    gn = P.dram_in("gn", [128, 8], F32)
    wgu = P.dram_in("wgu", [D, 2 * DFF], F32)
    wd = P.dram_in("wd", [DFF, D], F32)
    identd = P.dram_in("ident", [128, 128], BF16)
    if final:
        fn = P.dram_in("fnw", [D], F32)
    hout = P.dram_out("hout", [2048, D], F32)

    ident, ident_t = make_ident(P, identd)
    h = P.sb("h", [128, NT, D], F32)
    h_t = [T("h%d" % i) for i in range(NT)]
    xnT = P.sb("xnT", [128, 8, 2048], BF16)
    xn_t = [T("xn%d" % i) for i in range(NT)]
    wps, wps_t = load_w_bf16(P, "wps", wp, D, D)
    at = [P.sb("at%d" % i, [128, 8, 128], BF16) for i in range(2)]
    at_t = [T("at%d" % i) for i in range(2)]
    pf = [P.ps("pf%d" % i, [128, 512], F32) for i in range(6)]
    pf_t = [T("pf%d" % i, True) for i in range(6)]
    pb = [P.ps("pb%d" % i, [128, 8, 128], BF16) for i in range(2)]
    pb_t = [T("pb%d" % i, True) for i in range(2)]
    norm = NormCtx(P, gn, ident, ident_t, pb, pb_t)
    norm.dst_full = lambda k: xnT[:, :, k * 128:(k + 1) * 128]
    pfk = [0]

    def next_pf():
        i = pfk[0] % 6
        pfk[0] += 1
        return pf[i], pf_t[i]

    for t in range(NT):
        P.dma("sp", h[:, t, :], hin[t * 128:(t + 1) * 128, :], writes=[h_t[t]])
        a, a_t = at[t % 2], at_t[t % 2]
        P.dma("sp", a[:, :, :], aT[:, t * 128:(t + 1) * 128].rearrange("(c p) n -> p c n", p=128),
              writes=[a_t])
        for nh in range(2):
            ps, ps_t = next_pf()
            for c in range(8):
                P.op("pe", lambda e, c=c, ps=ps, a=a, nh=nh: e.matmul(
                    ps[:, :], lhsT=a[:, c, :], rhs=wps[:, c, nh * 512:(nh + 1) * 512],
                    start=(c == 0), stop=(c == 7)),
                    reads=[a_t, wps_t], writes=[ps_t], n=512)
            P.op("dve", lambda e, ps=ps, t=t, nh=nh: e.tensor_tensor(
                out=h[:, t, nh * 512:(nh + 1) * 512], in0=h[:, t, nh * 512:(nh + 1) * 512],
                in1=ps[:, :], op=ALU.add),
                reads=[ps_t, h_t[t]], writes=[h_t[t]], n=512)
        norm.run(h[:, t, :], h_t[t], lambda c, t=t: xnT[:, c, t * 128:(t + 1) * 128], xn_t[t])

    wg_b = [P.sb("wg%d" % i, [128, 8, 640], BF16) for i in range(2)]
    wu_b = [P.sb("wu%d" % i, [128, 8, 640], BF16) for i in range(2)]
    wd_b = [P.sb("wd%d" % i, [128, 5, 1024], BF16) for i in range(2)]
    wg_t = [[T("wg%d_%d" % (i, q)) for q in range(4)] for i in range(2)]
    wu_t = [[T("wu%d_%d" % (i, q)) for q in range(4)] for i in range(2)]
    wd_t = [[T("wd%d_%d" % (i, q)) for q in range(3)] for i in range(2)]
    sg = [P.sb("sg%d" % i, [128, 512], F32) for i in range(2)]
    sg_t = [T("sg%d" % i) for i in range(2)]
    aF = [P.sb("aF%d" % i, [128, 5, 512], BF16) for i in range(2)]
    aF_t = [T("aF%d" % i) for i in range(2)]
    kk = 0
    for pi, (c0, ncn) in enumerate(FFN_PARTS):
        b = pi % 2
        ncols = ncn * 128
        srcg = wgu[:, c0 * 128:c0 * 128 + ncols].rearrange("(c p) n -> p c n", p=128)
        srcu = wgu[:, DFF + c0 * 128:DFF + c0 * 128 + ncols].rearrange("(c p) n -> p c n", p=128)
        gate = [xn_t[4]] if pi == 0 else ([xn_t[12]] if pi == 1 else [])
        for q in range(4):
            P.dma("pool", wg_b[b][:, 2 * q:2 * q + 2, 0:ncols], srcg[:, 2 * q:2 * q + 2, :], reads=gate,
                  writes=[wg_t[b][q]])
            P.dma("pool", wu_b[b][:, 2 * q:2 * q + 2, 0:ncols], srcu[:, 2 * q:2 * q + 2, :], reads=gate,
                  writes=[wu_t[b][q]])
        srcd = wd[c0 * 128:(c0 + ncn) * 128, :].rearrange("(c p) n -> p c n", p=128)
        for q in range(0, ncn, 2):
            q1 = min(ncn, q + 2)
            P.dma("pool", wd_b[b][:, q:q1, :], srcd[:, q:q1, :], reads=gate, writes=[wd_t[b][q // 2]])
        for tg in range(4):
            af, af_t = aF[tg % 2], aF_t[tg % 2]
            for j in range(ncn):
                psg, psg_t = next_pf()
                psu, psu_t = next_pf()
                for c in range(8):
                    P.op("pe", lambda e, c=c, j=j, psg=psg, b=b, tg=tg: e.matmul(
                        psg[:, :], lhsT=wg_b[b][:, c, j * 128:(j + 1) * 128],
                        rhs=xnT[:, c, tg * 512:(tg + 1) * 512], start=(c == 0), stop=(c == 7)),
                        reads=[wg_t[b]] + xn_t[tg * 4:tg * 4 + 4], writes=[psg_t], n=512)
                for c in range(8):
                    P.op("pe", lambda e, c=c, j=j, psu=psu, b=b, tg=tg: e.matmul(
                        psu[:, :], lhsT=wu_b[b][:, c, j * 128:(j + 1) * 128],
                        rhs=xnT[:, c, tg * 512:(tg + 1) * 512], start=(c == 0), stop=(c == 7)),
                        reads=[wu_t[b]] + xn_t[tg * 4:tg * 4 + 4], writes=[psu_t], n=512)
                s, s_t = sg[kk % 2], sg_t[kk % 2]
                kk += 1
                P.op("act", lambda e, s=s, psg=psg: e.activation(out=s[:, :], in_=psg[:, :], func=AF.Silu),
                     reads=[psg_t], writes=[s_t], n=512)
                P.op("dve", lambda e, s=s, psu=psu, af=af, j=j: e.tensor_tensor(
                    out=af[:, j, :], in0=s[:, :], in1=psu[:, :], op=ALU.mult),
                    reads=[s_t, psu_t], writes=[af_t], n=512)
            for tt in range(4):
                t = tg * 4 + tt
                for nh in range(2):
                    ps, ps_t = next_pf()
                    for j in range(ncn):
                        P.op("pe", lambda e, j=j, ps=ps, af=af, tt=tt, nh=nh, b=b: e.matmul(
                            ps[:, :], lhsT=af[:, j, tt * 128:(tt + 1) * 128],
                            rhs=wd_b[b][:, j, nh * 512:(nh + 1) * 512],
                            start=(j == 0), stop=(j == ncn - 1)),
                            reads=[af_t, wd_t[b]], writes=[ps_t], n=512)
                    P.op("dve", lambda e, ps=ps, t=t, nh=nh: e.tensor_tensor(
                        out=h[:, t, nh * 512:(nh + 1) * 512], in0=h[:, t, nh * 512:(nh + 1) * 512],
                        in1=ps[:, :], op=ALU.add),
                        reads=[ps_t, h_t[t]], writes=[h_t[t]], n=512)

    if final:
        fw = P.sb("fw", [128, D], F32)
        fw_t = T("fw")
        P.dma("sp", fw[:, :], fn.partition_broadcast(128), writes=[fw_t])
        ss = P.sb("fss", [128, 2 * NT], F32)
        ss_t = T("fss")
        junk = norm.junk
        for t in range(NT):
            P.op("act", lambda e, t=t: e.activation(out=junk[:, :], in_=h[:, t, :], func=AF.Square,
                                                    accum_out=ss[:, 2 * t:2 * t + 1]),
                 reads=[h_t[t]], writes=[norm.junk_t, ss_t])
            P.op("dve", lambda e, t=t: e.tensor_scalar(out=ss[:, 2 * t + 1:2 * t + 2], in0=ss[:, 2 * t:2 * t + 1],
                                                       scalar1=1.0 / D, scalar2=EPS, op0=ALU.mult, op1=ALU.add),
                 reads=[ss_t], writes=[ss_t])
            P.op("act", lambda e, t=t: e.activation(out=ss[:, 2 * t + 1:2 * t + 2],
                                                    in_=ss[:, 2 * t + 1:2 * t + 2], func=AF.Sqrt),
                 reads=[ss_t], writes=[ss_t])
            P.op("dve", lambda e, t=t: e.reciprocal(out=ss[:, 2 * t + 1:2 * t + 2],
                                                    in_=ss[:, 2 * t + 1:2 * t + 2]),
                 reads=[ss_t], writes=[ss_t])
            P.op("dve", lambda e, t=t: e.scalar_tensor_tensor(
                out=h[:, t, :], in0=h[:, t, :], scalar=ss[:, 2 * t + 1:2 * t + 2], in1=fw[:, :],
                op0=ALU.mult, op1=ALU.mult),
                reads=[h_t[t], ss_t, fw_t], writes=[h_t[t]])
    for t in range(NT):
        P.dma("sp", hout[t * 128:(t + 1) * 128, :], h[:, t, :], reads=[h_t[t]], writes=[T("o%d" % t)], final=True)
    return P.build()


def build_mlstm():
    P = Prog()
    NCH = S // 128
    hin = P.dram_in("hin", [S, D], F32)
    gn = P.dram_in("gn", [128, 8], F32)
    wt_d = P.dram_in("wtok", [D, 772], F32)
    bg_d = P.dram_in("bg", [128, 4], F32)
    hn_d = P.dram_in("hn", [128, 256], F32)
    identd = P.dram_in("ident", [128, 128], BF16)
    U_d = P.dram_in("U", [128, 128], F32)
    ones_d = P.dram_in("ones", [128, 128], F32)
    yout = P.dram_out("y", [S, 256], BF16)

    ident, ident_t = make_ident(P, identd)
    U = P.sb("U_sb", [128, 128], F32)
    ONES = P.sb("ones_sb", [128, 128], F32)
    bg = P.sb("bg_sb", [128, 4], F32)
    hn = P.sb("hn_sb", [128, 256], F32)
    c_t = T("consts")
    P.dma("sp", U[:, :], U_d[:, :], writes=[c_t])
    c2_t = T("consts2")
    P.dma("sp", ONES[:, :], ones_d[:, :], writes=[c2_t])
    c3_t = T("consts3")
    P.dma("sp", bg[:, :], bg_d[:, :], writes=[c3_t])
    c4_t = T("consts4")
    P.dma("sp", hn[:, :], hn_d[:, :], writes=[c4_t])
    wt, wt_t = load_w_bf16(P, "wt_sb", wt_d, D, 772)

    def dbl(name, shape, dt):
        return [P.sb("%s%d" % (name, i), shape, dt) for i in range(2)], [T("%s%d" % (name, i)) for i in range(2)]

    xin, xin_t = dbl("xin", [128, D], F32)
    xnT, xnT_t = dbl("xnT", [128, 8, 128], BF16)
    qT, qT_t = dbl("qT", [64, 2, 128], BF16)
    kT, kT_t = dbl("kT", [64, 2, 128], BF16)
    ktok, ktok_t = dbl("ktok", [128, 128], BF16)
    og, og_t = dbl("og", [128, 256], F32)
    gt, gt_t = dbl("gt", [128, 24], F32)
    V1 = [dbl("V1_%d_" % h, [128, 129], BF16) for h in range(2)]
    V2 = [dbl("V2_%d_" % h, [128, 129], BF16) for h in range(2)]
    PT, PT_t = dbl("PT", [128, 128], BF16)
    hh, hh_t = dbl("hh", [128, 128], F32)
    sq = P.sb("sqj", [128, 128], BF16)
    sq_t = T("sqj")
    st, st_t = dbl("st", [128, 8], F32)
    yt, yt_t = dbl("yt", [128, 256], BF16)
    C = [P.sb("C%d" % h, [64, 129], F32) for h in range(2)]
    C_t = [T("C%d" % h) for h in range(2)]
    Cb = [dbl("Cb%d_" % h, [64, 129], BF16) for h in range(2)]

    pb = [P.ps("pbT", [128, 8, 128], BF16)]
    pb_t = [T("pbT", True)]
    pq = P.ps("pq", [128, 8, 128], BF16); pq_t = T("pq", True)
    qtok, qtok_t = dbl("qtok", [128, 128], BF16)
    p1 = P.ps("p1", [128, 512], F32); p1_t = T("p1", True)
    p2 = P.ps("p2", [128, 512], F32); p2_t = T("p2", True)
    pg = P.ps("pg", [128, 512], F32); pg_t = T("pg", True)
    pS = P.ps("pS", [128, 512], F32); pS_t = T("pS", True)
    pO = P.ps("pO", [128, 512], F32); pO_t = T("pO", True)
    pA = P.ps("pA", [128, 512], F32); pA_t = T("pA", True)
    norm = NormCtx(P, gn, ident, ident_t, pb, pb_t)
    norm.lnexp = True
    norm.dst_full = lambda k: xnT[k % 2][:, :, :]

    for h in range(2):
        P.op("dve", lambda e, h=h: e.memset(C[h][:, :], 0.0), writes=[C_t[h]])
        P.op("dve", lambda e, h=h: e.memset(Cb[h][0][0][:, :], 0.0), writes=[Cb[h][1][0]])

    for j in range(NCH):
        b = j % 2
        P.dma("sp", xin[b][:, :], hin[j * 128:(j + 1) * 128, :], writes=[xin_t[b]])
        norm.run(xin[b][:, :], xin_t[b], lambda c, b=b: xnT[b][:, c, :], xnT_t[b])
        for c in range(8):
            P.op("pe", lambda e, c=c, b=b: e.matmul(p1[:, 0:512], lhsT=xnT[b][:, c, :], rhs=wt[:, c, 0:512],
                                                    start=(c == 0), stop=(c == 7)),
                 reads=[wt_t, xnT_t[b]], writes=[p1_t], n=512)
        for c in range(8):
            P.op("pe", lambda e, c=c, b=b: e.matmul(p2[:, 0:260], lhsT=xnT[b][:, c, :], rhs=wt[:, c, 512:772],
                                                    start=(c == 0), stop=(c == 7)),
                 reads=[wt_t, xnT_t[b]], writes=[p2_t], n=260)
        P.op("act", lambda e, b=b: e.activation(out=ktok[b][:, :], in_=p1[:, 0:128], func=AF.Copy),
             reads=[p1_t], writes=[ktok_t[b]])
        P.op("act", lambda e, b=b: e.activation(out=qtok[b][:, :], in_=p1[:, 384:512], func=AF.Copy, scale=0.125),
             reads=[p1_t], writes=[qtok_t[b]])
        for h in range(2):
            P.op("pe", lambda e, h=h, b=b: e.transpose(out=pq[0:64, h, :], in_=qtok[b][:, h * 64:(h + 1) * 64],
                                                       identity=ident[:, :]),
                 reads=[qtok_t[b], ident_t], writes=[pq_t], n=64)
            P.op("pe", lambda e, h=h, b=b: e.transpose(out=pq[0:64, 2 + h, :], in_=ktok[b][:, h * 64:(h + 1) * 64],
                                                       identity=ident[:, :]),
                 reads=[ktok_t[b], ident_t], writes=[pq_t], n=64)
        P.op("act", lambda e, b=b: e.activation(out=qT[b][:, :, :], in_=pq[0:64, 0:2, :], func=AF.Copy),
             reads=[pq_t], writes=[qT_t[b]], n=256)
        P.op("dve", lambda e, b=b: e.tensor_copy(out=kT[b][:, :, :], in_=pq[0:64, 2:4, :]),
             reads=[pq_t], writes=[kT_t[b]], n=256)
        g = gt[b]
        g_t = gt_t[b]
        P.op("dve", lambda e, g=g: e.tensor_tensor(out=g[:, 0:4], in0=p2[:, 256:260], in1=bg[:, :], op=ALU.add),
             reads=[p2_t, c3_t], writes=[g_t])
        P.op("act", lambda e, g=g: e.activation(out=g[:, 4:8], in_=g[:, 0:4], func=AF.Tanh, scale=1.0 / 15.0),
             reads=[g_t], writes=[g_t])
        P.op("act", lambda e, g=g: e.activation(out=g[:, 8:10], in_=g[:, 6:8], func=AF.Exp, scale=-15.0),
             reads=[g_t], writes=[g_t])
        P.op("act", lambda e, g=g: e.activation(out=g[:, 10:12], in_=g[:, 8:10], func=AF.Ln, bias=1.0),
             reads=[g_t], writes=[g_t])
        P.op("pe", lambda e, g=g: e.matmul(pg[:, 0:2], lhsT=U[:, :], rhs=g[:, 10:12], start=True, stop=True),
             reads=[g_t, c_t], writes=[pg_t])
        P.op("pe", lambda e, g=g: e.matmul(pg[:, 2:4], lhsT=ONES[:, :], rhs=g[:, 10:12], start=True, stop=True),
             reads=[g_t, c2_t], writes=[pg_t])
        P.op("dve", lambda e, g=g: e.tensor_copy(out=g[:, 12:16], in_=pg[:, 0:4]), reads=[pg_t], writes=[g_t])
        P.op("dve", lambda e, g=g: e.tensor_tensor(out=g[:, 16:18], in0=g[:, 12:14], in1=g[:, 14:16],
                                                   op=ALU.subtract),
             reads=[g_t], writes=[g_t])
        s = st[b]
        s_t = st_t[b]
        for h in range(2):
            P.op("act", lambda e, g=g, h=h: e.activation(out=g[:, 18 + h:19 + h], in_=g[:, 4 + h:5 + h], func=AF.Exp,
                                                         scale=15.0, bias=g[:, 12 + h:13 + h]),
                 reads=[g_t], writes=[g_t])
            P.op("act", lambda e, g=g, h=h: e.activation(out=g[:, 20 + h:21 + h], in_=g[:, 4 + h:5 + h], func=AF.Exp,
                                                         scale=15.0, bias=g[:, 16 + h:17 + h]),
                 reads=[g_t], writes=[g_t])
        P.op("act", lambda e, g=g: e.activation(out=g[:, 22:24], in_=g[:, 12:14], func=AF.Exp, scale=-1.0),
             reads=[g_t], writes=[g_t])
        P.op("act", lambda e, g=g, s=s: e.activation(out=s[:, 6:8], in_=g[:, 14:16], func=AF.Exp, scale=-1.0),
             reads=[g_t], writes=[s_t])
        P.op("act", lambda e, b=b: e.activation(out=og[b][:, :], in_=p2[:, 0:256], func=AF.Exp, scale=-1.0),
             reads=[p2_t], writes=[og_t[b]], n=256)
        P.op("pool", lambda e, b=b: e.tensor_scalar_add(out=og[b][:, :], in0=og[b][:, :], scalar1=1.0),
             reads=[og_t[b]], writes=[og_t[b]], n=256)
        P.op("dve", lambda e, b=b: e.reciprocal(out=og[b][:, :], in_=og[b][:, :]),
             reads=[og_t[b]], writes=[og_t[b]], n=256)
        for h in range(2):
            for (V, col) in ((V1[h], 18 + h), (V2[h], 20 + h)):
                Vb, Vb_t = V[0][b], V[1][b]
                P.op("dve", lambda e, Vb=Vb, g=g, col=col, h=h: e.tensor_scalar(
                    out=Vb[:, 0:128], in0=p1[:, 128 + h * 128:256 + h * 128], scalar1=g[:, col:col + 1],
                    scalar2=None, op0=ALU.mult),
                    reads=[p1_t, g_t], writes=[Vb_t])
                P.op("pool", lambda e, Vb=Vb, g=g, col=col: e.tensor_copy(out=Vb[:, 128:129], in_=g[:, col:col + 1]),
                     reads=[g_t], writes=[Vb_t], n=1)
        for h in range(2):
            Cb_cur, Cb_cur_t = Cb[h][0][b], Cb[h][1][b]
            Cb_nxt, Cb_nxt_t = Cb[h][0][1 - b], Cb[h][1][1 - b]
            V1b, V1b_t = V1[h][0][b], V1[h][1][b]
            V2b, V2b_t = V2[h][0][b], V2[h][1][b]
            P.op("pe", lambda e, h=h, b=b: e.matmul(pS[:, 0:128], lhsT=kT[b][:, h, :], rhs=qT[b][:, h, :],
                                                    start=True, stop=True),
                 reads=[kT_t[b], qT_t[b]], writes=[pS_t])
            pt, pt_t = PT[h], PT_t[h]
            P.op("dve", lambda e, pt=pt: e.tensor_tensor(out=pt[:, :], in0=pS[:, 0:128], in1=U[:, :], op=ALU.mult),
                 reads=[pS_t, c_t], writes=[pt_t])
            P.op("pe", lambda e, h=h, b=b, Cb_cur=Cb_cur: e.matmul(pO[:, 0:129], lhsT=qT[b][:, h, :], rhs=Cb_cur[:, :],
                                                                   start=True, stop=False),
                 reads=[qT_t[b], Cb_cur_t], writes=[pO_t])
            P.op("pe", lambda e, pt=pt, V1b=V1b: e.matmul(pO[:, 0:129], lhsT=pt[:, :], rhs=V1b[:, :],
                                                          start=False, stop=True),
                 reads=[pt_t, V1b_t], writes=[pO_t])
            o = 3 * h
            P.op("dve", lambda e, s=s, g=g, h=h, o=o: e.tensor_tensor(out=s[:, o:o + 1], in0=pO[:, 128:129],
                                                                      in1=g[:, 22 + h:23 + h], op=ALU.mult),
                 reads=[pO_t, g_t], writes=[s_t])
            P.op("dve", lambda e, s=s, o=o: e.tensor_scalar(out=s[:, o + 1:o + 2], in0=s[:, o:o + 1], scalar1=-1.0,
                                                            scalar2=1.0, op0=ALU.mult, op1=ALU.max),
                 reads=[s_t], writes=[s_t])
            P.op("dve", lambda e, s=s, o=o: e.tensor_tensor(out=s[:, o:o + 1], in0=s[:, o:o + 1],
                                                            in1=s[:, o + 1:o + 2], op=ALU.max),
                 reads=[s_t], writes=[s_t])
            P.op("dve", lambda e, s=s, o=o: e.reciprocal(out=s[:, o:o + 1], in_=s[:, o:o + 1]),
                 reads=[s_t], writes=[s_t])
            P.op("dve", lambda e, s=s, g=g, h=h, o=o: e.tensor_tensor(out=s[:, o + 1:o + 2], in0=g[:, 22 + h:23 + h],
                                                                      in1=s[:, o:o + 1], op=ALU.mult),
                 reads=[s_t, g_t], writes=[s_t])
            hb, hb_t = hh[h], hh_t[h]
            P.op("dve", lambda e, hb=hb, s=s, o=o: e.tensor_scalar(out=hb[:, :], in0=pO[:, 0:128],
                                                                   scalar1=s[:, o + 1:o + 2], scalar2=None, op0=ALU.mult),
                 reads=[pO_t, s_t], writes=[hb_t])
            P.op("act", lambda e, hb=hb, s=s, o=o: e.activation(out=sq[:, :], in_=hb[:, :], func=AF.Square,
                                                                accum_out=s[:, o + 2:o + 3]),
                 reads=[hb_t], writes=[sq_t, s_t])
            P.op("dve", lambda e, s=s, o=o: e.tensor_scalar(out=s[:, o + 2:o + 3], in0=s[:, o + 2:o + 3],
                                                            scalar1=1.0 / 128.0, scalar2=EPS, op0=ALU.mult, op1=ALU.add),
                 reads=[s_t], writes=[s_t])
            P.op("act", lambda e, s=s, o=o: e.activation(out=s[:, o + 2:o + 3], in_=s[:, o + 2:o + 3], func=AF.Ln),
                 reads=[s_t], writes=[s_t])
            P.op("act", lambda e, s=s, o=o: e.activation(out=s[:, o + 2:o + 3], in_=s[:, o + 2:o + 3], func=AF.Exp,
                                                         scale=-0.5),
                 reads=[s_t], writes=[s_t])
            P.op("dve", lambda e, hb=hb, s=s, o=o, h=h: e.scalar_tensor_tensor(
                out=hb[:, :], in0=hb[:, :], scalar=s[:, o + 2:o + 3], in1=hn[:, h * 128:(h + 1) * 128],
                op0=ALU.mult, op1=ALU.mult),
                reads=[hb_t, s_t, c4_t], writes=[hb_t])
            P.op("dve", lambda e, hb=hb, h=h, b=b: e.tensor_tensor(out=yt[b][:, h * 128:(h + 1) * 128], in0=hb[:, :],
                                                                   in1=og[b][:, h * 128:(h + 1) * 128], op=ALU.mult),
                 reads=[hb_t, og_t[b]], writes=[yt_t[b]])
            P.op("pe", lambda e, h=h, b=b, V2b=V2b: e.matmul(pA[0:64, 0:129], lhsT=ktok[b][:, h * 64:(h + 1) * 64],
                                                             rhs=V2b[:, :], start=True, stop=True),
                 reads=[ktok_t[b], V2b_t], writes=[pA_t])
            P.op("dve", lambda e, h=h, s=s: e.scalar_tensor_tensor(
                out=C[h][:, :], in0=C[h][:, :], scalar=s[0:64, 6 + h:7 + h], in1=pA[0:64, 0:129],
                op0=ALU.mult, op1=ALU.add),
                reads=[C_t[h], s_t, pA_t], writes=[C_t[h]])
            P.op("pool", lambda e, h=h, Cb_nxt=Cb_nxt: e.tensor_copy(out=Cb_nxt[:, :], in_=C[h][:, :]),
                 reads=[C_t[h]], writes=[Cb_nxt_t], n=129)
        P.dma("sp", yout[j * 128:(j + 1) * 128, :], yt[b][:, :], reads=[yt_t[b]], writes=[T("yo%d" % j)], final=True)
    return P.build()


def build_attn():
    P = Prog()
    NT = S // 128
    xin_d = P.dram_in("xin", [S, D], F32)
    gn = P.dram_in("gn", [128, 8], F32)
    w_d = P.dram_in("wqkv", [D, 768], F32)
    cs_d = P.dram_in("cs", [S, 128], F32)
    identd = P.dram_in("ident", [128, 128], BF16)
    identf_d = P.dram_in("identf", [128, 128], F32)
    oneh_d = P.dram_in("onehot", [32, S], BF16)
    tri_d = P.dram_in("tri", [128, 128], BF16)
    M1_d = P.dram_in("M1", [128, 1024], F32)
    A30_d = P.dram_in("A30", [128, 1024], F32)
    Bc_d = P.dram_in("Bc", [128, 1024], F32)
    oT = P.dram_out("oT", [256, S], BF16)
    qTs = P.dram_tmp("qTs", [4, 64, S], BF16)
    kTs = P.dram_tmp("kTs", [4, 64, S], BF16)
    vs = P.dram_tmp("vs", [S, 256], BF16)

    ident, ident_t = make_ident(P, identd)
    identf = P.sb("identf_sb", [128, 128], F32)
    tri = P.sb("tri_sb", [128, 128], BF16)
    M1 = P.sb("M1_sb", [128, 1024], F32)
    A30 = P.sb("A30_sb", [128, 1024], F32)
    Bc = P.sb("Bc_sb", [128, 1024], F32)
    cst_t = []
    for dst, src in ((identf, identf_d), (tri, tri_d), (M1, M1_d), (A30, A30_d), (Bc, Bc_d)):
        t = T("c")
        cst_t.append(t)
        P.dma("sp", dst[:, :], src[:, :], writes=[t])
    w, w_t = load_w_bf16(P, "w_sb", w_d, D, 768)

    KA = P.sb("KA", [128, S], BF16)
    QA = P.sb("QA", [128, S], BF16)
    VA = P.sb("VA", [128, NT, 128], BF16)
    qabs = P.sb("qabs", [64, S], BF16)
    KAoh_t = T("KAoh")
    for q4 in range(4):
        P.dma("sp", KA[64:96, q4 * 2048:(q4 + 1) * 2048], oneh_d[:, q4 * 2048:(q4 + 1) * 2048], writes=[KAoh_t])
    VA1_t = T("VA1")
    P.op("pool", lambda e: e.memset(VA[:, :, 64:128], 1.0), writes=[VA1_t])

    def dbl(name, shape, dt):
        return [P.sb("%s%d" % (name, i), shape, dt) for i in range(2)], [T("%s%d" % (name, i)) for i in range(2)]

    xin, xin_t = dbl("xin_sb", [128, D], F32)
    cs, cs_t = dbl("cs_sb", [128, 128], F32)
    xnT, xnT_t = dbl("xnT", [128, 8, 128], BF16)
    qk32, qk32_t = dbl("qk32", [128, 512], F32)
    rt, rt_t = dbl("rt", [128, 4, 64], F32)
    qkb, qkb_t = dbl("qkb", [128, 512], BF16)
    vb, vb_t = dbl("vb", [128, 256], BF16)
    qkT, qkT_t = dbl("qkT", [64, 8, 128], BF16)
    ksum, ksum_t = dbl("ksum", [64, 4], F32)
    kms = P.sb("kms", [64, 4, 32], F32)
    kms_t = T("kms")
    kmeanb = P.sb("kmeanb", [64, 4, 32], BF16)
    kmeanb_t = T("kmeanb")

    pb = [P.ps("pbT", [128, 8, 128], BF16)]
    pb_t = [T("pbT", True)]
    pqT = P.ps("pqT", [128, 8, 128], BF16); pqT_t = T("pqT", True)
    pqk = P.ps("pqk", [128, 512], F32); pqk_t = T("pqk", True)
    pv = P.ps("pv", [128, 512], F32); pv_t = T("pv", True)
    pS = [P.ps("pS%d" % i, [128, 2, 512], F32) for i in range(2)]
    pS_t = [T("pS%d" % i, True) for i in range(2)]
    pO = [pqk, pqk]
    pO_t = [pqk_t, pqk_t]
    norm = NormCtx(P, gn, ident, ident_t, pb, pb_t)
    norm.dst_full = lambda k: xnT[k % 2][:, :, :]
    norm.act_scale = True
    kmx = P.sb("kmx", [64, 4], F32)
    kmx_t = T("kmx")
    kmt, kmt_t = dbl("kmt", [64, 4], F32)

    scr_t = []
    for j in range(NT):
        b = j % 2
        P.dma("sp", xin[b][:, :], xin_d[j * 128:(j + 1) * 128, :], writes=[xin_t[b]])
        P.dma("sp", cs[b][:, :], cs_d[j * 128:(j + 1) * 128, :], writes=[cs_t[b]])
        norm.run(xin[b][:, :], xin_t[b], lambda c, b=b: xnT[b][:, c, :], xnT_t[b])
        for c in range(8):
            P.op("pe", lambda e, c=c, b=b: e.matmul(pqk[:, :], lhsT=xnT[b][:, c, :], rhs=w[:, c, 0:512],
                                                    start=(c == 0), stop=(c == 7)),
                 reads=[w_t, xnT_t[b]], writes=[pqk_t], n=512)
        for c in range(8):
            P.op("pe", lambda e, c=c, b=b: e.matmul(pv[:, 0:256], lhsT=xnT[b][:, c, :], rhs=w[:, c, 512:768],
                                                    start=(c == 0), stop=(c == 7)),
                 reads=[w_t, xnT_t[b]], writes=[pv_t], n=256)
        P.op("act", lambda e, b=b: e.activation(out=qk32[b][:, 0:256], in_=pqk[:, 0:256], func=AF.Copy, scale=0.125),
             reads=[pqk_t], writes=[qk32_t[b]])
        P.op("act", lambda e, b=b: e.activation(out=qk32[b][:, 256:512], in_=pqk[:, 256:512], func=AF.Copy),
             reads=[pqk_t], writes=[qk32_t[b]])
        P.op("dve", lambda e, b=b: e.tensor_copy(out=vb[b][:, :], in_=pv[:, 0:256]), reads=[pv_t], writes=[vb_t[b]])
        tv = T("vs%d" % j)
        scr_t.append(tv)
        P.dma("sp", vs[j * 128:(j + 1) * 128, :], vb[b][:, :], reads=[vb_t[b]], writes=[tv])
        qv = qk32[b][:, :].rearrange("p (g d) -> p g d", d=64)
        x1, x2 = qv[:, :, 0:8], qv[:, :, 8:16]
        cosv = cs[b][:, 0:64].rearrange("p (g f) -> p g f", f=8)
        sinv = cs[b][:, 64:128].rearrange("p (g f) -> p g f", f=8)
        r = rt[b]
        rv = [r[:, i, :].rearrange("p (g f) -> p g f", f=8) for i in range(4)]
        for i, (a0, a1) in enumerate(((x1, cosv), (x2, sinv), (x2, cosv), (x1, sinv))):
            P.op("pool" if i % 2 else "dve",
                 lambda e, i=i, a0=a0, a1=a1, rv=rv: e.tensor_tensor(out=rv[i], in0=a0, in1=a1, op=ALU.mult),
                 reads=[qk32_t[b], cs_t[b]], writes=[rt_t[b]], n=64)
        P.op("dve", lambda e, x1=x1, rv=rv: e.tensor_tensor(out=x1, in0=rv[0], in1=rv[1], op=ALU.subtract),
             reads=[rt_t[b]], writes=[qk32_t[b]])
        P.op("dve", lambda e, x2=x2, rv=rv: e.tensor_tensor(out=x2, in0=rv[2], in1=rv[3], op=ALU.add),
             reads=[rt_t[b]], writes=[qk32_t[b]])
        P.op("act", lambda e, b=b: e.activation(out=qkb[b][:, :], in_=qk32[b][:, :], func=AF.Copy),
             reads=[qk32_t[b]], writes=[qkb_t[b]])
        for g in range(8):
            P.op("pe", lambda e, g=g, b=b: e.transpose(out=pqT[0:64, g, :], in_=qkb[b][:, g * 64:(g + 1) * 64],
                                                       identity=ident[:, :]),
                 reads=[qkb_t[b], ident_t], writes=[pqT_t])
        P.op("dve", lambda e, b=b: e.tensor_copy(out=qkT[b][:, :, :], in_=pqT[0:64, :, :]),
             reads=[pqT_t], writes=[qkT_t[b]])
        tq = T("qs%d" % j)
        tk = T("ks%d" % j)
        scr_t += [tq, tk]
        P.dma("sp", qTs[:, :, j * 128:(j + 1) * 128].rearrange("h d n -> d h n"), qkT[b][:, 0:4, :],
              reads=[qkT_t[b]], writes=[tq])
        P.dma("sp", kTs[:, :, j * 128:(j + 1) * 128].rearrange("h d n -> d h n"), qkT[b][:, 4:8, :],
              reads=[qkT_t[b]], writes=[tk])
        if j == 0:
            P.op("dve", lambda e, b=b: e.tensor_reduce(out=kmx[:, :], in_=qkT[b][:, 4:8, :], axis=AX.X, op=ALU.max,
                                                       apply_absolute_value=True),
                 reads=[qkT_t[b]], writes=[kmx_t])
        else:
            P.op("dve", lambda e, b=b: e.tensor_reduce(out=kmt[b][:, :], in_=qkT[b][:, 4:8, :], axis=AX.X, op=ALU.max,
                                                       apply_absolute_value=True),
                 reads=[qkT_t[b]], writes=[kmt_t[b]])
            P.op("dve", lambda e, b=b: e.tensor_tensor(out=kmx[:, :], in0=kmx[:, :], in1=kmt[b][:, :], op=ALU.max),
                 reads=[kmt_t[b], kmx_t], writes=[kmx_t])
        blk = j // 2
        if j % 2 == 0:
            P.op("dve", lambda e, b=b, blk=blk: e.tensor_reduce(out=kms[:, :, blk], in_=qkT[b][:, 4:8, :],
                                                                axis=AX.X, op=ALU.add),
                 reads=[qkT_t[b]], writes=[kms_t])
        else:
            P.op("dve", lambda e, b=b: e.tensor_reduce(out=ksum[b][:, :], in_=qkT[b][:, 4:8, :],
                                                       axis=AX.X, op=ALU.add),
                 reads=[qkT_t[b]], writes=[ksum_t[b]])
            P.op("dve", lambda e, b=b, blk=blk: e.tensor_tensor(out=kms[:, :, blk], in0=kms[:, :, blk],
                                                                in1=ksum[b][:, :], op=ALU.add),
                 reads=[ksum_t[b], kms_t], writes=[kms_t])
    P.op("act", lambda e: e.activation(out=kmeanb[:, :, :], in_=kms[:, :, :], func=AF.Copy, scale=1.0 / 256.0),
         reads=[kms_t], writes=[kmeanb_t])

    kmaxb = P.sb("kmaxb", [64, 4], BF16); kmaxb_t = T("kmaxb")
    P.op("dve", lambda e: e.tensor_copy(out=kmaxb[:, :], in_=kmx[:, :]), reads=[kmx_t], writes=[kmaxb_t])
    QA2 = [QA, P.sb("QA_b", [128, S], BF16)]
    qabs2 = [qabs, P.sb("qabs_b", [64, S], BF16)]
    qa_l = [[T("qal%d_%d" % (u, i)) for i in range(4)] for u in range(2)]
    qabs_l = [[T("qabs%d_%d" % (u, i)) for i in range(4)] for u in range(2)]
    QAb_t = [[T("QAb%d_%d" % (u, g)) for g in range(16)] for u in range(2)]
    ka_l = [T("kal%d" % i) for i in range(4)]
    va_l = [T("val%d" % i) for i in range(4)]
    gm, gm_t = dbl("gm", [128, 32], F32)
    sel, sel_t = dbl("sel", [128, 32], F32)
    top8, top8_t = dbl("top8", [128, 8], F32)
    mq, mq_t = dbl("mq", [128, 1], F32)
    BT, BT_t = dbl("BT", [128, 4, 96], F32)
    for i in range(2):
        P.op("pool", lambda e, i=i: e.memset(BT[i][:, :, :], 0.0), writes=[BT_t[i]])
    Pb = [P.sb("Pb%d" % i, [128, 2, 512], BF16) for i in range(3)]
    Pb_t = [T("Pb%d" % i) for i in range(3)]
    OS, OS_t = dbl("OS", [128, 512], F32)
    DN, DN_t = dbl("DN", [64, 512], F32)
    OTs, OTs_t = dbl("OTs", [64, 512], BF16)
    pG = pqT[:, :, :].rearrange("p g n -> p (g n)").bitcast(F32)
    pG_t = pqT_t
    pBT, pBT_t = pv, pv_t
    po, po_t = pqk, pqk_t
    cnt = {"kS": 0, "kP": 0, "gk": 0}

    def load_q(h, u):
        for q4 in range(4):
            sl = slice(q4 * 2048, (q4 + 1) * 2048)
            P.dma("sp", QA2[u][0:64, sl], qTs[h, :, sl], reads=scr_t, writes=[qa_l[u][q4]])
            P.op("act", lambda e, sl=sl, u=u: e.activation(out=qabs2[u][:, sl], in_=QA2[u][0:64, sl], func=AF.Abs),
                 reads=[qa_l[u][q4]], writes=[qabs_l[u][q4]], n=2048)
        if h == 0:
            pass

    def load_kv(h):
        for q4 in range(4):
            sl = slice(q4 * 2048, (q4 + 1) * 2048)
            P.dma("sp", KA[0:64, sl], kTs[h, :, sl], reads=scr_t, writes=[ka_l[q4]])
            P.dma("pool", VA[:, q4 * 16:(q4 + 1) * 16, 0:64],
                  vs[q4 * 2048:(q4 + 1) * 2048, h * 64:(h + 1) * 64].rearrange("(t p) c -> p t c", p=128),
                  reads=scr_t, writes=[va_l[q4]])

    def gating(h, u, g):
        QAu, qabsu = QA2[u], qabs2[u]
        bt, bt_t = BT[g % 2], BT_t[g % 2]
        for qi in range(4):
            qt = g * 4 + qi
            cur = qt // 2
            k2 = cnt["gk"] % 2
            cnt["gk"] += 1
            csl = slice(qt * 128, (qt + 1) * 128)
            P.op("pe", lambda e, csl=csl: e.matmul(pG[:, 0:32], lhsT=QAu[0:64, csl], rhs=kmeanb[:, h, :],
                                                   start=True, stop=True),
                 reads=[qa_l[u], kmeanb_t], writes=[pG_t], n=32)
            P.op("pe", lambda e, csl=csl: e.matmul(pG[:, 32:33], lhsT=qabsu[:, csl], rhs=kmaxb[:, h:h + 1],
                                                   start=True, stop=True),
                 reads=[qabs_l[u], kmaxb_t], writes=[pG_t], n=8)
            P.op("dve", lambda e, k2=k2, cur=cur: e.tensor_tensor(out=gm[k2][:, :], in0=pG[:, 0:32],
                                                                  in1=M1[:, cur * 32:(cur + 1) * 32], op=ALU.add),
                 reads=[pG_t, cst_t], writes=[gm_t[k2]], n=32)
            P.op("dve", lambda e, k2=k2: e.tensor_copy(out=mq[k2][:, :], in_=pG[:, 32:33]),
                 reads=[pG_t], writes=[mq_t[k2]], n=1)
            P.op("dve", lambda e, k2=k2: e.max(out=top8[k2][:, :], in_=gm[k2][:, :]),
                 reads=[gm_t[k2]], writes=[top8_t[k2]], n=32)
            P.op("dve", lambda e, k2=k2: e.tensor_scalar(out=sel[k2][:, :], in0=gm[k2][:, :],
                                                         scalar1=top8[k2][:, 2:3], scalar2=1.0,
                                                         op0=ALU.is_ge, op1=ALU.subtract),
                 reads=[gm_t[k2], top8_t[k2]], writes=[sel_t[k2]], n=32)
            P.op("dve", lambda e, k2=k2, cur=cur: e.tensor_tensor(out=sel[k2][:, :], in0=sel[k2][:, :],
                                                                  in1=A30[:, cur * 32:(cur + 1) * 32], op=ALU.mult),
                 reads=[sel_t[k2], cst_t], writes=[sel_t[k2]], n=32)
            P.op("dve", lambda e, k2=k2, cur=cur: e.tensor_tensor(out=sel[k2][:, :], in0=sel[k2][:, :],
                                                                  in1=Bc[:, cur * 32:(cur + 1) * 32], op=ALU.add),
                 reads=[sel_t[k2], cst_t], writes=[sel_t[k2]], n=32)
            P.op("dve", lambda e, k2=k2, qi=qi: e.tensor_scalar(out=bt[:, qi, 64:96], in0=sel[k2][:, :],
                                                                scalar1=mq[k2][:, 0:1], scalar2=None,
                                                                op0=ALU.subtract),
                 reads=[sel_t[k2], mq_t[k2]], writes=[bt_t], n=32)
            P.op("pe", lambda e, qi=qi: e.transpose(out=pBT[0:96, qi * 128:(qi + 1) * 128], in_=bt[:, qi, :],
                                                    identity=identf[:, :]),
                 reads=[bt_t, cst_t], writes=[pBT_t], n=512)
        P.op("act", lambda e: e.activation(out=QAu[64:96, g * 512:(g + 1) * 512], in_=pBT[64:96, :], func=AF.Copy),
             reads=[pBT_t], writes=[QAb_t[u][g]], n=512)

    def attention(h, u, g):
        QAu = QA2[u]
        nk = 4 * g + 4
        for kp in range(nk // 2):
            ps, ps_t = pS[cnt["kS"] % 2], pS_t[cnt["kS"] % 2]
            cnt["kS"] += 1
            pbuf, pbuf_t = Pb[cnt["kP"] % 3], Pb_t[cnt["kP"] % 3]
            cnt["kP"] += 1
            c0s = []
            for uu in range(2):
                kt = 2 * kp + uu
                i = kt - 4 * g
                c0 = max(i, 0) * 128
                c0s.append(c0)
                P.op("pe", lambda e, ps=ps, kt=kt, c0=c0, i=i, uu=uu: e.matmul(
                    ps[:, uu, c0:512], lhsT=KA[0:96, kt * 128:(kt + 1) * 128],
                    rhs=QAu[0:96, g * 512 + c0:(g + 1) * 512], start=True, stop=(i < 0)),
                    reads=[ka_l, KAoh_t, qa_l[u], QAb_t[u][g]], writes=[ps_t], n=512 - c0)
                if i >= 0:
                    P.op("pe", lambda e, ps=ps, c0=c0, uu=uu: e.matmul(ps[:, uu, c0:c0 + 128], lhsT=ident[:, :],
                                                                       rhs=tri[:, :], start=False, stop=True),
                         reads=[ident_t, cst_t], writes=[ps_t])
            cm = c0s[0]
            P.op("act", lambda e, ps=ps, pbuf=pbuf, cm=cm: e.activation(out=pbuf[:, :, cm:512], in_=ps[:, :, cm:512],
                                                                        func=AF.Exp),
                 reads=[ps_t], writes=[pbuf_t], n=2 * (512 - cm))
            for uu in range(2):
                kt = 2 * kp + uu
                c0 = c0s[uu]
                P.op("pe", lambda e, pbuf=pbuf, kt=kt, c0=c0, uu=uu: e.matmul(
                    po[:, c0:512], lhsT=VA[:, kt, :], rhs=pbuf[:, uu, c0:512], start=(kt == 0), stop=(kt == nk - 1)),
                    reads=[va_l, VA1_t, pbuf_t], writes=[po_t], n=512 - c0)
        o = g % 2
        P.op("act", lambda e, o=o: e.activation(out=OS[o][:, :], in_=po[:, :], func=AF.Copy),
             reads=[po_t], writes=[OS_t[o]], n=512)
        P.dma("sp", DN[o][:, :], OS[o][64:128, :], reads=[OS_t[o]], writes=[DN_t[o]])
        P.op("dve", lambda e, o=o: e.reciprocal(out=DN[o][:, :], in_=DN[o][:, :]), reads=[DN_t[o]], writes=[DN_t[o]],
             n=512)
        P.op("dve", lambda e, o=o: e.tensor_tensor(out=OTs[o][:, :], in0=OS[o][0:64, :], in1=DN[o][:, :], op=ALU.mult),
             reads=[OS_t[o], DN_t[o]], writes=[OTs_t[o]], n=512)
        P.dma("sp", oT[h * 64:(h + 1) * 64, g * 512:(g + 1) * 512], OTs[o][:, :], reads=[OTs_t[o]],
              writes=[T("oT")], final=True)

    load_q(0, 0)
    for g in range(16):
        gating(0, 0, g)
    for h in range(4):
        u = h % 2
        load_kv(h)
        if h < 3:
            load_q(h + 1, 1 - u)
        for g in range(16):
            attention(h, u, g)
            if h < 3:
                gating(h + 1, 1 - u, g)
    return P.build()


_PROGS = {}


def _prog(name):
    if name not in _PROGS:
        _PROGS[name] = {"attn": build_attn, "mlstm": build_mlstm,
                        "pf0": lambda: build_projffn(False), "pf1": lambda: build_projffn(True)}[name]()
    return _PROGS[name]


def _run(name, maps):
    res = run_bass_kernel_spmd(_prog(name), maps, core_ids=list(range(NCORE)))
    return res.results


def _gain_layout(g):
    return np.ascontiguousarray(np.asarray(g, np.float32).reshape(8, 128).T)


def _consts():
    bf = ml_dtypes.bfloat16
    c = {}
    c["ident"] = np.eye(128, dtype=np.float32).astype(bf)
    c["identf"] = np.eye(128, dtype=np.float32)
    pos = np.arange(S, dtype=np.float32)
    inv = (np.float32(500000.0) ** (-np.arange(0, 16, 2, dtype=np.float32) / np.float32(16))).astype(np.float32)
    ang = (pos[:, None] * inv[None, :]).astype(np.float32)
    cos = np.cos(ang).astype(np.float32)
    sin = np.sin(ang).astype(np.float32)
    c["cs"] = np.ascontiguousarray(np.concatenate([np.tile(cos, (1, 8)), np.tile(sin, (1, 8))], axis=1))
    blk = np.arange(S) // 256
    c["onehot"] = (blk[None, :] == np.arange(32)[:, None]).astype(np.float32).astype(bf)
    kk = np.arange(128)
    c["tri"] = np.where(kk[:, None] > kk[None, :], NEG, 0.0).astype(np.float32).astype(bf)
    cur = np.arange(32)[:, None]
    n = np.arange(32)[None, :]
    m1 = np.where(n < cur, 0.0, NEG).astype(np.float32).reshape(1, 1024)
    a30 = np.where(n < cur, -NEG, 0.0).astype(np.float32).reshape(1, 1024)
    bc = np.where(n <= cur, 0.0, NEG).astype(np.float32).reshape(1, 1024)
    c["M1"] = np.ascontiguousarray(np.tile(m1, (128, 1)))
    c["A30"] = np.ascontiguousarray(np.tile(a30, (128, 1)))
    c["Bc"] = np.ascontiguousarray(np.tile(bc, (128, 1)))
    c["U"] = np.triu(np.ones((128, 128), np.float32))
    c["ones"] = np.ones((128, 128), np.float32)
    return c


def kernel(x, attn_norm, attn_w_qkv, attn_w_o, mlstm_norm, mlstm_w_in, mlstm_b_gates,
           mlstm_head_norm, mlstm_w_out, ffn_norm, ffn_w_gate_up, ffn_w_down, final_norm):
    f32 = np.float32
    x = np.asarray(x, f32)
    c = _consts()
    wqkv = np.asarray(attn_w_qkv, f32)[0]
    maps = []
    for core in range(NCORE):
        b, hg = core // 4, core % 4
        cols = np.concatenate([np.arange(hg * 256, (hg + 1) * 256) + off for off in (0, 1024, 2048)])
        maps.append({"xin": x[b], "gn": _gain_layout(attn_norm[0]), "wqkv": np.ascontiguousarray(wqkv[:, cols]),
                     "cs": c["cs"], "ident": c["ident"], "identf": c["identf"], "onehot": c["onehot"],
                     "tri": c["tri"], "M1": c["M1"], "A30": c["A30"], "Bc": c["Bc"]})
    ra = _run("attn", maps)
    oT = [np.concatenate([ra[b * 4 + hg]["oT"] for hg in range(4)], axis=0) for b in range(B)]
    maps = []
    for core in range(NCORE):
        b, q = core // 4, core % 4
        sl = slice(q * 2048, (q + 1) * 2048)
        maps.append({"hin": x[b, sl], "aT": np.ascontiguousarray(oT[b][:, sl]), "wp": np.asarray(attn_w_o, f32)[0],
                     "gn": _gain_layout(ffn_norm[0]), "wgu": np.asarray(ffn_w_gate_up, f32)[0],
                     "wd": np.asarray(ffn_w_down, f32)[0], "ident": c["ident"]})
    rb = _run("pf0", maps)
    h1 = [np.concatenate([rb[b * 4 + q]["hout"] for q in range(4)], axis=0) for b in range(B)]
    win = np.asarray(mlstm_w_in, f32)[0]
    bgv = np.asarray(mlstm_b_gates, f32)[0]
    hnv = np.asarray(mlstm_head_norm, f32)[0]
    maps = []
    for core in range(NCORE):
        b, hp = core // 4, core % 4
        h0 = 2 * hp
        wq = win[:, h0 * 64:(h0 + 2) * 64]
        wk = win[:, 512 + h0 * 64:512 + (h0 + 2) * 64]
        wv = win[:, 1024 + h0 * 128:1024 + (h0 + 2) * 128]
        wo = win[:, 2048 + h0 * 128:2048 + (h0 + 2) * 128]
        gi = win[:, 3072 + h0:3072 + h0 + 2]
        gf = win[:, 3080 + h0:3080 + h0 + 2]
        wtok = np.ascontiguousarray(np.concatenate([wk, wv, wq, wo, gi, gf], axis=1))
        bg4 = np.concatenate([bgv[h0:h0 + 2], bgv[8 + h0:8 + h0 + 2]])
        maps.append({"hin": h1[b], "gn": _gain_layout(mlstm_norm[0]), "wtok": wtok,
                     "bg": np.ascontiguousarray(np.tile(bg4[None, :], (128, 1))),
                     "hn": np.ascontiguousarray(np.tile(hnv[None, h0 * 128:(h0 + 2) * 128], (128, 1))),
                     "ident": c["ident"], "U": c["U"], "ones": c["ones"]})
    rc = _run("mlstm", maps)
    yT = [np.ascontiguousarray(np.concatenate([rc[b * 4 + hp]["y"] for hp in range(4)], axis=1).T)
          for b in range(B)]
    maps = []
    for core in range(NCORE):
        b, q = core // 4, core % 4
        sl = slice(q * 2048, (q + 1) * 2048)
        maps.append({"hin": np.ascontiguousarray(h1[b][sl]), "aT": np.ascontiguousarray(yT[b][:, sl]),
                     "wp": np.asarray(mlstm_w_out, f32)[0], "gn": _gain_layout(ffn_norm[1]),
                     "wgu": np.asarray(ffn_w_gate_up, f32)[1], "wd": np.asarray(ffn_w_down, f32)[1],
                     "ident": c["ident"], "fnw": np.asarray(final_norm, f32)})
    rd = _run("pf1", maps)
    out = np.stack([np.concatenate([rd[b * 4 + q]["hout"] for q in range(4)], axis=0) for b in range(B)])
    return out.astype(f32)
```
